# Optimizing a Trainium2 kernel written in Bass

```python
import jax
import jax.numpy as jnp
from jax import lax
import numpy as np

D_MODEL = 2048
BATCH = 1
SEQ = 16384
DEPTH = 1

EPS = 1e-6

ATTN_Q_HEADS = 8
ATTN_KV_HEADS = 2
ATTN_HEAD_DIM = 128
ATTN_GROUP = ATTN_Q_HEADS // ATTN_KV_HEADS
WINDOW = 128
ATTN_BLOCK = 128
ROPE_THETA = 500000.0
ROPE_DIM = ATTN_HEAD_DIM // 4
ATTN_WIDTH = ATTN_Q_HEADS * ATTN_HEAD_DIM

GLA_HEADS = 4
GLA_DK = 128
GLA_DV = 256
GLA_GATE_RANK = 16
GLA_GATE_TAU = 16.0
GLA_CHUNK = 64
GLA_WIDTH = GLA_HEADS * GLA_DV

MIX_WIDTH = ATTN_WIDTH + GLA_WIDTH

IN_SPLIT_SIZES = (ATTN_WIDTH, ATTN_KV_HEADS * ATTN_HEAD_DIM, ATTN_KV_HEADS * ATTN_HEAD_DIM,
                  GLA_HEADS * GLA_DK, GLA_HEADS * GLA_DK, GLA_WIDTH, GLA_WIDTH,
                  GLA_GATE_RANK, GLA_GATE_RANK)
IN_WIDTH = sum(IN_SPLIT_SIZES)
IN_SPLIT_POINTS = tuple(int(v) for v in np.cumsum(IN_SPLIT_SIZES)[:-1])

N_GROUPS = 4
EXPERTS_PER_GROUP = 8
N_EXPERTS = N_GROUPS * EXPERTS_PER_GROUP
TOP_K = 2
D_FF_EXPERT = 1024
MOE_BLOCK = 128

kernel_name = 'hymba_swa_gla_hier_moe'


def rmsnorm(x, w):
    xf = x.astype(jnp.float32)
    y = xf * lax.rsqrt(jnp.mean(xf * xf, axis=-1, keepdims=True) + EPS)
    return (y * w.astype(jnp.float32)).astype(x.dtype)


def partial_rope(x, pos):
    half = ROPE_DIM // 2
    inv_freq = jnp.power(jnp.float32(ROPE_THETA), -jnp.arange(half, dtype=jnp.float32) * (2.0 / ROPE_DIM))
    ang = pos.astype(jnp.float32)[:, None] * inv_freq[None, :]
    cos = jnp.cos(ang)[None, :, None, :]
    sin = jnp.sin(ang)[None, :, None, :]
    xf = x.astype(jnp.float32)
    x1 = xf[..., :half]
    x2 = xf[..., half:ROPE_DIM]
    out = jnp.concatenate([x1 * cos - x2 * sin, x2 * cos + x1 * sin, xf[..., ROPE_DIM:]], axis=-1)
    return out.astype(x.dtype)


def windowed_gqa(q, k, v, sink):
    B, S = q.shape[0], q.shape[1]
    nb = S // ATTN_BLOCK
    qb = q.reshape(B, nb, ATTN_BLOCK, ATTN_KV_HEADS, ATTN_GROUP, ATTN_HEAD_DIM)

    def band(t):
        tp = jnp.pad(t, ((0, 0), (ATTN_BLOCK, ATTN_BLOCK), (0, 0), (0, 0)))
        tp = tp.reshape(B, nb + 2, ATTN_BLOCK, ATTN_KV_HEADS, ATTN_HEAD_DIM)
        return jnp.concatenate([tp[:, :-2], tp[:, 1:-1], tp[:, 2:]], axis=2)

    kb = band(k)
    vb = band(v)
    s = jnp.einsum('bnqhgd,bnkhd->bnhgqk', qb, kb,
                   preferred_element_type=jnp.float32) * (ATTN_HEAD_DIM ** -0.5)
    blk = jnp.arange(nb)
    qpos = blk[:, None] * ATTN_BLOCK + jnp.arange(ATTN_BLOCK)[None, :]
    kpos = (blk[:, None] - 1) * ATTN_BLOCK + jnp.arange(3 * ATTN_BLOCK)[None, :]
    rel = kpos[:, None, :] - qpos[:, :, None]
    valid = (jnp.abs(rel) <= WINDOW) & (kpos[:, None, :] >= 0) & (kpos[:, None, :] < S)
    s = jnp.where(valid[None, :, None, None], s, -jnp.inf)
    sink_col = jnp.broadcast_to(
        sink.astype(jnp.float32).reshape(1, 1, ATTN_KV_HEADS, ATTN_GROUP, 1, 1), s.shape[:-1] + (1,))
    p = jax.nn.softmax(jnp.concatenate([s, sink_col], axis=-1), axis=-1)[..., :-1]
    o = jnp.einsum('bnhgqk,bnkhd->bnqhgd', p.astype(v.dtype), vb)
    return o.reshape(B, S, ATTN_WIDTH)


def gla_chunked(q, k, v, log_a):
    B, S, H, dk = q.shape
    dv = v.shape[-1]
    n = S // GLA_CHUNK

    def chunks(t):
        return t.reshape(B, n, GLA_CHUNK, H, t.shape[-1]).transpose(0, 3, 1, 2, 4).astype(jnp.float32)

    q, k, v, log_a = chunks(q), chunks(k), chunks(v), chunks(log_a)
    b = jnp.cumsum(log_a, axis=-2)
    q_dec = q * jnp.exp(b)
    k_inv = k * jnp.exp(-b)
    k_end = k * jnp.exp(b[..., -1:, :] - b)
    tri = jnp.tril(jnp.ones((GLA_CHUNK, GLA_CHUNK), dtype=bool))
    att = jnp.where(tri, jnp.einsum('bhncd,bhnjd->bhncj', q_dec, k_inv), 0.0)
    o_intra = jnp.einsum('bhncj,bhnjv->bhncv', att, v)
    d_state = jnp.einsum('bhncd,bhncv->bhndv', k_end, v)
    chunk_decay = jnp.exp(b[..., -1, :])

    def step(state, inp):
        decay_n, ds_n = inp
        return decay_n[..., None] * state + ds_n, state

    state0 = jnp.zeros((B, H, dk, dv), jnp.float32)
    _, s_in = lax.scan(step, state0, (jnp.moveaxis(chunk_decay, 2, 0), jnp.moveaxis(d_state, 2, 0)))
    s_in = jnp.moveaxis(s_in, 0, 2)
    o = o_intra + jnp.einsum('bhncd,bhndv->bhncv', q_dec, s_in)
    return o.transpose(0, 2, 3, 1, 4).reshape(B, S, H, dv)


def gla_bidirectional(q, k, v, r, lr_f, lr_b, up_f, bias_f, up_b, bias_b, out_norm_w):
    B, S = q.shape[0], q.shape[1]
    q = q.reshape(B, S, GLA_HEADS, GLA_DK) * (GLA_DK ** -0.5)
    k = k.reshape(B, S, GLA_HEADS, GLA_DK)
    v = v.reshape(B, S, GLA_HEADS, GLA_DV)

    def log_gate(lr, up, bias):
        g = jnp.einsum('bsr,rc->bsc', lr, up) + bias
        return (jax.nn.log_sigmoid(g.astype(jnp.float32)) / GLA_GATE_TAU).reshape(B, S, GLA_HEADS, GLA_DK)

    flip = lambda t: jnp.flip(t, axis=1)
    o_f = gla_chunked(q, k, v, log_gate(lr_f, up_f, bias_f))
    o_b = flip(gla_chunked(flip(q), flip(k), flip(v), flip(log_gate(lr_b, up_b, bias_b))))
    o = rmsnorm(o_f + o_b, out_norm_w).reshape(B, S, GLA_WIDTH)
    return (o * jax.nn.silu(r.astype(jnp.float32))).astype(r.dtype)


def hybrid_mixer(x, ln_w, w_in, q_norm_w, k_norm_w, sink, attn_norm_w,
                 gate_up_f, gate_bias_f, gate_up_b, gate_bias_b, gla_norm_w, w_out):
    B, S, _ = x.shape
    xn = rmsnorm(x, ln_w)
    proj = xn @ w_in
    aq, ak, av, gq, gk, gv, gr, lrf, lrb = jnp.split(proj, IN_SPLIT_POINTS, axis=-1)
    pos = jnp.arange(S)
    aq = partial_rope(rmsnorm(aq.reshape(B, S, ATTN_Q_HEADS, ATTN_HEAD_DIM), q_norm_w), pos)
    ak = partial_rope(rmsnorm(ak.reshape(B, S, ATTN_KV_HEADS, ATTN_HEAD_DIM), k_norm_w), pos)
    av = av.reshape(B, S, ATTN_KV_HEADS, ATTN_HEAD_DIM)
    attn = rmsnorm(windowed_gqa(aq, ak, av, sink), attn_norm_w)
    gla = gla_bidirectional(gq, gk, gv, gr, lrf, lrb, gate_up_f, gate_bias_f,
                            gate_up_b, gate_bias_b, gla_norm_w)
    mixed = jnp.concatenate([attn, gla.astype(attn.dtype)], axis=-1)
    return x + mixed @ w_out


def routed_experts(xn, expert_id, weights, w_gate, w_up, w_down):
    T, D = xn.shape
    A = T * TOP_K
    flat_e = expert_id.reshape(A)
    flat_tok = jnp.repeat(jnp.arange(T, dtype=jnp.int32), TOP_K)
    flat_w = weights.reshape(A).astype(jnp.float32)
    counts = jnp.bincount(flat_e, length=N_EXPERTS)
    padded = (counts + MOE_BLOCK - 1) // MOE_BLOCK * MOE_BLOCK
    pad_end = jnp.cumsum(padded)
    pad_start = pad_end - padded
    start = jnp.cumsum(counts) - counts
    order = jnp.argsort(flat_e)
    sorted_e = flat_e[order]
    slot = pad_start[sorted_e] + jnp.arange(A) - start[sorted_e]
    n_blocks = -(-A // MOE_BLOCK) + N_EXPERTS
    n_slots = n_blocks * MOE_BLOCK
    slot_tok = jnp.zeros((n_slots,), jnp.int32).at[slot].set(flat_tok[order])
    slot_w = jnp.zeros((n_slots,), jnp.float32).at[slot].set(flat_w[order])
    block_e = jnp.minimum(jnp.searchsorted(pad_end, jnp.arange(n_blocks) * MOE_BLOCK, side='right'),
                          N_EXPERTS - 1)
    xs = xn[slot_tok].reshape(n_blocks, MOE_BLOCK, D)

    def expert_block(args):
        xb, e = args
        hid = jax.nn.silu(xb @ w_gate[e]) * (xb @ w_up[e])
        return hid @ w_down[e]

    ys = lax.map(expert_block, (xs, block_e)).reshape(n_slots, D)
    ys = ys * slot_w[:, None].astype(ys.dtype)
    return jnp.zeros((T, D), ys.dtype).at[slot_tok].add(ys)


def hier_moe(h, ln_w, w_group, b_group, w_router, b_router, w_gate, w_up, w_down):
    B, S, D = h.shape
    T = B * S
    xn = rmsnorm(h, ln_w).reshape(T, D)
    group_logits = (xn @ w_group).astype(jnp.float32) + b_group.astype(jnp.float32)
    group_p = jax.nn.softmax(group_logits, axis=-1)
    g_sel = jnp.argmax(group_logits, axis=-1)
    g_gate = jnp.take_along_axis(group_p, g_sel[:, None], axis=-1)
    e_logits = ((xn @ w_router).astype(jnp.float32) + b_router.astype(jnp.float32)).reshape(
        T, N_GROUPS, EXPERTS_PER_GROUP)
    in_group = jnp.take_along_axis(e_logits, g_sel[:, None, None], axis=1)[:, 0]
    top_vals, top_idx = lax.top_k(in_group, TOP_K)
    weights = g_gate * jax.nn.softmax(top_vals, axis=-1)
    expert_id = g_sel[:, None].astype(jnp.int32) * EXPERTS_PER_GROUP + top_idx.astype(jnp.int32)
    y = routed_experts(xn, expert_id, weights, w_gate, w_up, w_down)
    return h + y.reshape(B, S, D).astype(h.dtype)


def setup_inputs(seed: int = 0) -> dict:
    key = jax.random.key(seed)
    ks = jax.random.split(key, 21)
    f32 = jnp.float32
    nrm = lambda k, shape, scale: jax.random.normal(k, shape, f32) * scale
    gain = lambda k, shape: 1.0 + 0.02 * jax.random.normal(k, shape, f32)
    return {
        'x': nrm(ks[0], (BATCH, SEQ, D_MODEL), 1.0),
        'ln1_w': gain(ks[1], (DEPTH, D_MODEL)),
        'w_in': nrm(ks[2], (DEPTH, D_MODEL, IN_WIDTH), D_MODEL ** -0.5),
        'q_norm_w': gain(ks[3], (DEPTH, ATTN_HEAD_DIM)),
        'k_norm_w': gain(ks[4], (DEPTH, ATTN_HEAD_DIM)),
        'attn_sink': nrm(ks[5], (DEPTH, ATTN_Q_HEADS), 0.5),
        'attn_out_norm_w': gain(ks[6], (DEPTH, ATTN_WIDTH)),
        'gla_gate_up_f': nrm(ks[7], (DEPTH, GLA_GATE_RANK, GLA_HEADS * GLA_DK), GLA_GATE_RANK ** -0.5),
        'gla_gate_bias_f': nrm(ks[8], (DEPTH, GLA_HEADS * GLA_DK), 0.02),
        'gla_gate_up_b': nrm(ks[9], (DEPTH, GLA_GATE_RANK, GLA_HEADS * GLA_DK), GLA_GATE_RANK ** -0.5),
        'gla_gate_bias_b': nrm(ks[10], (DEPTH, GLA_HEADS * GLA_DK), 0.02),
        'gla_out_norm_w': gain(ks[11], (DEPTH, GLA_DV)),
        'w_out': nrm(ks[12], (DEPTH, MIX_WIDTH, D_MODEL), MIX_WIDTH ** -0.5),
        'ln2_w': gain(ks[13], (DEPTH, D_MODEL)),
        'w_group': nrm(ks[14], (DEPTH, D_MODEL, N_GROUPS), D_MODEL ** -0.5),
        'b_group': nrm(ks[15], (DEPTH, N_GROUPS), 0.01),
        'w_router': nrm(ks[16], (DEPTH, D_MODEL, N_EXPERTS), D_MODEL ** -0.5),
        'b_router': nrm(ks[17], (DEPTH, N_EXPERTS), 0.01),
        'w_gate_e': nrm(ks[18], (DEPTH, N_EXPERTS, D_MODEL, D_FF_EXPERT), D_MODEL ** -0.5),
        'w_up_e': nrm(ks[19], (DEPTH, N_EXPERTS, D_MODEL, D_FF_EXPERT), D_MODEL ** -0.5),
        'w_down_e': nrm(ks[20], (DEPTH, N_EXPERTS, D_FF_EXPERT, D_MODEL), D_FF_EXPERT ** -0.5),
    }


def reference(x, ln1_w, w_in, q_norm_w, k_norm_w, attn_sink, attn_out_norm_w,
              gla_gate_up_f, gla_gate_bias_f, gla_gate_up_b, gla_gate_bias_b, gla_out_norm_w,
              w_out, ln2_w, w_group, b_group, w_router, b_router, w_gate_e, w_up_e, w_down_e):
    h = x
    for l in range(DEPTH):
        h = hybrid_mixer(h, ln1_w[l], w_in[l], q_norm_w[l], k_norm_w[l], attn_sink[l],
                         attn_out_norm_w[l], gla_gate_up_f[l], gla_gate_bias_f[l],
                         gla_gate_up_b[l], gla_gate_bias_b[l], gla_out_norm_w[l], w_out[l])
        h = hier_moe(h, ln2_w[l], w_group[l], b_group[l], w_router[l], b_router[l],
                     w_gate_e[l], w_up_e[l], w_down_e[l])
    return h
```

```python
import math
import os
from contextlib import ExitStack

import numpy as np
import ml_dtypes
import concourse.bass as bass
import concourse.mybir as mybir
from concourse.bass_utils import run_bass_kernel_spmd

F32 = mybir.dt.float32
BF16 = mybir.dt.bfloat16
I32 = mybir.dt.int32
NBLK = 64
ALU = mybir.AluOpType
AF = mybir.ActivationFunctionType
AX = mybir.AxisListType

KDBG = os.environ.get('KDBG', '')
NCORES = 8
SEQ = 16384
D = 2048
TPC = SEQ // NCORES
NT = TPC // 128
NTH = NT + 2
NPRE = (NCORES - 1) * NT
INW = 4640
NE = 32
DFF = 1024
EPS = 1e-6
C_AQ, C_AK, C_AV, C_GQ, C_GK, C_GV, C_GR, C_LR = 0, 1024, 1280, 1536, 2048, 2560, 3584, 4608


class Eng:
    def __init__(self, fw, name, handle):
        self.name = name
        self.h = handle
        self.sem = fw.new_sem("s_" + name)
        self.count = 0
        self.seen = {}
        self.pend = []


class Buf:
    def __init__(self, fw, t, name=""):
        self.t = t
        self.name = name
        self.last_w = None
        self.reads = []
        self.dsem = None
        self.dcount = 0
        self.is_psum = False

    def __getitem__(self, idx):
        return self.t[idx]


class FW:
    def __init__(self, nc, stack):
        self.nc = nc
        self.gstack = stack
        self.nsem = 0
        self.pe = Eng(self, "pe", nc.tensor)
        self.dve = Eng(self, "dve", nc.vector)
        self.act = Eng(self, "act", nc.scalar)
        self.pool = Eng(self, "pool", nc.gpsimd)
        self.sp = Eng(self, "sp", nc.sync)
        self.pe.seen[id(self.pe.sem)] = 1 << 60
        self.engs = [self.pe, self.dve, self.act, self.pool, self.sp]
        self.dbufs = []
        self.n_inst = 0
        self.n_wait = 0

    def new_sem(self, name):
        self.nsem += 1
        return self.gstack.enter_context(self.nc.semaphore("%s_%d" % (name, self.nsem)))

    def sbuf(self, st, name, shape, dt):
        return Buf(self, st.enter_context(self.nc.sbuf_tensor(name, list(shape), dt)), name)

    def psum(self, st, name, shape, dt):
        b = Buf(self, st.enter_context(self.nc.psum_tensor(name, list(shape), dt)), name)
        b.is_psum = True
        return b

    def _wait(self, eng, dep):
        if dep is None:
            return
        sem, val = dep
        key = id(sem)
        if eng.seen.get(key, 0) >= val:
            return
        eng.h.wait_ge(sem, val)
        eng.seen[key] = val
        self.n_wait += 1

    def _deps(self, eng, reads, writes):
        for b in reads:
            self._wait(eng, b.last_w)
            if b.is_psum:
                for r in b.reads:
                    self._wait(eng, r)
        for b in writes:
            self._wait(eng, b.last_w)
            for r in b.reads:
                self._wait(eng, r)

    def op(self, eng, fn, reads=(), writes=(), inc=True):
        self._deps(eng, reads, writes)
        inst = fn()
        self.n_inst += 1
        eng.pend.append((tuple(reads), tuple(writes)))
        if not inc:
            return inst
        eng.count += 1
        inst.then_inc(eng.sem, 1)
        tok = (eng.sem, eng.count)
        for rs, ws in eng.pend:
            for b in rs:
                b.reads.append(tok)
            for b in ws:
                b.last_w = tok
                b.reads = []
        eng.pend = []
        return inst

    def v(self, fn, reads=(), writes=()):
        return self.op(self.dve, fn, reads, writes)

    def a(self, fn, reads=(), writes=()):
        return self.op(self.act, fn, reads, writes)

    def mm(self, outB, out_ap, lB, l_ap, rB, r_ap, start=True, stop=True):
        nc = self.nc
        return self.op(self.pe, lambda: nc.tensor.matmul(out_ap, lhsT=l_ap, rhs=r_ap, start=start, stop=stop),
                       reads=[lB, rB], writes=[outB], inc=stop)

    def tr(self, outB, out_ap, inB, in_ap, idB):
        nc = self.nc
        return self.op(self.pe, lambda: nc.tensor.transpose(out=out_ap, in_=in_ap, identity=idB[:]),
                       reads=[inB, idB], writes=[outB])

    def dma(self, eng, out_ap, in_ap, reads=(), writes=()):
        self._deps(eng, reads, writes)
        owner = writes[0] if writes else reads[0]
        if owner.dsem is None:
            owner.dsem = self.new_sem("d_" + owner.name)
            self.dbufs.append(owner)
        inst = eng.h.dma_start(out=out_ap, in_=in_ap)
        owner.dcount += 16
        inst.then_inc(owner.dsem, 16)
        tok = (owner.dsem, owner.dcount)
        for b in reads:
            b.reads.append(tok)
        for b in writes:
            b.last_w = tok
            b.reads = []
        self.n_inst += 1
        return inst

    def barrier(self):
        for e in self.engs:
            assert not e.pend
        for e in self.engs:
            for o in self.engs:
                if o is not e and o.count > 0:
                    self._wait(e, (o.sem, o.count))
            for b in self.dbufs:
                self._wait(e, (b.dsem, b.dcount))

    def finish(self, eng, bufs):
        for b in bufs:
            self._wait(eng, b.last_w)
            for r in b.reads:
                self._wait(eng, r)


def _scope(flag):
    if flag:
        with ExitStack() as st:
            yield st


def build_program(ne=NE, npre=NPRE, phases=("pre", "xn", "attn", "gla", "out", "moe"), sparse=True):
    nc = bass.Bass("TRN2", target_bir_lowering=False)
    dt_in = {}

    def din(name, shape, dt=F32):
        t = nc.dram_tensor(name, list(shape), dt, kind="ExternalInput")
        dt_in[name] = t
        return t

    xh = din("xh", [NTH * 128, D])
    xpre = din("xpre", [max(npre, 1) * 128, D])
    pre_sc = din("pre_sc", [64, max(npre, 1)])
    pre_bi = din("pre_bi", [64, max(npre, 1)])
    pre_m = din("pre_m", [128, max(npre, 1), 2])
    pre_f = din("pre_f", [128, max(npre, 1), 3])
    main_bi = din("main_bi", [64, 1])
    cosq = din("cosq", [NTH * 128, 128])
    sinq = din("sinq", [NTH * 128, 128])
    maskP = din("maskP", [128, 512], BF16)
    maskN = din("maskN", [128, 512], BF16)
    maskP0 = din("maskP0", [128, 512], BF16)
    maskNL = din("maskNL", [128, 512], BF16)
    tri = din("tri", [4, 128, 128])
    ident_b = din("ident_b", [128, 128], BF16)
    ident_f = din("ident_f", [128, 128])
    ones_c = din("ones_c", [128, 1])
    ln1_bc = din("ln1_bc", [128, D])
    w_in = din("w_in", [D, INW])
    w_lr = din("w_lr", [D, 64])
    qn_bc = din("qn_bc", [128, 128])
    kn_bc = din("kn_bc", [128, 128])
    sink_bc = din("sink_bc", [128, 8])
    an_bc = din("an_bc", [128, 1024])
    up_pad = din("up_pad", [64, 512])
    gn_bc = din("gn_bc", [128, 256])
    w_out = din("w_out", [D, D])
    ln2_bc = din("ln2_bc", [128, D])
    w_rt = din("w_rt", [D, 36])
    b_rt = din("b_rt", [128, 36])
    if ne > 0:
        wg = din("wg", [2 * ne * 128, 8192])
        wu = din("wu", [2 * ne * 128, 8192])
        wd = din("wd", [2 * ne * 128, 8192])
    y = nc.dram_tensor("y", [TPC, D], F32, kind="ExternalOutput")
    mix_d = nc.dram_tensor("mix_d", [TPC, D], BF16, kind="Internal")
    h_d = nc.dram_tensor("h_d", [TPC, D], F32, kind="Internal")
    xs_d = nc.dram_tensor("xs_d", [NT, 128, 16, 128], BF16, kind="Internal")
    xn2_d = nc.dram_tensor("xn2_d", [TPC, D], BF16, kind="Internal")
    xsort_d = nc.dram_tensor("xsort_d", [NBLK * 128, D], BF16, kind="Internal")
    oall_d = nc.dram_tensor("oall_d", [NBLK * 128, D], F32, kind="Internal")
    ones_m = din("ones_m", [128, 128])
    if ne > 0 and sparse:
        wgc = nc.dram_tensor("wgc", [2 * ne * 128, 8192], BF16, kind="Internal")
        wuc = nc.dram_tensor("wuc", [2 * ne * 128, 8192], BF16, kind="Internal")
        wdc = nc.dram_tensor("wdc", [2 * ne * 128, 8192], BF16, kind="Internal")
    iota_p = din("iota_p", [128, 1])

    V, A = nc.vector, nc.scalar
    QS = 1.0 / math.sqrt(128.0)

    with ExitStack() as gst:
        fw = FW(nc, gst)
        DB = {k: Buf(fw, t, k) for k, t in dt_in.items()}
        yB = Buf(fw, y, "y"); mixB = Buf(fw, mix_d, "mix_d"); hB = Buf(fw, h_d, "h_d"); xsB = Buf(fw, xs_d, "xs_d")
        xn2B = Buf(fw, xn2_d, "xn2_d"); xsortB = Buf(fw, xsort_d, "xsort_d"); oallB = Buf(fw, oall_d, "oall_d")
        conv_jobs = []
        if ne > 0 and sparse and "moe" in phases:
            wgcB = Buf(fw, wgc, "wgc"); wucB = Buf(fw, wuc, "wuc"); wdcB = Buf(fw, wdc, "wdc")
            for r0 in range(0, 2 * ne * 128, 128):
                for (src, nm, dst, dB) in ((wg, "wg", wgc, wgcB), (wu, "wu", wuc, wucB), (wd, "wd", wdc, wdcB)):
                    conv_jobs.append((src, nm, dst, dB, r0))

        def pump_conv(n):
            for _ in range(min(n, len(conv_jobs))):
                src, nm, dst, dB, r0 = conv_jobs.pop(0)
                fw.dma(pool, dst[r0:r0 + 128, :], src[r0:r0 + 128, :], reads=[DB[nm]], writes=[dB])
        sp, pool = fw.sp, fw.pool

        idb = fw.sbuf(gst, "idb", [128, 128], BF16)
        idf = fw.sbuf(gst, "idf", [128, 128], F32)
        ones = fw.sbuf(gst, "ones", [128, 1], F32)
        trif = fw.sbuf(gst, "trif", [128, 4, 128], F32)
        upp = fw.sbuf(gst, "upp", [64, 512], BF16)
        Sst = [[fw.sbuf(gst, "S_%d_%d" % (d, h), [128, 256], F32) for h in range(4)] for d in range(2)]
        wE = fw.sbuf(gst, "wE", [128, NT, NE], F32)
        A1 = fw.sbuf(gst, "A1", [128, NT, NE], F32)
        A2 = fw.sbuf(gst, "A2", [128, NT, NE], F32)
        W01 = fw.sbuf(gst, "W01", [128, NT, 2], F32)
        onesm = fw.sbuf(gst, "onesm", [128, 128], F32)
        fw.dma(sp, onesm[:], ones_m[:, :], reads=[DB["ones_m"]], writes=[onesm])
        fw.dma(sp, idb[:], ident_b[:, :], reads=[DB["ident_b"]], writes=[idb])
        fw.dma(sp, idf[:], ident_f[:, :], reads=[DB["ident_f"]], writes=[idf])
        fw.dma(sp, ones[:], ones_c[:, :], reads=[DB["ones_c"]], writes=[ones])
        fw.dma(sp, trif[:], tri.ap().rearrange("m p n -> p m n"), reads=[DB["tri"]], writes=[trif])
        fw.dma(pool, upp[:], up_pad[:, :], reads=[DB["up_pad"]], writes=[upp])
        for d in range(2):
            for h in range(4):
                fw.v(lambda: V.memset(Sst[d][h][:], 0.0), writes=[Sst[d][h]])
        trib = fw.sbuf(gst, "trib", [128, 4, 128], BF16)
        onesb = fw.sbuf(gst, "onesb", [128, 1], BF16)
        fw.v(lambda: V.tensor_copy(out=trib[:], in_=trif[:]), reads=[trif], writes=[trib])
        fw.v(lambda: V.tensor_copy(out=onesb[:], in_=ones[:]), reads=[ones], writes=[onesb])

        def rms_rstd(src_ap, srcB, junk, ss, n):
            fw.a(lambda: A.activation(out=junk[:, 0:n], in_=src_ap, func=AF.Square, accum_out=ss[:, 0:1]),
                 reads=[srcB], writes=[junk, ss])
            fw.a(lambda: A.activation(out=ss[:, 0:1], in_=ss[:, 0:1], func=AF.Sqrt, scale=1.0 / n, bias=EPS),
                 reads=[ss], writes=[ss])
            fw.v(lambda: V.reciprocal(out=ss[:, 0:1], in_=ss[:, 0:1]), reads=[ss], writes=[ss])

        def norm1_tile(st_bufs, x_src_ap, xsrcB, dstT, dst_ap_fn):
            xt, xb, junk, ss, ln1, pT = st_bufs
            fw.dma(sp, xt[:], x_src_ap, reads=[xsrcB], writes=[xt])
            rms_rstd(xt[:], xt, junk, ss, D)
            fw.v(lambda: V.scalar_tensor_tensor(out=xb[:], in0=xt[:], scalar=ss[:, 0:1], in1=ln1[:], op0=ALU.mult, op1=ALU.mult),
                 reads=[xt, ss, ln1], writes=[xb])
            for g in range(4):
                p = pT[g % 2]
                for j in range(4):
                    kc = 4 * g + j
                    fw.tr(p, p[:, j, :], xb, xb[:, kc * 128:(kc + 1) * 128], idb)
                if g % 2 == 0:
                    fw.v(lambda: V.tensor_copy(out=dst_ap_fn(4 * g), in_=p[:]), reads=[p], writes=[dstT])
                else:
                    fw.a(lambda: A.copy(out=dst_ap_fn(4 * g), in_=p[:]), reads=[p], writes=[dstT])

        if npre > 0 and "pre" in phases:
            with ExitStack() as st:
                def two(name, shape, dt):
                    return [fw.sbuf(st, "%s_%d" % (name, i), shape, dt) for i in range(2)]
                xt = two("p1_xt", [128, D], F32); xb = two("p1_xb", [128, D], BF16)
                junk = fw.sbuf(st, "p1_junk", [128, D], F32); ss = two("p1_ss", [128, 4], F32)
                ln1 = fw.sbuf(st, "p1_ln1", [128, D], F32)
                xT = two("p1_xT", [128, 16, 128], BF16)
                wk = fw.sbuf(st, "p1_wk", [128, 16, 512], BF16)
                wv = fw.sbuf(st, "p1_wv", [128, 16, 1024], BF16)
                wl = fw.sbuf(st, "p1_wl", [128, 16, 64], BF16)
                psc = fw.sbuf(st, "p1_psc", [64, npre], F32); pbi = fw.sbuf(st, "p1_pbi", [64, npre], F32)
                pm = fw.sbuf(st, "p1_pm", [128, npre, 2], F32)
                gk = two("p1_gk", [128, 512], F32); gv = two("p1_gv", [128, 1024], BF16)
                lrT = two("p1_lrT", [64, 128], BF16)
                spf = two("p1_sp", [128, 512], BF16); spe = two("p1_spe", [128, 512], F32); e3 = two("p1_e3", [128, 512], F32)
                kend = two("p1_kend", [128, 512], BF16)
                tsel = two("p1_tsel", [128, 128], BF16); ttmp = two("p1_ttmp", [128, 128], F32)
                dec = two("p1_dec", [128, 4], F32); dd = two("p1_dd", [128, 2, 4], F32)
                om = two("p1_om", [128, 2], F32)
                dsm = two("p1_dsm", [128, 256], F32)
                pT = [fw.psum(st, "p1_pT%d" % i, [128, 4, 128], BF16) for i in range(2)]
                ps_k = fw.psum(st, "p1_psk", [128, 512], F32)
                ps_v = [fw.psum(st, "p1_psv%d" % i, [128, 512], F32) for i in range(2)]
                ps_l = fw.psum(st, "p1_psl", [64, 128], F32)
                ps_g = fw.psum(st, "p1_psg", [128, 512], F32)
                ps_d = [fw.psum(st, "p1_psd0", [128, 512], F32)] * 2
                pf = fw.sbuf(st, "p1_pf", [128, npre, 3], F32)
                Scur = [fw.sbuf(st, "p1_Scur%d" % h, [128, 256], F32) for h in range(4)]
                for h in range(4):
                    fw.v(lambda: V.memset(Scur[h][:], 0.0), writes=[Scur[h]])
                fw.dma(sp, pf[:], pre_f[:, :, :], reads=[DB["pre_f"]], writes=[pf])
                fw.dma(sp, ln1[:], ln1_bc[:, :], reads=[DB["ln1_bc"]], writes=[ln1])
                fw.dma(sp, psc[:], pre_sc[:, :], reads=[DB["pre_sc"]], writes=[psc])
                fw.dma(sp, pbi[:], pre_bi[:, :], reads=[DB["pre_bi"]], writes=[pbi])
                fw.dma(sp, pm[:], pre_m[:, :, :], reads=[DB["pre_m"]], writes=[pm])
                fw.dma(pool, wk[:], w_in[:, C_GK:C_GK + 512].rearrange("(kc p) n -> p kc n", p=128), reads=[DB["w_in"]], writes=[wk])
                fw.dma(pool, wv[:], w_in[:, C_GV:C_GV + 1024].rearrange("(kc p) n -> p kc n", p=128), reads=[DB["w_in"]], writes=[wv])
                fw.dma(pool, wl[:], w_lr.ap().rearrange("(kc p) n -> p kc n", p=128), reads=[DB["w_lr"]], writes=[wl])

                def front(s):
                    q = s % 2
                    xTs = xT[q]
                    xt_, xb_, ss_ = xt[q], xb[q], ss[q]
                    fw.dma(sp, xt_[:], xpre[s * 128:(s + 1) * 128, :], reads=[DB["xpre"]], writes=[xt_])
                    rms_rstd(xt_[:], xt_, junk, ss_, D)
                    fw.v(lambda: V.scalar_tensor_tensor(out=xb_[:], in0=xt_[:], scalar=ss_[:, 0:1], in1=ln1[:], op0=ALU.mult, op1=ALU.mult),
                         reads=[xt_, ss_, ln1], writes=[xb_])
                    yield
                    for g in range(4):
                        p = pT[g % 2]
                        for j in range(4):
                            kc = 4 * g + j
                            fw.tr(p, p[:, j, :], xb_, xb_[:, kc * 128:(kc + 1) * 128], idb)
                        if g % 2 == 0:
                            fw.v(lambda: V.tensor_copy(out=xTs[:, 4 * g:4 * g + 4, :], in_=p[:]), reads=[p], writes=[xTs])
                        else:
                            fw.a(lambda: A.copy(out=xTs[:, 4 * g:4 * g + 4, :], in_=p[:]), reads=[p], writes=[xTs])
                        yield
                    for kc in range(16):
                        fw.mm(ps_k, ps_k[:], xTs, xTs[:, kc, :], wk, wk[:, kc, :], start=(kc == 0), stop=(kc == 15))
                    fw.a(lambda: A.copy(out=gk[q][:], in_=ps_k[:]), reads=[ps_k], writes=[gk[q]])
                    yield
                    for half in range(2):
                        for kc in range(16):
                            fw.mm(ps_v[half], ps_v[half][:], xTs, xTs[:, kc, :], wv, wv[:, kc, half * 512:(half + 1) * 512],
                                  start=(kc == 0), stop=(kc == 15))
                        fw.v(lambda: V.tensor_copy(out=gv[q][:, half * 512:(half + 1) * 512], in_=ps_v[half][:]), reads=[ps_v[half]], writes=[gv[q]])
                        yield
                    for kc in range(16):
                        fw.mm(ps_l, ps_l[:], wl, wl[:, kc, :], xTs, xTs[:, kc, :], start=(kc == 0), stop=(kc == 15))
                    fw.v(lambda: V.tensor_scalar(out=lrT[q][:], in0=ps_l[:], scalar1=psc[:, s:s + 1], scalar2=pbi[:, s:s + 1],
                                                 op0=ALU.mult, op1=ALU.add), reads=[ps_l, psc, pbi], writes=[lrT[q]])
                    fw.v(lambda: V.tensor_scalar(out=ttmp[q][:], in0=trif[:, 3, :], scalar1=pm[:, s, 1:2], scalar2=None, op0=ALU.mult),
                         reads=[trif, pm], writes=[ttmp[q]])
                    fw.v(lambda: V.scalar_tensor_tensor(out=tsel[q][:], in0=trif[:, 2, :], scalar=pm[:, s, 0:1], in1=ttmp[q][:],
                                                        op0=ALU.mult, op1=ALU.add), reads=[trif, pm, ttmp[q]], writes=[tsel[q]])
                    yield

                def back(s):
                    q = s % 2
                    fw.mm(ps_g, ps_g[:], lrT[q], lrT[q][:], upp, upp[:])
                    yield
                    fw.a(lambda: A.activation(out=spe[q][:], in_=ps_g[:], func=AF.Exp, scale=-1.0), reads=[ps_g], writes=[spe[q]])
                    fw.a(lambda: A.activation(out=spf[q][:], in_=spe[q][:], func=AF.Ln, bias=1.0), reads=[spe[q]], writes=[spf[q]])
                    yield
                    fw.mm(ps_g, ps_g[:], tsel[q], tsel[q][:], spf[q], spf[q][:])
                    yield
                    fw.a(lambda: A.activation(out=e3[q][:], in_=ps_g[:], func=AF.Exp, scale=-1.0 / 16), reads=[ps_g], writes=[e3[q]])
                    fw.v(lambda: V.tensor_tensor(out=kend[q][:], in0=gk[q][:], in1=e3[q][:], op=ALU.mult), reads=[gk[q], e3[q]], writes=[kend[q]])
                    yield
                    for h in range(4):
                        fw.op(fw.pe, lambda: nc.tensor.matmul(ps_g[:, h:h + 1], lhsT=spf[q][:, h * 128:(h + 1) * 128], rhs=onesb[:, 0:1], start=True, stop=True),
                              reads=[spf[q], onesb], writes=[ps_g], inc=(h == 3))
                    yield
                    fw.a(lambda: A.activation(out=dec[q][:], in_=ps_g[:, 0:4], func=AF.Exp, scale=-1.0 / 16), reads=[ps_g], writes=[dec[q]])
                    fw.v(lambda: V.tensor_scalar(out=dd[q][:, 0, :], in0=dec[q][:], scalar1=pf[:, s, 0:1], scalar2=None, op0=ALU.mult),
                         reads=[dec[q], pf], writes=[dd[q]])
                    yield
                    for h in range(4):
                        pd = ps_d[0]
                        fw.mm(pd, pd[:, 0:256], kend[q], kend[q][:, h * 128:(h + 1) * 128], gv[q], gv[q][:, h * 256:(h + 1) * 256])
                        fw.v(lambda: V.scalar_tensor_tensor(out=Scur[h][:], in0=Scur[h][:], scalar=dd[q][:, 0, h:h + 1], in1=pd[:, 0:256],
                                                            op0=ALU.mult, op1=ALU.add), reads=[Scur[h], dd[q], pd], writes=[Scur[h]])
                        fw.v(lambda: V.scalar_tensor_tensor(out=Sst[0][h][:], in0=Scur[h][:], scalar=pf[:, s, 1:2], in1=Sst[0][h][:],
                                                            op0=ALU.mult, op1=ALU.add), reads=[Scur[h], pf, Sst[0][h]], writes=[Sst[0][h]])
                        if h % 2 == 1:
                            yield

                def zip_emit(*gens):
                    gens = [g for g in gens if g is not None]
                    while gens:
                        for g in list(gens):
                            try:
                                next(g)
                            except StopIteration:
                                gens.remove(g)

                zip_emit(front(0))
                for s in range(npre):
                    zip_emit(back(s), front(s + 1) if s + 1 < npre else None)
                    pump_conv(1)
                for h in range(4):
                    fw.v(lambda: V.tensor_scalar(out=Sst[1][h][:], in0=Scur[h][:], scalar1=pf[:, 0, 2:3], scalar2=None, op0=ALU.mult),
                         reads=[Scur[h], pf], writes=[Sst[1][h]])
            fw.barrier()

        with ExitStack() as st2:
            xnT = fw.sbuf(st2, "xnT", [128, 16, NTH * 128], BF16)
            for st in _scope("xn" in phases):
                xt = fw.sbuf(st, "p2_xt", [128, D], F32); xb = fw.sbuf(st, "p2_xb", [128, D], BF16)
                junk = fw.sbuf(st, "p2_junk", [128, D], F32); ss = fw.sbuf(st, "p2_ss", [128, 4], F32)
                ln1 = fw.sbuf(st, "p2_ln1", [128, D], F32)
                pT = [fw.psum(st, "p2_pT%d" % i, [128, 4, 128], BF16) for i in range(2)]
                fw.dma(sp, ln1[:], ln1_bc[:, :], reads=[DB["ln1_bc"]], writes=[ln1])
                for t in range(NTH):
                    norm1_tile((xt, xb, junk, ss, ln1, pT), xh[t * 128:(t + 1) * 128, :], DB["xh"], xnT,
                               lambda kc: xnT[:, kc:kc + 4, t * 128:(t + 1) * 128])
                    pump_conv(1)
            fw.barrier()

            for st in _scope("attn" in phases):
                aqT = fw.sbuf(st, "aqT", [128, NT, 1024], BF16)
                akT = fw.sbuf(st, "akT", [128, 2, NTH * 128], BF16)
                av = fw.sbuf(st, "av", [128, NTH, 2, 129], BF16)
                wq = [fw.sbuf(st, "wq%d" % i, [128, 16, 512], BF16) for i in range(2)]
                qn = fw.sbuf(st, "qn", [128, 128], F32); kn = fw.sbuf(st, "kn", [128, 128], F32)
                cs = fw.sbuf(st, "cs", [128, 128], F32); sn = fw.sbuf(st, "sn", [128, 128], F32)
                qf = fw.sbuf(st, "qf", [128, 512], F32); qb = fw.sbuf(st, "qb", [128, 512], BF16)
                junk = fw.sbuf(st, "a_junk", [128, 1024], F32); ss = fw.sbuf(st, "a_ss", [128, 8], F32)
                r1 = fw.sbuf(st, "r1", [128, 4, 16], F32); r2 = fw.sbuf(st, "r2", [128, 4, 16], F32)
                r3 = fw.sbuf(st, "r3", [128, 4, 16], F32); r4 = fw.sbuf(st, "r4", [128, 4, 16], F32)
                mP = fw.sbuf(st, "mP", [128, 512], BF16); mN = fw.sbuf(st, "mN", [128, 512], BF16)
                mP0 = fw.sbuf(st, "mP0", [128, 512], BF16); mNL = fw.sbuf(st, "mNL", [128, 512], BF16)
                esk = fw.sbuf(st, "esk", [128, 8], F32)
                anw = fw.sbuf(st, "anw", [128, 1024], F32)
                pTt = [fw.sbuf(st, "pTt%d" % i, [128, 512], BF16) for i in range(3)]
                ao = fw.sbuf(st, "ao", [128, 1024], F32); aob = fw.sbuf(st, "aob", [128, 1024], BF16)
                den = fw.sbuf(st, "den", [128, 4], F32)
                pst1 = ExitStack()
                ps_q = [fw.psum(pst1, "ps_q%d" % i, [128, 512], F32) for i in range(2)]
                ps_t = [fw.psum(pst1, "ps_t%d" % i, [128, 4, 128], BF16) for i in range(2)]
                for (dst, src, nm) in ((qn, qn_bc, "qn_bc"), (kn, kn_bc, "kn_bc"), (esk, sink_bc, "sink_bc"), (anw, an_bc, "an_bc")):
                    fw.dma(sp, dst[:], src[:, :], reads=[DB[nm]], writes=[dst])
                for (dst, src, nm) in ((mP, maskP, "maskP"), (mN, maskN, "maskN"), (mP0, maskP0, "maskP0"), (mNL, maskNL, "maskNL")):
                    fw.dma(sp, dst[:], src[:, :], reads=[DB[nm]], writes=[dst])
                fw.a(lambda: A.activation(out=esk[:], in_=esk[:], func=AF.Exp), reads=[esk], writes=[esk])
                fw.v(lambda: V.memset(av[:], 1.0), writes=[av])

                def qk_post(ps, nh, nw, dstT, dst_fn, t):
                    for h in range(nh):
                        fw.a(lambda: A.activation(out=junk[:, 0:128], in_=ps[:, h * 128:(h + 1) * 128], func=AF.Square, accum_out=ss[:, h:h + 1]),
                             reads=[ps], writes=[junk, ss])
                    fw.a(lambda: A.activation(out=ss[:, 0:nh], in_=ss[:, 0:nh], func=AF.Sqrt, scale=1.0 / 128, bias=EPS), reads=[ss], writes=[ss])
                    fw.v(lambda: V.reciprocal(out=ss[:, 0:nh], in_=ss[:, 0:nh]), reads=[ss], writes=[ss])
                    for h in range(nh):
                        fw.v(lambda: V.scalar_tensor_tensor(out=qf[:, h * 128:(h + 1) * 128], in0=ps[:, h * 128:(h + 1) * 128], scalar=ss[:, h:h + 1],
                                                            in1=nw[:], op0=ALU.mult, op1=ALU.mult), reads=[ps, ss, nw], writes=[qf])
                    q3 = qf[:, 0:nh * 128].rearrange("p (h d) -> p h d", d=128)
                    c3 = cs[:, 0:nh * 16].rearrange("p (h d) -> p h d", d=16)
                    s3 = sn[:, 0:nh * 16].rearrange("p (h d) -> p h d", d=16)
                    x1, x2 = q3[:, :, 0:16], q3[:, :, 16:32]
                    fw.v(lambda: V.tensor_tensor(out=r1[:, 0:nh, :], in0=x1, in1=c3, op=ALU.mult), reads=[qf, cs], writes=[r1])
                    fw.v(lambda: V.tensor_tensor(out=r2[:, 0:nh, :], in0=x2, in1=s3, op=ALU.mult), reads=[qf, sn], writes=[r2])
                    fw.v(lambda: V.tensor_tensor(out=r3[:, 0:nh, :], in0=x2, in1=c3, op=ALU.mult), reads=[qf, cs], writes=[r3])
                    fw.v(lambda: V.tensor_tensor(out=r4[:, 0:nh, :], in0=x1, in1=s3, op=ALU.mult), reads=[qf, sn], writes=[r4])
                    fw.v(lambda: V.tensor_tensor(out=x1, in0=r1[:, 0:nh, :], in1=r2[:, 0:nh, :], op=ALU.subtract), reads=[r1, r2], writes=[qf])
                    fw.v(lambda: V.tensor_tensor(out=x2, in0=r3[:, 0:nh, :], in1=r4[:, 0:nh, :], op=ALU.add), reads=[r3, r4], writes=[qf])
                    fw.a(lambda: A.copy(out=qb[:, 0:nh * 128], in_=qf[:, 0:nh * 128]), reads=[qf], writes=[qb])
                    p = ps_t[t % 2]
                    for h in range(nh):
                        fw.tr(p, p[:, h, :], qb, qb[:, h * 128:(h + 1) * 128], idb)
                    fw.v(lambda: V.tensor_copy(out=dst_fn(None), in_=p[:, 0:nh, :]), reads=[p], writes=[dstT])

                groups = [(C_AQ, "q", 0), (C_AQ + 512, "q", 4), (C_AK, "kv", 0)]
                for gi, (c0, kind, h0) in enumerate(groups):
                    w = wq[gi % 2]
                    fw.dma(pool, w[:], w_in[:, c0:c0 + 512].rearrange("(kc p) n -> p kc n", p=128), reads=[DB["w_in"]], writes=[w])
                    tiles = range(1, NT + 1) if kind == "q" else range(NTH)
                    for t in tiles:
                        if t % 2 == 0:
                            pump_conv(1)
                        ps = ps_q[t % 2]
                        for kc in range(16):
                            fw.mm(ps, ps[:], xnT, xnT[:, kc, t * 128:(t + 1) * 128], w, w[:, kc, :], start=(kc == 0), stop=(kc == 15))
                        fw.dma(sp, cs[:], cosq[t * 128:(t + 1) * 128, :], reads=[DB["cosq"]], writes=[cs])
                        fw.dma(sp, sn[:], sinq[t * 128:(t + 1) * 128, :], reads=[DB["sinq"]], writes=[sn])
                        if kind == "q":
                            qk_post(ps, 4, qn, aqT, lambda h: aqT[:, t - 1, h0 * 128:(h0 + 4) * 128].rearrange("p (h d) -> p h d", h=4), t)
                        else:
                            qk_post(ps, 2, kn, akT, lambda h: akT[:, 0:2, t * 128:(t + 1) * 128], t)
                            for hk in range(2):
                                fw.a(lambda: A.copy(out=av[:, t, hk, 0:128], in_=ps[:, 256 + hk * 128:256 + (hk + 1) * 128]), reads=[ps], writes=[av])

                fw.barrier()
                pst1.close()
                ps_s = [fw.psum(st, "ps_s%d" % i, [128, 512], F32) for i in range(3)]
                ps_o = [fw.psum(st, "ps_o%d" % i, [128, 2, 129], F32) for i in range(2)]
                for i in range(NT):
                    pump_conv(1)
                    for hk in range(2):
                        for kb in range(3):
                            kt = i + kb
                            fw.mm(ps_s[kb], ps_s[kb][:], akT, akT[:, hk, kt * 128:(kt + 1) * 128],
                                  aqT, aqT[:, i, hk * 512:(hk + 1) * 512])
                            fw.a(lambda: A.activation(out=pTt[kb][:], in_=ps_s[kb][:], func=AF.Exp, scale=QS), reads=[ps_s[kb]], writes=[pTt[kb]])
                        mk = mP0 if i == 0 else mP
                        fw.v(lambda: V.tensor_tensor(out=pTt[0][:], in0=pTt[0][:], in1=mk[:], op=ALU.mult), reads=[pTt[0], mk], writes=[pTt[0]])
                        mk2 = mNL if i == NT - 1 else mN
                        fw.v(lambda: V.tensor_tensor(out=pTt[2][:], in0=pTt[2][:], in1=mk2[:], op=ALU.mult), reads=[pTt[2], mk2], writes=[pTt[2]])
                        for g in range(4):
                            po = ps_o[g // 2]
                            for kb in range(3):
                                kt = i + kb
                                fw.mm(po, po[:, g % 2, :], pTt[kb], pTt[kb][:, g * 128:(g + 1) * 128], av, av[:, kt, hk, :],
                                      start=(kb == 0), stop=(kb == 2))
                        for g in range(4):
                            po = ps_o[g // 2]
                            hq = 4 * hk + g
                            fw.v(lambda: V.tensor_scalar(out=den[:, g:g + 1], in0=po[:, g % 2, 128:129], scalar1=esk[:, hq:hq + 1], scalar2=None, op0=ALU.add),
                                 reads=[po, esk], writes=[den])
                        fw.v(lambda: V.reciprocal(out=den[:], in_=den[:]), reads=[den], writes=[den])
                        for g in range(4):
                            po = ps_o[g // 2]
                            hq = 4 * hk + g
                            fw.v(lambda: V.tensor_scalar(out=ao[:, hq * 128:(hq + 1) * 128], in0=po[:, g % 2, 0:128], scalar1=den[:, g:g + 1], scalar2=None, op0=ALU.mult),
                                 reads=[po, den], writes=[ao])
                    rms_rstd(ao[:], ao, junk, ss, 1024)
                    fw.v(lambda: V.scalar_tensor_tensor(out=aob[:], in0=ao[:], scalar=ss[:, 0:1], in1=anw[:], op0=ALU.mult, op1=ALU.mult),
                         reads=[ao, ss, anw], writes=[aob])
                    fw.dma(sp, mix_d[i * 128:(i + 1) * 128, 0:1024], aob[:], reads=[aob], writes=[mixB])
            fw.barrier()

            for st in _scope("gla" in phases):
                wa = [fw.sbuf(st, "g_w%d" % i, [128, 16, 256], BF16) for i in range(2)]
                wl = fw.sbuf(st, "g_wl", [128, 16, 64], BF16)
                mbi = fw.sbuf(st, "g_mbi", [64, 1], F32)
                lrT = fw.sbuf(st, "g_lrT", [64, TPC], BF16)
                gqT = fw.sbuf(st, "g_qT", [128, TPC], BF16); gkT = fw.sbuf(st, "g_kT", [128, TPC], BF16)
                gkt = fw.sbuf(st, "g_kt", [128, NT, 128], F32)
                gvt = fw.sbuf(st, "g_vt", [128, NT, 256], BF16)
                srt = fw.sbuf(st, "g_sr", [128, NT, 256], BF16)
                oacc2 = [fw.sbuf(st, "g_oacc%d" % i, [128, NT, 256], F32) for i in range(2)]
                gnw = fw.sbuf(st, "g_nw", [128, 256], F32)
                def two(name, shape, dt):
                    return [fw.sbuf(st, "%s_%d" % (name, i), shape, dt) for i in range(2)]
                spf2 = two("g_sp", [128, 128], BF16); spe2 = two("g_spe", [128, 128], F32)
                e1s = two("g_e1", [128, 128], F32); e2s = two("g_e2", [128, 128], F32); e3s = two("g_e3", [128, 128], F32)
                qds = two("g_qd", [128, 128], BF16); kis = two("g_ki", [128, 128], BF16); kes = two("g_ke", [128, 128], BF16)
                ams = two("g_am", [128, 128], BF16)
                decs = two("g_dec", [128, 1], F32)
                Sbs = two("g_Sb", [128, 256], BF16)
                junk = fw.sbuf(st, "g_junk", [128, 256], F32); ss = fw.sbuf(st, "g_ss", [128, 4], F32)
                ob = fw.sbuf(st, "g_ob", [128, 256], BF16); of = fw.sbuf(st, "g_of", [128, 256], F32)
                PS = [fw.psum(st, "g_ps%d" % i, [128, 512], F32) for i in range(8)]
                ps_p = [PS[0], PS[4]]
                fw.dma(sp, gnw[:], gn_bc[:, :], reads=[DB["gn_bc"]], writes=[gnw])
                fw.dma(sp, mbi[:], main_bi[:, :], reads=[DB["main_bi"]], writes=[mbi])
                fw.dma(pool, wl[:], w_lr.ap().rearrange("(kc p) n -> p kc n", p=128), reads=[DB["w_lr"]], writes=[wl])
                XO = 128
                for nt in range(TPC // 512):
                    pl = ps_p[nt % 2]
                    for kc in range(16):
                        fw.mm(pl, pl[0:64, :], wl, wl[:, kc, :], xnT, xnT[:, kc, XO + nt * 512:XO + (nt + 1) * 512], start=(kc == 0), stop=(kc == 15))
                    fw.v(lambda: V.tensor_scalar(out=lrT[:, nt * 512:(nt + 1) * 512], in0=pl[0:64, :], scalar1=mbi[:, 0:1], scalar2=None, op0=ALU.add),
                         reads=[pl, mbi], writes=[lrT])
                wi = 0
                for h in range(4):
                    w = wa[wi % 2]; wi += 1
                    fw.dma(pool, w[:, :, 0:128], w_in[:, C_GQ + h * 128:C_GQ + (h + 1) * 128].rearrange("(kc p) n -> p kc n", p=128), reads=[DB["w_in"]], writes=[w])
                    fw.dma(pool, w[:, :, 128:256], w_in[:, C_GK + h * 128:C_GK + (h + 1) * 128].rearrange("(kc p) n -> p kc n", p=128), reads=[DB["w_in"]], writes=[w])
                    for nt in range(TPC // 512):
                        for (j, dst) in ((0, gqT), (1, gkT)):
                            pl = ps_p[j]
                            for kc in range(16):
                                fw.mm(pl, pl[:], w, w[:, kc, j * 128:(j + 1) * 128], xnT, xnT[:, kc, XO + nt * 512:XO + (nt + 1) * 512], start=(kc == 0), stop=(kc == 15))
                            fw.a(lambda: A.copy(out=dst[:, nt * 512:(nt + 1) * 512], in_=pl[:]), reads=[pl], writes=[dst])
                    for t in range(NT):
                        pl = ps_p[t % 2]
                        for kc in range(16):
                            fw.mm(pl, pl[:, 0:128], xnT, xnT[:, kc, XO + t * 128:XO + (t + 1) * 128], w, w[:, kc, 128:256], start=(kc == 0), stop=(kc == 15))
                        fw.v(lambda: V.tensor_copy(out=gkt[:, t, :], in_=pl[:, 0:128]), reads=[pl], writes=[gkt])
                    for (c0, dst, isr) in ((C_GV + h * 256, gvt, False), (C_GR + h * 256, srt, True)):
                        w2 = wa[wi % 2]; wi += 1
                        fw.dma(pool, w2[:], w_in[:, c0:c0 + 256].rearrange("(kc p) n -> p kc n", p=128), reads=[DB["w_in"]], writes=[w2])
                        for t in range(NT):
                            pl = ps_p[t % 2]
                            for kc in range(16):
                                fw.mm(pl, pl[:, 0:256], xnT, xnT[:, kc, XO + t * 128:XO + (t + 1) * 128], w2, w2[:, kc, :], start=(kc == 0), stop=(kc == 15))
                            if isr:
                                fw.a(lambda: A.activation(out=dst[:, t, :], in_=pl[:, 0:256], func=AF.Silu), reads=[pl], writes=[dst])
                            else:
                                fw.v(lambda: V.tensor_copy(out=dst[:, t, :], in_=pl[:, 0:256]), reads=[pl], writes=[dst])
                    def chain(d):
                        S = Sst[d][h]
                        order = range(NT) if d == 0 else range(NT - 1, -1, -1)
                        last = 127 if d == 0 else 0
                        pA, pR, pO, pD = PS[4 * d], PS[4 * d + 1], PS[4 * d + 2], PS[4 * d + 3]
                        spe_, spf_, e1_, e2_, e3_ = spe2[d], spf2[d], e1s[d], e2s[d], e3s[d]
                        qd_, ki_, ke_, am_, dec_, Sb_, oa_ = qds[d], kis[d], kes[d], ams[d], decs[d], Sbs[d], oacc2[d]
                        for n in order:
                            if d == 0 and n % 4 == 0:
                                pump_conv(1)
                            tk = slice(n * 128, (n + 1) * 128)
                            fw.a(lambda: A.copy(out=Sb_[:], in_=S[:]), reads=[S], writes=[Sb_])
                            fw.mm(pA, pA[:, 0:128], lrT, lrT[32 * d:32 * d + 32, tk], upp, upp[32 * d:32 * d + 32, h * 128:(h + 1) * 128])
                            yield
                            fw.a(lambda: A.activation(out=spe_[:], in_=pA[:, 0:128], func=AF.Exp, scale=-1.0), reads=[pA], writes=[spe_])
                            fw.a(lambda: A.activation(out=spf_[:], in_=spe_[:], func=AF.Ln, bias=1.0), reads=[spe_], writes=[spf_])
                            yield
                            fw.mm(pA, pA[:, 0:128], spf_, spf_[:], trib, trib[:, d, :])
                            fw.mm(pR, pR[:, 0:128], trib, trib[:, 2 + d, :], spf_, spf_[:])
                            yield
                            fw.a(lambda: A.activation(out=e1_[:], in_=pA[:, 0:128], func=AF.Exp, scale=-1.0 / 16), reads=[pA], writes=[e1_])
                            fw.a(lambda: A.activation(out=e2_[:], in_=pA[:, 0:128], func=AF.Exp, scale=1.0 / 16), reads=[pA], writes=[e2_])
                            fw.a(lambda: A.activation(out=e3_[:], in_=pR[:, 0:128], func=AF.Exp, scale=-1.0 / 16), reads=[pR], writes=[e3_])
                            fw.a(lambda: A.copy(out=dec_[:], in_=e1_[:, last:last + 1]), reads=[e1_], writes=[dec_])
                            yield
                            fw.v(lambda: V.scalar_tensor_tensor(out=qd_[:], in0=gqT[:, tk], scalar=QS, in1=e1_[:], op0=ALU.mult, op1=ALU.mult),
                                 reads=[gqT, e1_], writes=[qd_])
                            fw.v(lambda: V.tensor_tensor(out=ki_[:], in0=gkT[:, tk], in1=e2_[:], op=ALU.mult), reads=[gkT, e2_], writes=[ki_])
                            fw.v(lambda: V.tensor_tensor(out=ke_[:], in0=gkt[:, n, :], in1=e3_[:], op=ALU.mult), reads=[gkt, e3_], writes=[ke_])
                            yield
                            fw.mm(pA, pA[:, 0:128], ki_, ki_[:], qd_, qd_[:])
                            yield
                            fw.v(lambda: V.tensor_tensor(out=am_[:], in0=pA[:, 0:128], in1=trif[:, d, :], op=ALU.mult), reads=[pA, trif], writes=[am_])
                            yield
                            fw.mm(pO, pO[:, 0:256], am_, am_[:], gvt, gvt[:, n, :], start=True, stop=False)
                            fw.mm(pO, pO[:, 0:256], qd_, qd_[:], Sb_, Sb_[:], start=False, stop=True)
                            fw.mm(pD, pD[:, 0:256], ke_, ke_[:], gvt, gvt[:, n, :])
                            yield
                            fw.a(lambda: A.copy(out=oa_[:, n, :], in_=pO[:, 0:256]), reads=[pO], writes=[oa_])
                            fw.v(lambda: V.scalar_tensor_tensor(out=S[:], in0=S[:], scalar=dec_[:, 0:1], in1=pD[:, 0:256], op0=ALU.mult, op1=ALU.add),
                                 reads=[S, dec_, pD], writes=[S])
                            yield

                    gens = [chain(0), chain(1)]
                    while gens:
                        for g_ in list(gens):
                            try:
                                next(g_)
                            except StopIteration:
                                gens.remove(g_)
                    for n in range(NT):
                        fw.v(lambda: V.tensor_tensor(out=of[:], in0=oacc2[0][:, n, :], in1=oacc2[1][:, n, :], op=ALU.add), reads=[oacc2[0], oacc2[1]], writes=[of])
                        rms_rstd(of[:], of, junk, ss, 256)
                        fw.v(lambda: V.scalar_tensor_tensor(out=of[:], in0=of[:], scalar=ss[:, 0:1], in1=gnw[:], op0=ALU.mult, op1=ALU.mult),
                             reads=[of, ss, gnw], writes=[of])
                        fw.v(lambda: V.tensor_tensor(out=ob[:], in0=of[:], in1=srt[:, n, :], op=ALU.mult), reads=[of, srt], writes=[ob])
                        fw.dma(sp, mix_d[n * 128:(n + 1) * 128, 1024 + h * 256:1024 + (h + 1) * 256], ob[:], reads=[ob], writes=[mixB])
            fw.barrier()

        for st in _scope("out" in phases):
            wo = fw.sbuf(st, "wo", [128, 16, D], BF16)
            wr = fw.sbuf(st, "wr", [128, 16, 36], F32)
            brt = fw.sbuf(st, "brt", [128, 36], F32)
            ln2 = fw.sbuf(st, "ln2", [128, D], F32)
            def two(name, shape, dt):
                return [fw.sbuf(st, "%s_%d" % (name, i), shape, dt) for i in range(2)]
            junk = fw.sbuf(st, "o_junk", [128, D], F32)
            PAR = dict(mt=two("mt", [128, D], BF16), mT=two("mT", [128, 16, 128], BF16), xt=two("o_xt", [128, D], F32),
                       ht=two("o_ht", [128, D], F32), ss=two("o_ss", [128, 4], F32), xn2=two("o_xn2", [128, D], F32),
                       xTf=two("o_xTf", [128, 16, 128], F32), xTb=two("o_xTb", [128, 16, 128], BF16), lg=two("lg", [128, 36], F32),
                       gm=two("gm", [128, 8], F32), ohg=two("ohg", [128, 4], F32), ge=two("ge", [128, 4], F32),
                       ig=two("ig", [128, 8], F32), tmp8=two("tmp8", [128, 8], F32), oh1=two("oh1", [128, 8], F32),
                       oh2=two("oh2", [128, 8], F32), w8=two("w8", [128, 8], F32))
            ps_t = [fw.psum(st, "o_pst%d" % i, [128, 4, 128], BF16) for i in range(2)]
            ps_h = [fw.psum(st, "o_psh%d" % i, [128, 512], F32) for i in range(4)]
            ps_f = [fw.psum(st, "o_psf%d" % i, [128, 4, 128], F32) for i in range(1)]
            ps_l = fw.psum(st, "o_psl", [128, 36], F32)
            for half in range(2):
                fw.dma(pool, wo[:, :, half * 1024:(half + 1) * 1024], w_out[:, half * 1024:(half + 1) * 1024].rearrange("(kc p) n -> p kc n", p=128),
                       reads=[DB["w_out"]], writes=[wo])
            fw.dma(sp, wr[:], w_rt.ap().rearrange("(kc p) n -> p kc n", p=128), reads=[DB["w_rt"]], writes=[wr])
            fw.dma(sp, brt[:], b_rt[:, :], reads=[DB["b_rt"]], writes=[brt])
            fw.dma(sp, ln2[:], ln2_bc[:, :], reads=[DB["ln2_bc"]], writes=[ln2])
            def tile_gen(t):
                mt, mT, xt, ht, ss, xn2, xTf, xTb, lg, gm, ohg, ge, ig, tmp8, oh1, oh2, w8 = [PAR[k][t % 2] for k in (
                    "mt", "mT", "xt", "ht", "ss", "xn2", "xTf", "xTb", "lg", "gm", "ohg", "ge", "ig", "tmp8", "oh1", "oh2", "w8")]
                rows = slice(t * 128, (t + 1) * 128)
                pump_conv(1)
                fw.dma(sp, mt[:], mix_d[rows, :], reads=[mixB], writes=[mt])
                fw.dma(sp, xt[:], xh[128 + t * 128:128 + (t + 1) * 128, :], reads=[DB["xh"]], writes=[xt])
                for g in range(4):
                    p = ps_t[g % 2]
                    for j in range(4):
                        kc = 4 * g + j
                        fw.tr(p, p[:, j, :], mt, mt[:, kc * 128:(kc + 1) * 128], idb)
                    if g % 2 == 0:
                        fw.v(lambda: V.tensor_copy(out=mT[:, 4 * g:4 * g + 4, :], in_=p[:]), reads=[p], writes=[mT])
                    else:
                        fw.a(lambda: A.copy(out=mT[:, 4 * g:4 * g + 4, :], in_=p[:]), reads=[p], writes=[mT])
                    if g % 2 == 1:
                        yield
                for dc in range(4):
                    for kc in range(16):
                        fw.mm(ps_h[dc], ps_h[dc][:], mT, mT[:, kc, :], wo, wo[:, kc, dc * 512:(dc + 1) * 512], start=(kc == 0), stop=(kc == 15))
                    fw.v(lambda: V.tensor_tensor(out=ht[:, dc * 512:(dc + 1) * 512], in0=ps_h[dc][:], in1=xt[:, dc * 512:(dc + 1) * 512], op=ALU.add),
                         reads=[ps_h[dc], xt], writes=[ht])
                    yield
                fw.dma(sp, h_d[rows, :], ht[:], reads=[ht], writes=[hB])
                rms_rstd(ht[:], ht, junk, ss, D)
                fw.v(lambda: V.scalar_tensor_tensor(out=xn2[:], in0=ht[:], scalar=ss[:, 0:1], in1=ln2[:], op0=ALU.mult, op1=ALU.mult),
                     reads=[ht, ss, ln2], writes=[xn2])
                yield
                for g in range(4):
                    p = ps_f[0]
                    for j in range(4):
                        kc = 4 * g + j
                        fw.mm(p, p[:, j, :], xn2, xn2[:, kc * 128:(kc + 1) * 128], idf, idf[:])
                    fw.a(lambda: A.copy(out=xTf[:, 4 * g:4 * g + 4, :], in_=p[:]), reads=[p], writes=[xTf])
                    if not sparse:
                        fw.v(lambda: V.tensor_copy(out=xTb[:, 4 * g:4 * g + 4, :], in_=p[:]), reads=[p], writes=[xTb])
                    yield
                if sparse:
                    fw.a(lambda: A.copy(out=mt[:], in_=xn2[:]), reads=[xn2], writes=[mt])
                    fw.dma(sp, xn2_d[rows, :], mt[:], reads=[mt], writes=[xn2B])
                else:
                    fw.dma(sp, xs_d[t, :, :, :], xTb[:], reads=[xTb], writes=[xsB])
                for kc in range(16):
                    fw.mm(ps_l, ps_l[:], xTf, xTf[:, kc, :], wr, wr[:, kc, :], start=(kc == 0), stop=(kc == 15))
                fw.v(lambda: V.tensor_tensor(out=lg[:], in0=ps_l[:], in1=brt[:], op=ALU.add), reads=[ps_l, brt], writes=[lg])
                yield
                fw.v(lambda: V.tensor_reduce(out=gm[:, 0:1], in_=lg[:, 0:4], axis=AX.X, op=ALU.max), reads=[lg], writes=[gm])
                fw.v(lambda: V.tensor_scalar(out=ohg[:], in0=lg[:, 0:4], scalar1=gm[:, 0:1], scalar2=None, op0=ALU.is_equal), reads=[lg, gm], writes=[ohg])
                fw.v(lambda: V.tensor_scalar(out=ge[:], in0=lg[:, 0:4], scalar1=gm[:, 0:1], scalar2=None, op0=ALU.subtract), reads=[lg, gm], writes=[ge])
                fw.a(lambda: A.activation(out=ge[:], in_=ge[:], func=AF.Exp, accum_out=gm[:, 1:2]), reads=[ge], writes=[ge, gm])
                fw.v(lambda: V.reciprocal(out=gm[:, 2:3], in_=gm[:, 1:2]), reads=[gm], writes=[gm])
                yield
                fw.v(lambda: V.tensor_scalar(out=ig[:], in0=lg[:, 4:12], scalar1=ohg[:, 0:1], scalar2=None, op0=ALU.mult), reads=[lg, ohg], writes=[ig])
                for g in range(1, 4):
                    fw.v(lambda: V.scalar_tensor_tensor(out=ig[:], in0=lg[:, 4 + 8 * g:12 + 8 * g], scalar=ohg[:, g:g + 1], in1=ig[:], op0=ALU.mult, op1=ALU.add),
                         reads=[lg, ohg, ig], writes=[ig])
                yield
                fw.v(lambda: V.tensor_reduce(out=gm[:, 3:4], in_=ig[:], axis=AX.X, op=ALU.max), reads=[ig], writes=[gm])
                fw.v(lambda: V.tensor_scalar(out=oh1[:], in0=ig[:], scalar1=gm[:, 3:4], scalar2=None, op0=ALU.is_equal), reads=[ig, gm], writes=[oh1])
                fw.v(lambda: V.scalar_tensor_tensor(out=tmp8[:], in0=oh1[:], scalar=-1e30, in1=ig[:], op0=ALU.mult, op1=ALU.add), reads=[oh1, ig], writes=[tmp8])
                fw.v(lambda: V.tensor_reduce(out=gm[:, 4:5], in_=tmp8[:], axis=AX.X, op=ALU.max), reads=[tmp8], writes=[gm])
                fw.v(lambda: V.tensor_scalar(out=oh2[:], in0=tmp8[:], scalar1=gm[:, 4:5], scalar2=None, op0=ALU.is_equal), reads=[tmp8, gm], writes=[oh2])
                yield
                fw.v(lambda: V.tensor_tensor(out=gm[:, 5:6], in0=gm[:, 4:5], in1=gm[:, 3:4], op=ALU.subtract), reads=[gm], writes=[gm])
                fw.a(lambda: A.activation(out=gm[:, 5:6], in_=gm[:, 5:6], func=AF.Exp), reads=[gm], writes=[gm])
                fw.v(lambda: V.tensor_scalar(out=gm[:, 6:7], in0=gm[:, 5:6], scalar1=1.0, scalar2=None, op0=ALU.add), reads=[gm], writes=[gm])
                fw.v(lambda: V.reciprocal(out=gm[:, 6:7], in_=gm[:, 6:7]), reads=[gm], writes=[gm])
                fw.v(lambda: V.tensor_tensor(out=gm[:, 6:7], in0=gm[:, 6:7], in1=gm[:, 2:3], op=ALU.mult), reads=[gm], writes=[gm])
                fw.v(lambda: V.tensor_tensor(out=gm[:, 7:8], in0=gm[:, 6:7], in1=gm[:, 5:6], op=ALU.mult), reads=[gm], writes=[gm])
                fw.v(lambda: V.tensor_scalar(out=w8[:], in0=oh1[:], scalar1=gm[:, 6:7], scalar2=None, op0=ALU.mult), reads=[oh1, gm], writes=[w8])
                fw.v(lambda: V.scalar_tensor_tensor(out=w8[:], in0=oh2[:], scalar=gm[:, 7:8], in1=w8[:], op0=ALU.mult, op1=ALU.add), reads=[oh2, gm, w8], writes=[w8])
                for g in range(4):
                    fw.v(lambda: V.tensor_scalar(out=wE[:, t, g * 8:(g + 1) * 8], in0=w8[:], scalar1=ohg[:, g:g + 1], scalar2=None, op0=ALU.mult),
                         reads=[w8, ohg], writes=[wE])
                    fw.v(lambda: V.tensor_scalar(out=A1[:, t, g * 8:(g + 1) * 8], in0=oh1[:], scalar1=ohg[:, g:g + 1], scalar2=None, op0=ALU.mult),
                         reads=[oh1, ohg], writes=[A1])
                    fw.v(lambda: V.tensor_scalar(out=A2[:, t, g * 8:(g + 1) * 8], in0=oh2[:], scalar1=ohg[:, g:g + 1], scalar2=None, op0=ALU.mult),
                         reads=[oh2, ohg], writes=[A2])
                fw.v(lambda: V.tensor_copy(out=W01[:, t, :], in_=gm[:, 6:8]), reads=[gm], writes=[W01])
                yield

            gens, t_next, rounds = [], 0, 0
            while gens or t_next < NT:
                if t_next < NT and len(gens) < 2 and (not gens or rounds % 8 == 0):
                    gens.append(tile_gen(t_next)); t_next += 1
                for g_ in list(gens):
                    try:
                        next(g_)
                    except StopIteration:
                        gens.remove(g_)
                rounds += 1
        fw.barrier()


        def dmaf(eng, fn, reads, writes):
            fw._deps(eng, reads, writes)
            owner = writes[0]
            if owner.dsem is None:
                owner.dsem = fw.new_sem("d_" + owner.name)
                fw.dbufs.append(owner)
            inst = fn()
            owner.dcount += 16
            inst.then_inc(owner.dsem, 16)
            tok = (owner.dsem, owner.dcount)
            for b in reads:
                b.reads.append(tok)
            for b in writes:
                b.last_w = tok
                b.reads = []
            fw.n_inst += 1

        pump_conv(len(conv_jobs))
        for st4 in _scope("moe" in phases and ne > 0 and sparse):
            s0i = fw.sbuf(st4, "s0i", [128, NT], I32); s1i = fw.sbuf(st4, "s1i", [128, NT], I32)
            ebi = fw.sbuf(st4, "ebi", [128, 2, NBLK], I32)
            iop = fw.sbuf(st4, "iop", [128, 1], F32)
            fw.dma(sp, iop[:], iota_p[:, :], reads=[DB["iota_p"]], writes=[iop])
            for st in _scope(True):
                Aa = fw.sbuf(st, "Aa", [128, NT, NE], F32)
                Acum = fw.sbuf(st, "Acum", [128, NE], F32)
                Pr = fw.sbuf(st, "Pr", [128, NT, NE], F32)
                cnt = fw.sbuf(st, "cnt", [128, NE], F32); pad = fw.sbuf(st, "pad", [128, NE], F32)
                sa = fw.sbuf(st, "sa", [128, NE], F32); sb = fw.sbuf(st, "sb", [128, NE], F32)
                pst = fw.sbuf(st, "pst", [128, NE], F32); pen = fw.sbuf(st, "pen", [128, NE], F32)
                tmpA = fw.sbuf(st, "tmpA", [128, NT, NE], F32)
                s0f = fw.sbuf(st, "s0f", [128, NT], F32); s1f = fw.sbuf(st, "s1f", [128, NT], F32)
                ebf = fw.sbuf(st, "ebf", [128, NBLK], F32); cmpb = fw.sbuf(st, "cmpb", [128, NE], F32)
                ps_r = fw.psum(st, "r_ps", [128, NE], F32)
                fw.v(lambda: V.tensor_tensor(out=Aa[:], in0=A1[:], in1=A2[:], op=ALU.add), reads=[A1, A2], writes=[Aa])
                fw.v(lambda: V.memset(Acum[:], 0.0), writes=[Acum])
                for t in range(NT):
                    fw.mm(ps_r, ps_r[:], trif, trif[:, 3, :], Aa, Aa[:, t, :], start=True, stop=False)
                    fw.mm(ps_r, ps_r[:], onesm, onesm[:], Acum, Acum[:], start=False, stop=True)
                    fw.a(lambda: A.copy(out=Pr[:, t, :], in_=ps_r[:]), reads=[ps_r], writes=[Pr])
                    fw.v(lambda: V.tensor_tensor(out=Acum[:], in0=Acum[:], in1=Aa[:, t, :], op=ALU.add), reads=[Acum, Aa], writes=[Acum])
                fw.mm(ps_r, ps_r[:], onesm, onesm[:], Acum, Acum[:])
                fw.a(lambda: A.copy(out=cnt[:], in_=ps_r[:]), reads=[ps_r], writes=[cnt])
                fw.v(lambda: V.tensor_scalar(out=sa[:], in0=cnt[:], scalar1=1.0 / 128, scalar2=0.49609375, op0=ALU.mult, op1=ALU.add), reads=[cnt], writes=[sa])
                fw.v(lambda: V.tensor_scalar(out=sb[:], in0=sa[:], scalar1=8388608.0, scalar2=None, op0=ALU.add), reads=[sa], writes=[sb])
                fw.v(lambda: V.tensor_scalar(out=sa[:], in0=sb[:], scalar1=-8388608.0, scalar2=None, op0=ALU.add), reads=[sb], writes=[sa])
                fw.v(lambda: V.tensor_scalar(out=pad[:], in0=sa[:], scalar1=128.0, scalar2=None, op0=ALU.mult), reads=[sa], writes=[pad])
                fw.v(lambda: V.tensor_copy(out=sa[:], in_=pad[:]), reads=[pad], writes=[sa])
                cur, nxt = sa, sb
                for sh in (1, 2, 4, 8, 16):
                    fw.v(lambda: V.tensor_copy(out=nxt[:, 0:sh], in_=cur[:, 0:sh]), reads=[cur], writes=[nxt])
                    fw.v(lambda: V.tensor_tensor(out=nxt[:, sh:NE], in0=cur[:, sh:NE], in1=cur[:, 0:NE - sh], op=ALU.add), reads=[cur], writes=[nxt])
                    cur, nxt = nxt, cur
                fw.v(lambda: V.tensor_copy(out=pen[:], in_=cur[:]), reads=[cur], writes=[pen])
                fw.v(lambda: V.tensor_tensor(out=pst[:], in0=pen[:], in1=pad[:], op=ALU.subtract), reads=[pen, pad], writes=[pst])
                for t in range(NT):
                    fw.v(lambda: V.tensor_tensor(out=Pr[:, t, :], in0=Pr[:, t, :], in1=pst[:], op=ALU.add), reads=[Pr, pst], writes=[Pr])
                for (Ak, sf, si) in ((A1, s0f, s0i), (A2, s1f, s1i)):
                    fw.v(lambda: V.tensor_tensor(out=tmpA[:], in0=Ak[:], in1=Pr[:], op=ALU.mult), reads=[Ak, Pr], writes=[tmpA])
                    fw.v(lambda: V.tensor_reduce(out=sf[:], in_=tmpA[:], axis=AX.X, op=ALU.add), reads=[tmpA], writes=[sf])
                    fw.v(lambda: V.tensor_copy(out=si[:], in_=sf[:]), reads=[sf], writes=[si])
                for b in range(NBLK):
                    fw.v(lambda: V.tensor_scalar(out=cmpb[:], in0=pen[:], scalar1=float(128 * b), scalar2=None, op0=ALU.is_le), reads=[pen], writes=[cmpb])
                    fw.v(lambda: V.tensor_reduce(out=ebf[:, b:b + 1], in_=cmpb[:], axis=AX.X, op=ALU.add), reads=[cmpb], writes=[ebf])
                fw.v(lambda: V.tensor_scalar(out=ebf[:], in0=ebf[:], scalar1=float(ne - 1), scalar2=None, op0=ALU.min), reads=[ebf], writes=[ebf])
                fw.v(lambda: V.tensor_scalar(out=ebf[:], in0=ebf[:], scalar1=128.0, scalar2=iop[:, 0:1], op0=ALU.mult, op1=ALU.add), reads=[ebf, iop], writes=[ebf])
                fw.v(lambda: V.tensor_copy(out=ebi[:, 0, :], in_=ebf[:]), reads=[ebf], writes=[ebi])
                fw.v(lambda: V.tensor_scalar(out=ebf[:], in0=ebf[:], scalar1=float(ne * 128), scalar2=None, op0=ALU.add), reads=[ebf], writes=[ebf])
                fw.v(lambda: V.tensor_copy(out=ebi[:, 1, :], in_=ebf[:]), reads=[ebf], writes=[ebi])
            fw.barrier()
            for st in _scope(True):
                xrow = [fw.sbuf(st, "xrow%d" % i, [128, D], BF16) for i in range(2)]
                for t in range(NT):
                    xr = xrow[t % 2]
                    fw.dma(sp, xr[:], xn2_d[t * 128:(t + 1) * 128, :], reads=[xn2B], writes=[xr])
                    for si in (s0i, s1i):
                        dmaf(pool, lambda: nc.gpsimd.indirect_dma_start(out=xsort_d[:, :], out_offset=bass.IndirectOffsetOnAxis(ap=si[:, t:t + 1], axis=0),
                                                                        in_=xr[:], in_offset=None), reads=[xr, si], writes=[xsortB])
            fw.barrier()
            for st in _scope(True):
                xblk = [fw.sbuf(st, "xblk%d" % i, [128, D], BF16) for i in range(2)]
                xsT = [fw.sbuf(st, "xsT%d" % i, [128, 16, 128], BF16) for i in range(2)]
                wgb = [fw.sbuf(st, "wgb%d" % i, [128, 16, 512], BF16) for i in range(2)]
                wub = [fw.sbuf(st, "wub%d" % i, [128, 16, 512], BF16) for i in range(2)]
                wdb = [fw.sbuf(st, "wdb%d" % i, [128, 4, D], BF16) for i in range(2)]
                hidT = [fw.sbuf(st, "hidT%d" % i, [128, 512], BF16) for i in range(2)]
                outb = [fw.sbuf(st, "outb%d" % i, [128, D], F32) for i in range(2)]
                ps_t = [fw.psum(st, "m_pst%d" % i, [128, 4, 128], BF16) for i in range(2)]
                hidm = [fw.sbuf(st, "hidm%d" % i, [128, 512], BF16) for i in range(2)]
                pg = fw.psum(st, "m_pg", [128, 512], F32); pu = fw.psum(st, "m_pu", [128, 512], F32)
                po = [fw.psum(st, "m_po%d" % i, [128, 512], F32) for i in range(4)]
                it = 0
                for b in range(NBLK):
                    xb_, xT_ = xblk[b % 2], xsT[b % 2]
                    fw.dma(sp, xb_[:], xsort_d[b * 128:(b + 1) * 128, :], reads=[xsortB], writes=[xb_])
                    for g in range(4):
                        p = ps_t[g % 2]
                        for j in range(4):
                            kc = 4 * g + j
                            fw.tr(p, p[:, j, :], xb_, xb_[:, kc * 128:(kc + 1) * 128], idb)
                        if g % 2 == 0:
                            fw.v(lambda: V.tensor_copy(out=xT_[:, 4 * g:4 * g + 4, :], in_=p[:]), reads=[p], writes=[xT_])
                        else:
                            fw.a(lambda: A.copy(out=xT_[:, 4 * g:4 * g + 4, :], in_=p[:]), reads=[p], writes=[xT_])
                    for half in range(2):
                        s_ = it % 2; it += 1
                        for (tab, tB, dstb) in ((wgc, wgcB, wgb[s_]), (wuc, wucB, wub[s_]), (wdc, wdcB, wdb[s_])):
                            dmaf(pool, lambda: nc.gpsimd.indirect_dma_start(out=dstb[:].rearrange("p k n -> p (k n)"), out_offset=None, in_=tab[:, :],
                                                                            in_offset=bass.IndirectOffsetOnAxis(ap=ebi[:, half, b:b + 1], axis=0)),
                                 reads=[tB, ebi], writes=[dstb])
                        for kc in range(16):
                            fw.mm(pg, pg[:], xT_, xT_[:, kc, :], wgb[s_], wgb[s_][:, kc, :], start=(kc == 0), stop=(kc == 15))
                        for kc in range(16):
                            fw.mm(pu, pu[:], xT_, xT_[:, kc, :], wub[s_], wub[s_][:, kc, :], start=(kc == 0), stop=(kc == 15))
                        hm = hidm[s_]
                        fw.a(lambda: A.activation(out=hm[:], in_=pg[:], func=AF.Silu), reads=[pg], writes=[hm])
                        fw.v(lambda: V.tensor_tensor(out=hm[:], in0=hm[:], in1=pu[:], op=ALU.mult), reads=[hm, pu], writes=[hm])
                        hT = hidT[s_]
                        p = ps_t[s_]
                        for f in range(4):
                            fw.tr(p, p[:, f, :], hm, hm[:, f * 128:(f + 1) * 128], idb)
                        fw.v(lambda: V.tensor_copy(out=hT[:].rearrange("p (f n) -> p f n", f=4), in_=p[:]), reads=[p], writes=[hT])
                        for dc in range(4):
                            for f in range(4):
                                first = (half == 0 and f == 0); last = (half == 1 and f == 3)
                                fw.op(fw.pe, lambda: nc.tensor.matmul(po[dc][:], lhsT=hT[:, f * 128:(f + 1) * 128], rhs=wdb[s_][:, f, dc * 512:(dc + 1) * 512], start=first, stop=last),
                                      reads=[hT, wdb[s_]], writes=[po[dc]], inc=(f == 3))
                    ob = outb[b % 2]
                    for dc in range(4):
                        if dc % 2 == 0:
                            fw.a(lambda: A.copy(out=ob[:, dc * 512:(dc + 1) * 512], in_=po[dc][:]), reads=[po[dc]], writes=[ob])
                        else:
                            fw.v(lambda: V.tensor_copy(out=ob[:, dc * 512:(dc + 1) * 512], in_=po[dc][:]), reads=[po[dc]], writes=[ob])
                    fw.dma(sp, oall_d[b * 128:(b + 1) * 128, :], ob[:], reads=[ob], writes=[oallB])
            fw.barrier()
            for st in _scope(True):
                g0 = [fw.sbuf(st, "g0_%d" % i, [128, D], F32) for i in range(2)]
                g1 = [fw.sbuf(st, "g1_%d" % i, [128, D], F32) for i in range(2)]
                hin = [fw.sbuf(st, "c_hin%d" % i, [128, D], F32) for i in range(2)]
                for t in range(NT):
                    rows = slice(t * 128, (t + 1) * 128)
                    a0, a1, hh = g0[t % 2], g1[t % 2], hin[t % 2]
                    fw.dma(sp, hh[:], h_d[rows, :], reads=[hB], writes=[hh])
                    dmaf(pool, lambda: nc.gpsimd.indirect_dma_start(out=a0[:], out_offset=None, in_=oall_d[:, :],
                                                                    in_offset=bass.IndirectOffsetOnAxis(ap=s0i[:, t:t + 1], axis=0)), reads=[oallB, s0i], writes=[a0])
                    dmaf(pool, lambda: nc.gpsimd.indirect_dma_start(out=a1[:], out_offset=None, in_=oall_d[:, :],
                                                                    in_offset=bass.IndirectOffsetOnAxis(ap=s1i[:, t:t + 1], axis=0)), reads=[oallB, s1i], writes=[a1])
                    fw.v(lambda: V.scalar_tensor_tensor(out=hh[:], in0=a0[:], scalar=W01[:, t, 0:1], in1=hh[:], op0=ALU.mult, op1=ALU.add), reads=[a0, W01, hh], writes=[hh])
                    fw.v(lambda: V.scalar_tensor_tensor(out=hh[:], in0=a1[:], scalar=W01[:, t, 1:2], in1=hh[:], op0=ALU.mult, op1=ALU.add), reads=[a1, W01, hh], writes=[hh])
                    fw.dma(sp, y[rows, :], hh[:], reads=[hh], writes=[yB])
        fw.barrier()

        for st in _scope("moe" in phases and ne > 0 and not sparse):
            xsT = fw.sbuf(st, "xsT", [128, 16, 512], BF16)
            yacc = fw.sbuf(st, "yacc", [128, 4, D], F32)
            hin = fw.sbuf(st, "hin", [128, D], F32)
            wgb = [fw.sbuf(st, "wgb%d" % i, [128, 16, 512], BF16) for i in range(2)]
            wub = [fw.sbuf(st, "wub%d" % i, [128, 16, 512], BF16) for i in range(2)]
            wdb = [fw.sbuf(st, "wdb%d" % i, [128, 4, D], BF16) for i in range(2)]
            hid = [[fw.sbuf(st, "hid%d_%d" % (i, f), [128, 512], BF16) for f in range(4)] for i in range(2)]
            pg = [fw.psum(st, "pg%d" % i, [128, 512], F32) for i in range(2)]
            pu = [fw.psum(st, "pu%d" % i, [128, 512], F32) for i in range(2)]
            pd = [fw.psum(st, "pd%d" % i, [128, 512], F32) for i in range(2)]
            it = 0
            for sti in range(NT // 4):
                for tt in range(4):
                    fw.dma(sp, xsT[:, :, tt * 128:(tt + 1) * 128], xs_d[sti * 4 + tt, :, :, :], reads=[xsB], writes=[xsT])
                fw.v(lambda: V.memset(yacc[:], 0.0), writes=[yacc])
                for e in range(ne):
                    for half in range(2):
                        s = it % 2
                        c0 = half * 512
                        r0 = (half * ne + e) * 128
                        fw.dma(pool, wgb[s][:].rearrange("p k n -> p (k n)"), wg[r0:r0 + 128, :], reads=[DB["wg"]], writes=[wgb[s]])
                        fw.dma(pool, wub[s][:].rearrange("p k n -> p (k n)"), wu[r0:r0 + 128, :], reads=[DB["wu"]], writes=[wub[s]])
                        fw.dma(pool, wdb[s][:].rearrange("p k n -> p (k n)"), wd[r0:r0 + 128, :], reads=[DB["wd"]], writes=[wdb[s]])
                        for f in range(4):
                            ps = (it * 4 + f) % 2
                            for kc in range(16):
                                fw.mm(pg[ps], pg[ps][:], wgb[s], wgb[s][:, kc, f * 128:(f + 1) * 128], xsT, xsT[:, kc, :], start=(kc == 0), stop=(kc == 15))
                            for kc in range(16):
                                fw.mm(pu[ps], pu[ps][:], wub[s], wub[s][:, kc, f * 128:(f + 1) * 128], xsT, xsT[:, kc, :], start=(kc == 0), stop=(kc == 15))
                            hs = hid[s][f]
                            fw.a(lambda: A.activation(out=hs[:], in_=pg[ps][:], func=AF.Silu), reads=[pg[ps]], writes=[hs])
                            fw.v(lambda: V.tensor_tensor(out=hs[:], in0=hs[:], in1=pu[ps][:], op=ALU.mult), reads=[hs, pu[ps]], writes=[hs])
                        j = 0
                        for tt in range(4):
                            for dc in range(4):
                                pp = pd[j % 2]; j += 1
                                for f in range(4):
                                    fw.mm(pp, pp[:], hid[s][f], hid[s][f][:, tt * 128:(tt + 1) * 128], wdb[s], wdb[s][:, f, dc * 512:(dc + 1) * 512],
                                          start=(f == 0), stop=(f == 3))
                                fw.v(lambda: V.scalar_tensor_tensor(out=yacc[:, tt, dc * 512:(dc + 1) * 512], in0=pp[:], scalar=wE[:, sti * 4 + tt, e:e + 1],
                                                                    in1=yacc[:, tt, dc * 512:(dc + 1) * 512], op0=ALU.mult, op1=ALU.add),
                                     reads=[pp, wE, yacc], writes=[yacc])
                        it += 1
                for tt in range(4):
                    rows = slice((sti * 4 + tt) * 128, (sti * 4 + tt + 1) * 128)
                    fw.dma(sp, hin[:], h_d[rows, :], reads=[hB], writes=[hin])
                    fw.v(lambda: V.tensor_tensor(out=yacc[:, tt, :], in0=yacc[:, tt, :], in1=hin[:], op=ALU.add), reads=[yacc, hin], writes=[yacc])
                    fw.dma(sp, y[rows, :], yacc[:, tt, :], reads=[yacc], writes=[yB])
        if not ("moe" in phases and ne > 0):
            for st in _scope(True):
                hin = fw.sbuf(st, "dbg_h", [128, D], F32)
                for t in range(NT):
                    fw.dma(sp, hin[:], h_d[t * 128:(t + 1) * 128, :], reads=[hB], writes=[hin])
                    fw.dma(sp, y[t * 128:(t + 1) * 128, :], hin[:], reads=[hin], writes=[yB])
        fw.finish(fw.sp, [yB])
        fw.barrier()
        stats = (fw.n_inst, fw.n_wait, fw.nsem)
    return nc, stats


def _rope_tables(pos):
    half = 16
    inv_freq = np.power(np.float32(500000.0), -np.arange(half, dtype=np.float32) * np.float32(2.0 / 32)).astype(np.float32)
    ang = pos.astype(np.float32)[:, None] * inv_freq[None, :]
    return np.cos(ang).astype(np.float32), np.sin(ang).astype(np.float32)


def make_in_maps(inputs, cores=range(NCORES), ne=NE, npre=NPRE):
    f32 = np.float32
    bf = ml_dtypes.bfloat16
    x = np.asarray(inputs["x"], f32).reshape(SEQ, D)
    w_in = np.ascontiguousarray(np.asarray(inputs["w_in"], f32)[0])
    rep = lambda v: np.ascontiguousarray(np.broadcast_to(np.asarray(v, f32).reshape(1, -1), (128, np.asarray(v).size)))
    j = np.arange(128)[:, None]; i = np.arange(128)[None, :]
    tri = np.stack([(j <= i), (j >= i), (j > i), (j < i)]).astype(f32)
    mP = np.tile((j >= i).astype(f32), (1, 4)).astype(bf)
    mN = np.tile((j <= i).astype(f32), (1, 4)).astype(bf)
    w_lr = np.zeros((D, 64), f32); w_lr[:, 0:16] = w_in[:, C_LR:C_LR + 16]; w_lr[:, 32:48] = w_in[:, C_LR + 16:C_LR + 32]
    up_pad = np.zeros((64, 512), f32)
    up_pad[0:16] = np.asarray(inputs["gla_gate_up_f"], f32)[0]; up_pad[16] = np.asarray(inputs["gla_gate_bias_f"], f32)[0]
    up_pad[32:48] = np.asarray(inputs["gla_gate_up_b"], f32)[0]; up_pad[48] = np.asarray(inputs["gla_gate_bias_b"], f32)[0]
    w_rt = np.ascontiguousarray(np.concatenate([np.asarray(inputs["w_group"], f32)[0], np.asarray(inputs["w_router"], f32)[0]], axis=1))
    b_rt = rep(np.concatenate([np.asarray(inputs["b_group"], f32)[0], np.asarray(inputs["b_router"], f32)[0]]))
    main_bi = np.zeros((64, 1), f32); main_bi[16] = 1.0; main_bi[48] = 1.0
    shared = dict(
        tri=tri, maskP=mP, maskN=mN, ident_b=np.eye(128, dtype=f32).astype(bf), ident_f=np.eye(128, dtype=f32),
        ones_c=np.ones((128, 1), f32), ones_m=np.ones((128, 128), f32), iota_p=np.arange(128, dtype=f32).reshape(128, 1), ln1_bc=rep(inputs["ln1_w"]), w_in=w_in, w_lr=w_lr,
        qn_bc=rep(inputs["q_norm_w"]), kn_bc=rep(inputs["k_norm_w"]), sink_bc=rep(inputs["attn_sink"]),
        an_bc=rep(inputs["attn_out_norm_w"]), up_pad=up_pad, gn_bc=rep(inputs["gla_out_norm_w"]),
        w_out=np.ascontiguousarray(np.asarray(inputs["w_out"], f32)[0]), ln2_bc=rep(inputs["ln2_w"]), w_rt=w_rt, b_rt=b_rt,
        main_bi=main_bi,
    )
    if ne > 0:
        gu = lambda w: np.ascontiguousarray(np.asarray(w, f32)[0][:ne].reshape(ne, 16, 128, 2, 512).transpose(3, 0, 2, 1, 4)).reshape(2 * ne * 128, 8192)
        shared.update(wg=gu(inputs["w_gate_e"]), wu=gu(inputs["w_up_e"]),
                      wd=np.ascontiguousarray(np.asarray(inputs["w_down_e"], f32)[0][:ne].reshape(ne, 2, 4, 128, D).transpose(1, 0, 3, 2, 4)).reshape(2 * ne * 128, 8192))
    maps = []
    nch = SEQ // 128
    for c in cores:
        m = dict(shared)
        lo = c * TPC - 128
        xh = np.zeros((NTH * 128, D), f32)
        a, b = max(lo, 0), min(lo + NTH * 128, SEQ)
        xh[a - lo:b - lo] = x[a:b]
        m["xh"] = xh
        pos = np.arange(lo, lo + NTH * 128)
        cs, sn = _rope_tables(pos)
        m["cosq"] = np.ascontiguousarray(np.tile(cs, (1, 8))); m["sinq"] = np.ascontiguousarray(np.tile(sn, (1, 8)))
        m["maskP0"] = mP if c > 0 else np.zeros_like(mP)
        m["maskNL"] = mN if c < NCORES - 1 else np.zeros_like(mN)
        fwd = list(range(0, c * NT))
        bwd = list(range(nch - 1, (c + 1) * NT - 1, -1))
        slots = (fwd + bwd)[:npre] if npre < NPRE else (fwd + bwd)
        n = max(npre, 1)
        xp = np.zeros((n * 128, D), f32)
        sc = np.zeros((64, n), f32); bi = np.zeros((64, n), f32); pm = np.zeros((128, n, 2), f32)
        pf = np.zeros((128, n, 3), f32); pf[:, :, 0] = 1.0
        nf_ = min(len(fwd), len(slots))
        if nf_ < len(slots):
            pf[:, nf_, 0] = 0.0
            pf[:, :, 2] = 1.0
        if nf_ > 0:
            pf[:, nf_ - 1, 1] = 1.0
        for s, ch in enumerate(slots):
            xp[s * 128:(s + 1) * 128] = x[ch * 128:(ch + 1) * 128]
            isf = s < len(fwd)
            if isf:
                sc[0:32, s] = 1.0; bi[16, s] = 1.0; pm[:, s, 0] = 1.0
            else:
                sc[32:64, s] = 1.0; bi[48, s] = 1.0; pm[:, s, 1] = 1.0
        m["xpre"] = xp; m["pre_sc"] = sc; m["pre_bi"] = bi; m["pre_m"] = pm; m["pre_f"] = pf
        maps.append(m)
    return maps


_CACHE = {}


def kernel(**inputs):
    if "nc" not in _CACHE:
        _CACHE["nc"] = build_program()[0]
    nc = _CACHE["nc"]
    maps = make_in_maps(inputs)
    res = run_bass_kernel_spmd(nc, maps, core_ids=list(range(NCORES)))
    out = np.concatenate([np.asarray(r["y"], np.float32) for r in res.results], axis=0)
    return out.reshape(1, SEQ, D)
```

```python
import math
import os
from contextlib import ExitStack

import numpy as np
import ml_dtypes
import concourse.bass as bass
import concourse.mybir as mybir
from concourse.bass_utils import run_bass_kernel_spmd

F32 = mybir.dt.float32
BF16 = mybir.dt.bfloat16
I32 = mybir.dt.int32
NBLK = 64
ALU = mybir.AluOpType
AF = mybir.ActivationFunctionType
AX = mybir.AxisListType

KDBG = os.environ.get('KDBG', '')
NCORES = 8
SEQ = 16384
D = 2048
TPC = SEQ // NCORES
NT = TPC // 128
NTH = NT + 2
NPRE = (NCORES - 1) * NT
INW = 4640
NE = 32
DFF = 1024
EPS = 1e-6
C_AQ, C_AK, C_AV, C_GQ, C_GK, C_GV, C_GR, C_LR = 0, 1024, 1280, 1536, 2048, 2560, 3584, 4608


class Eng:
    def __init__(self, fw, name, handle):
        self.name = name
        self.h = handle
        self.sem = fw.new_sem("s_" + name)
        self.count = 0
        self.seen = {}
        self.pend = []


class Buf:
    def __init__(self, fw, t, name=""):
        self.t = t
        self.name = name
        self.last_w = None
        self.reads = []
        self.dsem = None
        self.dcount = 0
        self.is_psum = False

    def __getitem__(self, idx):
        return self.t[idx]


class FW:
    def __init__(self, nc, stack):
        self.nc = nc
        self.gstack = stack
        self.nsem = 0
        self.pe = Eng(self, "pe", nc.tensor)
        self.dve = Eng(self, "dve", nc.vector)
        self.act = Eng(self, "act", nc.scalar)
        self.pool = Eng(self, "pool", nc.gpsimd)
        self.sp = Eng(self, "sp", nc.sync)
        self.pe.seen[id(self.pe.sem)] = 1 << 60
        self.engs = [self.pe, self.dve, self.act, self.pool, self.sp]
        self.dbufs = []
        self.n_inst = 0
        self.n_wait = 0

    def new_sem(self, name):
        self.nsem += 1
        return self.gstack.enter_context(self.nc.semaphore("%s_%d" % (name, self.nsem)))

    def sbuf(self, st, name, shape, dt):
        return Buf(self, st.enter_context(self.nc.sbuf_tensor(name, list(shape), dt)), name)

    def psum(self, st, name, shape, dt):
        b = Buf(self, st.enter_context(self.nc.psum_tensor(name, list(shape), dt)), name)
        b.is_psum = True
        return b

    def _wait(self, eng, dep):
        if dep is None:
            return
        sem, val = dep
        key = id(sem)
        if eng.seen.get(key, 0) >= val:
            return
        eng.h.wait_ge(sem, val)
        eng.seen[key] = val
        self.n_wait += 1

    def _deps(self, eng, reads, writes):
        for b in reads:
            self._wait(eng, b.last_w)
            if b.is_psum:
                for r in b.reads:
                    self._wait(eng, r)
        for b in writes:
            self._wait(eng, b.last_w)
            for r in b.reads:
                self._wait(eng, r)

    def op(self, eng, fn, reads=(), writes=(), inc=True):
        self._deps(eng, reads, writes)
        inst = fn()
        self.n_inst += 1
        eng.pend.append((tuple(reads), tuple(writes)))
        if not inc:
            return inst
        eng.count += 1
        inst.then_inc(eng.sem, 1)
        tok = (eng.sem, eng.count)
        for rs, ws in eng.pend:
            for b in rs:
                b.reads.append(tok)
            for b in ws:
                b.last_w = tok
                b.reads = []
        eng.pend = []
        return inst

    def v(self, fn, reads=(), writes=()):
        return self.op(self.dve, fn, reads, writes)

    def a(self, fn, reads=(), writes=()):
        return self.op(self.act, fn, reads, writes)

    def mm(self, outB, out_ap, lB, l_ap, rB, r_ap, start=True, stop=True):
        nc = self.nc
        return self.op(self.pe, lambda: nc.tensor.matmul(out_ap, lhsT=l_ap, rhs=r_ap, start=start, stop=stop),
                       reads=[lB, rB], writes=[outB], inc=stop)

    def tr(self, outB, out_ap, inB, in_ap, idB):
        nc = self.nc
        return self.op(self.pe, lambda: nc.tensor.transpose(out=out_ap, in_=in_ap, identity=idB[:]),
                       reads=[inB, idB], writes=[outB])

    def dma(self, eng, out_ap, in_ap, reads=(), writes=()):
        self._deps(eng, reads, writes)
        owner = writes[0] if writes else reads[0]
        if owner.dsem is None:
            owner.dsem = self.new_sem("d_" + owner.name)
            self.dbufs.append(owner)
        inst = eng.h.dma_start(out=out_ap, in_=in_ap)
        owner.dcount += 16
        inst.then_inc(owner.dsem, 16)
        tok = (owner.dsem, owner.dcount)
        for b in reads:
            b.reads.append(tok)
        for b in writes:
            b.last_w = tok
            b.reads = []
        self.n_inst += 1
        return inst

    def barrier(self):
        for e in self.engs:
            assert not e.pend
        for e in self.engs:
            for o in self.engs:
                if o is not e and o.count > 0:
                    self._wait(e, (o.sem, o.count))
            for b in self.dbufs:
                self._wait(e, (b.dsem, b.dcount))

    def finish(self, eng, bufs):
        for b in bufs:
            self._wait(eng, b.last_w)
            for r in b.reads:
                self._wait(eng, r)


def _scope(flag):
    if flag:
        with ExitStack() as st:
            yield st


def build_program(ne=NE, npre=NPRE, phases=("pre", "xn", "attn", "gla", "out", "moe"), sparse=True):
    nc = bass.Bass("TRN2", target_bir_lowering=False)
    dt_in = {}

    def din(name, shape, dt=F32):
        t = nc.dram_tensor(name, list(shape), dt, kind="ExternalInput")
        dt_in[name] = t
        return t

    xh = din("xh", [NTH * 128, D])
    xpre = din("xpre", [max(npre, 1) * 128, D])
    pre_sc = din("pre_sc", [64, max(npre, 1)])
    pre_bi = din("pre_bi", [64, max(npre, 1)])
    pre_m = din("pre_m", [128, max(npre, 1), 2])
    pre_f = din("pre_f", [128, max(npre, 1), 3])
    main_bi = din("main_bi", [64, 1])
    cosq = din("cosq", [NTH * 128, 128])
    sinq = din("sinq", [NTH * 128, 128])
    maskP = din("maskP", [128, 512], BF16)
    maskN = din("maskN", [128, 512], BF16)
    maskP0 = din("maskP0", [128, 512], BF16)
    maskNL = din("maskNL", [128, 512], BF16)
    tri = din("tri", [4, 128, 128])
    ident_b = din("ident_b", [128, 128], BF16)
    ident_f = din("ident_f", [128, 128])
    ones_c = din("ones_c", [128, 1])
    ln1_bc = din("ln1_bc", [128, D])
    w_in = din("w_in", [D, INW])
    w_lr = din("w_lr", [D, 64])
    qn_bc = din("qn_bc", [128, 128])
    kn_bc = din("kn_bc", [128, 128])
    sink_bc = din("sink_bc", [128, 8])
    an_bc = din("an_bc", [128, 1024])
    up_pad = din("up_pad", [64, 512])
    gn_bc = din("gn_bc", [128, 256])
    w_out = din("w_out", [D, D])
    ln2_bc = din("ln2_bc", [128, D])
    w_rt = din("w_rt", [D, 36])
    b_rt = din("b_rt", [128, 36])
    if ne > 0:
        wg = din("wg", [2 * ne * 128, 8192])
        wu = din("wu", [2 * ne * 128, 8192])
        wd = din("wd", [2 * ne * 128, 8192])
    y = nc.dram_tensor("y", [TPC, D], F32, kind="ExternalOutput")
    mix_d = nc.dram_tensor("mix_d", [TPC, D], BF16, kind="Internal")
    h_d = nc.dram_tensor("h_d", [TPC, D], F32, kind="Internal")
    xs_d = nc.dram_tensor("xs_d", [NT, 128, 16, 128], BF16, kind="Internal")
    xn2_d = nc.dram_tensor("xn2_d", [TPC, D], BF16, kind="Internal")
    xsort_d = nc.dram_tensor("xsort_d", [NBLK * 128, D], BF16, kind="Internal")
    oall_d = nc.dram_tensor("oall_d", [NBLK * 128, D], F32, kind="Internal")
    ones_m = din("ones_m", [128, 128])
    if ne > 0 and sparse:
        wgc = nc.dram_tensor("wgc", [2 * ne * 128, 8192], BF16, kind="Internal")
        wuc = nc.dram_tensor("wuc", [2 * ne * 128, 8192], BF16, kind="Internal")
        wdc = nc.dram_tensor("wdc", [2 * ne * 128, 8192], BF16, kind="Internal")
    iota_p = din("iota_p", [128, 1])

    V, A = nc.vector, nc.scalar
    QS = 1.0 / math.sqrt(128.0)

    with ExitStack() as gst:
        fw = FW(nc, gst)
        DB = {k: Buf(fw, t, k) for k, t in dt_in.items()}
        yB = Buf(fw, y, "y"); mixB = Buf(fw, mix_d, "mix_d"); hB = Buf(fw, h_d, "h_d"); xsB = Buf(fw, xs_d, "xs_d")
        xn2B = Buf(fw, xn2_d, "xn2_d"); xsortB = Buf(fw, xsort_d, "xsort_d"); oallB = Buf(fw, oall_d, "oall_d")
        conv_jobs = []
        if ne > 0 and sparse and "moe" in phases:
            wgcB = Buf(fw, wgc, "wgc"); wucB = Buf(fw, wuc, "wuc"); wdcB = Buf(fw, wdc, "wdc")
            for r0 in range(0, 2 * ne * 128, 128):
                for (src, nm, dst, dB) in ((wg, "wg", wgc, wgcB), (wu, "wu", wuc, wucB), (wd, "wd", wdc, wdcB)):
                    conv_jobs.append((src, nm, dst, dB, r0))

        def pump_conv(n):
            for _ in range(min(n, len(conv_jobs))):
                src, nm, dst, dB, r0 = conv_jobs.pop(0)
                fw.dma(pool, dst[r0:r0 + 128, :], src[r0:r0 + 128, :], reads=[DB[nm]], writes=[dB])
        sp, pool = fw.sp, fw.pool

        idb = fw.sbuf(gst, "idb", [128, 128], BF16)
        idf = fw.sbuf(gst, "idf", [128, 128], F32)
        ones = fw.sbuf(gst, "ones", [128, 1], F32)
        trif = fw.sbuf(gst, "trif", [128, 4, 128], F32)
        upp = fw.sbuf(gst, "upp", [64, 512], BF16)
        Sst = [[fw.sbuf(gst, "S_%d_%d" % (d, h), [128, 256], F32) for h in range(4)] for d in range(2)]
        wE = fw.sbuf(gst, "wE", [128, NT, NE], F32)
        A1 = fw.sbuf(gst, "A1", [128, NT, NE], F32)
        A2 = fw.sbuf(gst, "A2", [128, NT, NE], F32)
        W01 = fw.sbuf(gst, "W01", [128, NT, 2], F32)
        onesm = fw.sbuf(gst, "onesm", [128, 128], F32)
        fw.dma(sp, onesm[:], ones_m[:, :], reads=[DB["ones_m"]], writes=[onesm])
        fw.dma(sp, idb[:], ident_b[:, :], reads=[DB["ident_b"]], writes=[idb])
        fw.dma(sp, idf[:], ident_f[:, :], reads=[DB["ident_f"]], writes=[idf])
        fw.dma(sp, ones[:], ones_c[:, :], reads=[DB["ones_c"]], writes=[ones])
        fw.dma(sp, trif[:], tri.ap().rearrange("m p n -> p m n"), reads=[DB["tri"]], writes=[trif])
        fw.dma(pool, upp[:], up_pad[:, :], reads=[DB["up_pad"]], writes=[upp])
        for d in range(2):
            for h in range(4):
                fw.v(lambda: V.memset(Sst[d][h][:], 0.0), writes=[Sst[d][h]])
        trib = fw.sbuf(gst, "trib", [128, 4, 128], BF16)
        onesb = fw.sbuf(gst, "onesb", [128, 1], BF16)
        fw.v(lambda: V.tensor_copy(out=trib[:], in_=trif[:]), reads=[trif], writes=[trib])
        fw.v(lambda: V.tensor_copy(out=onesb[:], in_=ones[:]), reads=[ones], writes=[onesb])

        def rms_rstd(src_ap, srcB, junk, ss, n):
            fw.a(lambda: A.activation(out=junk[:, 0:n], in_=src_ap, func=AF.Square, accum_out=ss[:, 0:1]),
                 reads=[srcB], writes=[junk, ss])
            fw.a(lambda: A.activation(out=ss[:, 0:1], in_=ss[:, 0:1], func=AF.Sqrt, scale=1.0 / n, bias=EPS),
                 reads=[ss], writes=[ss])
            fw.v(lambda: V.reciprocal(out=ss[:, 0:1], in_=ss[:, 0:1]), reads=[ss], writes=[ss])

        def norm1_tile(st_bufs, x_src_ap, xsrcB, dstT, dst_ap_fn):
            xt, xb, junk, ss, ln1, pT = st_bufs
            fw.dma(sp, xt[:], x_src_ap, reads=[xsrcB], writes=[xt])
            rms_rstd(xt[:], xt, junk, ss, D)
            fw.v(lambda: V.scalar_tensor_tensor(out=xb[:], in0=xt[:], scalar=ss[:, 0:1], in1=ln1[:], op0=ALU.mult, op1=ALU.mult),
                 reads=[xt, ss, ln1], writes=[xb])
            for g in range(4):
                p = pT[g % 2]
                for j in range(4):
                    kc = 4 * g + j
                    fw.tr(p, p[:, j, :], xb, xb[:, kc * 128:(kc + 1) * 128], idb)
                if g % 2 == 0:
                    fw.v(lambda: V.tensor_copy(out=dst_ap_fn(4 * g), in_=p[:]), reads=[p], writes=[dstT])
                else:
                    fw.a(lambda: A.copy(out=dst_ap_fn(4 * g), in_=p[:]), reads=[p], writes=[dstT])

        if npre > 0 and "pre" in phases:
            with ExitStack() as st:
                def two(name, shape, dt):
                    return [fw.sbuf(st, "%s_%d" % (name, i), shape, dt) for i in range(2)]
                xt = two("p1_xt", [128, D], F32); xb = two("p1_xb", [128, D], BF16)
                junk = fw.sbuf(st, "p1_junk", [128, D], F32); ss = two("p1_ss", [128, 4], F32)
                ln1 = fw.sbuf(st, "p1_ln1", [128, D], F32)
                xT = two("p1_xT", [128, 16, 128], BF16)
                wk = fw.sbuf(st, "p1_wk", [128, 16, 512], BF16)
                wv = fw.sbuf(st, "p1_wv", [128, 16, 1024], BF16)
                wl = fw.sbuf(st, "p1_wl", [128, 16, 64], BF16)
                psc = fw.sbuf(st, "p1_psc", [64, npre], F32); pbi = fw.sbuf(st, "p1_pbi", [64, npre], F32)
                pm = fw.sbuf(st, "p1_pm", [128, npre, 2], F32)
                gk = two("p1_gk", [128, 512], F32); gv = two("p1_gv", [128, 1024], BF16)
                lrT = two("p1_lrT", [64, 128], BF16)
                spf = two("p1_sp", [128, 512], BF16); spe = two("p1_spe", [128, 512], F32); e3 = two("p1_e3", [128, 512], F32)
                kend = two("p1_kend", [128, 512], BF16)
                tsel = two("p1_tsel", [128, 128], BF16); ttmp = two("p1_ttmp", [128, 128], F32)
                dec = two("p1_dec", [128, 4], F32); dd = two("p1_dd", [128, 2, 4], F32)
                om = two("p1_om", [128, 2], F32)
                dsm = two("p1_dsm", [128, 256], F32)
                pT = [fw.psum(st, "p1_pT%d" % i, [128, 4, 128], BF16) for i in range(2)]
                ps_k = fw.psum(st, "p1_psk", [128, 512], F32)
                ps_v = [fw.psum(st, "p1_psv%d" % i, [128, 512], F32) for i in range(2)]
                ps_l = fw.psum(st, "p1_psl", [64, 128], F32)
                ps_g = fw.psum(st, "p1_psg", [128, 512], F32)
                ps_d = [fw.psum(st, "p1_psd0", [128, 512], F32)] * 2
                pf = fw.sbuf(st, "p1_pf", [128, npre, 3], F32)
                Scur = [fw.sbuf(st, "p1_Scur%d" % h, [128, 256], F32) for h in range(4)]
                for h in range(4):
                    fw.v(lambda: V.memset(Scur[h][:], 0.0), writes=[Scur[h]])
                fw.dma(sp, pf[:], pre_f[:, :, :], reads=[DB["pre_f"]], writes=[pf])
                fw.dma(sp, ln1[:], ln1_bc[:, :], reads=[DB["ln1_bc"]], writes=[ln1])
                fw.dma(sp, psc[:], pre_sc[:, :], reads=[DB["pre_sc"]], writes=[psc])
                fw.dma(sp, pbi[:], pre_bi[:, :], reads=[DB["pre_bi"]], writes=[pbi])
                fw.dma(sp, pm[:], pre_m[:, :, :], reads=[DB["pre_m"]], writes=[pm])
                fw.dma(pool, wk[:], w_in[:, C_GK:C_GK + 512].rearrange("(kc p) n -> p kc n", p=128), reads=[DB["w_in"]], writes=[wk])
                fw.dma(pool, wv[:], w_in[:, C_GV:C_GV + 1024].rearrange("(kc p) n -> p kc n", p=128), reads=[DB["w_in"]], writes=[wv])
                fw.dma(pool, wl[:], w_lr.ap().rearrange("(kc p) n -> p kc n", p=128), reads=[DB["w_lr"]], writes=[wl])

                def front(s):
                    q = s % 2
                    xTs = xT[q]
                    xt_, xb_, ss_ = xt[q], xb[q], ss[q]
                    fw.dma(sp, xt_[:], xpre[s * 128:(s + 1) * 128, :], reads=[DB["xpre"]], writes=[xt_])
                    rms_rstd(xt_[:], xt_, junk, ss_, D)
                    fw.v(lambda: V.scalar_tensor_tensor(out=xb_[:], in0=xt_[:], scalar=ss_[:, 0:1], in1=ln1[:], op0=ALU.mult, op1=ALU.mult),
                         reads=[xt_, ss_, ln1], writes=[xb_])
                    yield
                    for g in range(4):
                        p = pT[g % 2]
                        for j in range(4):
                            kc = 4 * g + j
                            fw.tr(p, p[:, j, :], xb_, xb_[:, kc * 128:(kc + 1) * 128], idb)
                        if g % 2 == 0:
                            fw.v(lambda: V.tensor_copy(out=xTs[:, 4 * g:4 * g + 4, :], in_=p[:]), reads=[p], writes=[xTs])
                        else:
                            fw.a(lambda: A.copy(out=xTs[:, 4 * g:4 * g + 4, :], in_=p[:]), reads=[p], writes=[xTs])
                        yield
                    for kc in range(16):
                        fw.mm(ps_k, ps_k[:], xTs, xTs[:, kc, :], wk, wk[:, kc, :], start=(kc == 0), stop=(kc == 15))
                    fw.a(lambda: A.copy(out=gk[q][:], in_=ps_k[:]), reads=[ps_k], writes=[gk[q]])
                    yield
                    for half in range(2):
                        for kc in range(16):
                            fw.mm(ps_v[half], ps_v[half][:], xTs, xTs[:, kc, :], wv, wv[:, kc, half * 512:(half + 1) * 512],
                                  start=(kc == 0), stop=(kc == 15))
                        fw.v(lambda: V.tensor_copy(out=gv[q][:, half * 512:(half + 1) * 512], in_=ps_v[half][:]), reads=[ps_v[half]], writes=[gv[q]])
                        yield
                    for kc in range(16):
                        fw.mm(ps_l, ps_l[:], wl, wl[:, kc, :], xTs, xTs[:, kc, :], start=(kc == 0), stop=(kc == 15))
                    fw.v(lambda: V.tensor_scalar(out=lrT[q][:], in0=ps_l[:], scalar1=psc[:, s:s + 1], scalar2=pbi[:, s:s + 1],
                                                 op0=ALU.mult, op1=ALU.add), reads=[ps_l, psc, pbi], writes=[lrT[q]])
                    fw.v(lambda: V.tensor_scalar(out=ttmp[q][:], in0=trif[:, 3, :], scalar1=pm[:, s, 1:2], scalar2=None, op0=ALU.mult),
                         reads=[trif, pm], writes=[ttmp[q]])
                    fw.v(lambda: V.scalar_tensor_tensor(out=tsel[q][:], in0=trif[:, 2, :], scalar=pm[:, s, 0:1], in1=ttmp[q][:],
                                                        op0=ALU.mult, op1=ALU.add), reads=[trif, pm, ttmp[q]], writes=[tsel[q]])
                    yield

                def back(s):
                    q = s % 2
                    fw.mm(ps_g, ps_g[:], lrT[q], lrT[q][:], upp, upp[:])
                    yield
                    fw.a(lambda: A.activation(out=spe[q][:], in_=ps_g[:], func=AF.Exp, scale=-1.0), reads=[ps_g], writes=[spe[q]])
                    fw.a(lambda: A.activation(out=spf[q][:], in_=spe[q][:], func=AF.Ln, bias=1.0), reads=[spe[q]], writes=[spf[q]])
                    yield
                    fw.mm(ps_g, ps_g[:], tsel[q], tsel[q][:], spf[q], spf[q][:])
                    yield
                    fw.a(lambda: A.activation(out=e3[q][:], in_=ps_g[:], func=AF.Exp, scale=-1.0 / 16), reads=[ps_g], writes=[e3[q]])
                    fw.v(lambda: V.tensor_tensor(out=kend[q][:], in0=gk[q][:], in1=e3[q][:], op=ALU.mult), reads=[gk[q], e3[q]], writes=[kend[q]])
                    yield
                    for h in range(4):
                        fw.op(fw.pe, lambda: nc.tensor.matmul(ps_g[:, h:h + 1], lhsT=spf[q][:, h * 128:(h + 1) * 128], rhs=onesb[:, 0:1], start=True, stop=True),
                              reads=[spf[q], onesb], writes=[ps_g], inc=(h == 3))
                    yield
                    fw.a(lambda: A.activation(out=dec[q][:], in_=ps_g[:, 0:4], func=AF.Exp, scale=-1.0 / 16), reads=[ps_g], writes=[dec[q]])
                    fw.v(lambda: V.tensor_scalar(out=dd[q][:, 0, :], in0=dec[q][:], scalar1=pf[:, s, 0:1], scalar2=None, op0=ALU.mult),
                         reads=[dec[q], pf], writes=[dd[q]])
                    yield
                    for h in range(4):
                        pd = ps_d[0]
                        fw.mm(pd, pd[:, 0:256], kend[q], kend[q][:, h * 128:(h + 1) * 128], gv[q], gv[q][:, h * 256:(h + 1) * 256])
                        fw.v(lambda: V.scalar_tensor_tensor(out=Scur[h][:], in0=Scur[h][:], scalar=dd[q][:, 0, h:h + 1], in1=pd[:, 0:256],
                                                            op0=ALU.mult, op1=ALU.add), reads=[Scur[h], dd[q], pd], writes=[Scur[h]])
                        fw.v(lambda: V.scalar_tensor_tensor(out=Sst[0][h][:], in0=Scur[h][:], scalar=pf[:, s, 1:2], in1=Sst[0][h][:],
                                                            op0=ALU.mult, op1=ALU.add), reads=[Scur[h], pf, Sst[0][h]], writes=[Sst[0][h]])
                        if h % 2 == 1:
                            yield

                def zip_emit(*gens):
                    gens = [g for g in gens if g is not None]
                    while gens:
                        for g in list(gens):
                            try:
                                next(g)
                            except StopIteration:
                                gens.remove(g)

                zip_emit(front(0))
                for s in range(npre):
                    zip_emit(back(s), front(s + 1) if s + 1 < npre else None)
                    pump_conv(1)
                for h in range(4):
                    fw.v(lambda: V.tensor_scalar(out=Sst[1][h][:], in0=Scur[h][:], scalar1=pf[:, 0, 2:3], scalar2=None, op0=ALU.mult),
                         reads=[Scur[h], pf], writes=[Sst[1][h]])
            fw.barrier()

        with ExitStack() as st2:
            xnT = fw.sbuf(st2, "xnT", [128, 16, NTH * 128], BF16)
            for st in _scope("xn" in phases):
                xt = fw.sbuf(st, "p2_xt", [128, D], F32); xb = fw.sbuf(st, "p2_xb", [128, D], BF16)
                junk = fw.sbuf(st, "p2_junk", [128, D], F32); ss = fw.sbuf(st, "p2_ss", [128, 4], F32)
                ln1 = fw.sbuf(st, "p2_ln1", [128, D], F32)
                pT = [fw.psum(st, "p2_pT%d" % i, [128, 4, 128], BF16) for i in range(2)]
                fw.dma(sp, ln1[:], ln1_bc[:, :], reads=[DB["ln1_bc"]], writes=[ln1])
                for t in range(NTH):
                    norm1_tile((xt, xb, junk, ss, ln1, pT), xh[t * 128:(t + 1) * 128, :], DB["xh"], xnT,
                               lambda kc: xnT[:, kc:kc + 4, t * 128:(t + 1) * 128])
            fw.barrier()

            for st in _scope("attn" in phases):
                aqT = fw.sbuf(st, "aqT", [128, NT, 1024], BF16)
                akT = fw.sbuf(st, "akT", [128, 2, NTH * 128], BF16)
                av = fw.sbuf(st, "av", [128, NTH, 2, 129], BF16)
                wq = [fw.sbuf(st, "wq%d" % i, [128, 16, 512], BF16) for i in range(2)]
                qn = fw.sbuf(st, "qn", [128, 128], F32); kn = fw.sbuf(st, "kn", [128, 128], F32)
                cs = fw.sbuf(st, "cs", [128, 128], F32); sn = fw.sbuf(st, "sn", [128, 128], F32)
                qf = fw.sbuf(st, "qf", [128, 512], F32); qb = fw.sbuf(st, "qb", [128, 512], BF16)
                junk = fw.sbuf(st, "a_junk", [128, 1024], F32); ss = fw.sbuf(st, "a_ss", [128, 8], F32)
                r1 = fw.sbuf(st, "r1", [128, 4, 16], F32); r2 = fw.sbuf(st, "r2", [128, 4, 16], F32)
                r3 = fw.sbuf(st, "r3", [128, 4, 16], F32); r4 = fw.sbuf(st, "r4", [128, 4, 16], F32)
                mP = fw.sbuf(st, "mP", [128, 512], BF16); mN = fw.sbuf(st, "mN", [128, 512], BF16)
                mP0 = fw.sbuf(st, "mP0", [128, 512], BF16); mNL = fw.sbuf(st, "mNL", [128, 512], BF16)
                esk = fw.sbuf(st, "esk", [128, 8], F32)
                anw = fw.sbuf(st, "anw", [128, 1024], F32)
                pTt = [fw.sbuf(st, "pTt%d" % i, [128, 512], BF16) for i in range(3)]
                ao = fw.sbuf(st, "ao", [128, 1024], F32); aob = fw.sbuf(st, "aob", [128, 1024], BF16)
                den = fw.sbuf(st, "den", [128, 4], F32)
                pst1 = ExitStack()
                ps_q = [fw.psum(pst1, "ps_q%d" % i, [128, 512], F32) for i in range(2)]
                ps_t = [fw.psum(pst1, "ps_t%d" % i, [128, 4, 128], BF16) for i in range(2)]
                for (dst, src, nm) in ((qn, qn_bc, "qn_bc"), (kn, kn_bc, "kn_bc"), (esk, sink_bc, "sink_bc"), (anw, an_bc, "an_bc")):
                    fw.dma(sp, dst[:], src[:, :], reads=[DB[nm]], writes=[dst])
                for (dst, src, nm) in ((mP, maskP, "maskP"), (mN, maskN, "maskN"), (mP0, maskP0, "maskP0"), (mNL, maskNL, "maskNL")):
                    fw.dma(sp, dst[:], src[:, :], reads=[DB[nm]], writes=[dst])
                fw.a(lambda: A.activation(out=esk[:], in_=esk[:], func=AF.Exp), reads=[esk], writes=[esk])
                fw.v(lambda: V.memset(av[:], 1.0), writes=[av])

                def qk_post(ps, nh, nw, dstT, dst_fn, t):
                    for h in range(nh):
                        fw.a(lambda: A.activation(out=junk[:, 0:128], in_=ps[:, h * 128:(h + 1) * 128], func=AF.Square, accum_out=ss[:, h:h + 1]),
                             reads=[ps], writes=[junk, ss])
                    fw.a(lambda: A.activation(out=ss[:, 0:nh], in_=ss[:, 0:nh], func=AF.Sqrt, scale=1.0 / 128, bias=EPS), reads=[ss], writes=[ss])
                    fw.v(lambda: V.reciprocal(out=ss[:, 0:nh], in_=ss[:, 0:nh]), reads=[ss], writes=[ss])
                    for h in range(nh):
                        fw.v(lambda: V.scalar_tensor_tensor(out=qf[:, h * 128:(h + 1) * 128], in0=ps[:, h * 128:(h + 1) * 128], scalar=ss[:, h:h + 1],
                                                            in1=nw[:], op0=ALU.mult, op1=ALU.mult), reads=[ps, ss, nw], writes=[qf])
                    q3 = qf[:, 0:nh * 128].rearrange("p (h d) -> p h d", d=128)
                    c3 = cs[:, 0:nh * 16].rearrange("p (h d) -> p h d", d=16)
                    s3 = sn[:, 0:nh * 16].rearrange("p (h d) -> p h d", d=16)
                    x1, x2 = q3[:, :, 0:16], q3[:, :, 16:32]
                    fw.v(lambda: V.tensor_tensor(out=r1[:, 0:nh, :], in0=x1, in1=c3, op=ALU.mult), reads=[qf, cs], writes=[r1])
                    fw.v(lambda: V.tensor_tensor(out=r2[:, 0:nh, :], in0=x2, in1=s3, op=ALU.mult), reads=[qf, sn], writes=[r2])
                    fw.v(lambda: V.tensor_tensor(out=r3[:, 0:nh, :], in0=x2, in1=c3, op=ALU.mult), reads=[qf, cs], writes=[r3])
                    fw.v(lambda: V.tensor_tensor(out=r4[:, 0:nh, :], in0=x1, in1=s3, op=ALU.mult), reads=[qf, sn], writes=[r4])
                    fw.v(lambda: V.tensor_tensor(out=x1, in0=r1[:, 0:nh, :], in1=r2[:, 0:nh, :], op=ALU.subtract), reads=[r1, r2], writes=[qf])
                    fw.v(lambda: V.tensor_tensor(out=x2, in0=r3[:, 0:nh, :], in1=r4[:, 0:nh, :], op=ALU.add), reads=[r3, r4], writes=[qf])
                    fw.a(lambda: A.copy(out=qb[:, 0:nh * 128], in_=qf[:, 0:nh * 128]), reads=[qf], writes=[qb])
                    p = ps_t[t % 2]
                    for h in range(nh):
                        fw.tr(p, p[:, h, :], qb, qb[:, h * 128:(h + 1) * 128], idb)
                    fw.v(lambda: V.tensor_copy(out=dst_fn(None), in_=p[:, 0:nh, :]), reads=[p], writes=[dstT])

                groups = [(C_AQ, "q", 0), (C_AQ + 512, "q", 4), (C_AK, "kv", 0)]
                for gi, (c0, kind, h0) in enumerate(groups):
                    w = wq[gi % 2]
                    fw.dma(pool, w[:], w_in[:, c0:c0 + 512].rearrange("(kc p) n -> p kc n", p=128), reads=[DB["w_in"]], writes=[w])
                    tiles = range(1, NT + 1) if kind == "q" else range(NTH)
                    for t in tiles:
                        if t % 2 == 0:
                            pump_conv(1)
                        ps = ps_q[t % 2]
                        for kc in range(16):
                            fw.mm(ps, ps[:], xnT, xnT[:, kc, t * 128:(t + 1) * 128], w, w[:, kc, :], start=(kc == 0), stop=(kc == 15))
                        fw.dma(sp, cs[:], cosq[t * 128:(t + 1) * 128, :], reads=[DB["cosq"]], writes=[cs])
                        fw.dma(sp, sn[:], sinq[t * 128:(t + 1) * 128, :], reads=[DB["sinq"]], writes=[sn])
                        if kind == "q":
                            qk_post(ps, 4, qn, aqT, lambda h: aqT[:, t - 1, h0 * 128:(h0 + 4) * 128].rearrange("p (h d) -> p h d", h=4), t)
                        else:
                            qk_post(ps, 2, kn, akT, lambda h: akT[:, 0:2, t * 128:(t + 1) * 128], t)
                            for hk in range(2):
                                fw.a(lambda: A.copy(out=av[:, t, hk, 0:128], in_=ps[:, 256 + hk * 128:256 + (hk + 1) * 128]), reads=[ps], writes=[av])

                fw.barrier()
                pst1.close()
                ps_s = [fw.psum(st, "ps_s%d" % i, [128, 512], F32) for i in range(3)]
                ps_o = [fw.psum(st, "ps_o%d" % i, [128, 2, 129], F32) for i in range(2)]
                for i in range(NT):
                    pump_conv(2)
                    for hk in range(2):
                        for kb in range(3):
                            kt = i + kb
                            fw.mm(ps_s[kb], ps_s[kb][:], akT, akT[:, hk, kt * 128:(kt + 1) * 128],
                                  aqT, aqT[:, i, hk * 512:(hk + 1) * 512])
                            fw.a(lambda: A.activation(out=pTt[kb][:], in_=ps_s[kb][:], func=AF.Exp, scale=QS), reads=[ps_s[kb]], writes=[pTt[kb]])
                        mk = mP0 if i == 0 else mP
                        fw.v(lambda: V.tensor_tensor(out=pTt[0][:], in0=pTt[0][:], in1=mk[:], op=ALU.mult), reads=[pTt[0], mk], writes=[pTt[0]])
                        mk2 = mNL if i == NT - 1 else mN
                        fw.v(lambda: V.tensor_tensor(out=pTt[2][:], in0=pTt[2][:], in1=mk2[:], op=ALU.mult), reads=[pTt[2], mk2], writes=[pTt[2]])
                        for g in range(4):
                            po = ps_o[g // 2]
                            for kb in range(3):
                                kt = i + kb
                                fw.mm(po, po[:, g % 2, :], pTt[kb], pTt[kb][:, g * 128:(g + 1) * 128], av, av[:, kt, hk, :],
                                      start=(kb == 0), stop=(kb == 2))
                        for g in range(4):
                            po = ps_o[g // 2]
                            hq = 4 * hk + g
                            fw.v(lambda: V.tensor_scalar(out=den[:, g:g + 1], in0=po[:, g % 2, 128:129], scalar1=esk[:, hq:hq + 1], scalar2=None, op0=ALU.add),
                                 reads=[po, esk], writes=[den])
                        fw.v(lambda: V.reciprocal(out=den[:], in_=den[:]), reads=[den], writes=[den])
                        for g in range(4):
                            po = ps_o[g // 2]
                            hq = 4 * hk + g
                            fw.v(lambda: V.tensor_scalar(out=ao[:, hq * 128:(hq + 1) * 128], in0=po[:, g % 2, 0:128], scalar1=den[:, g:g + 1], scalar2=None, op0=ALU.mult),
                                 reads=[po, den], writes=[ao])
                    rms_rstd(ao[:], ao, junk, ss, 1024)
                    fw.v(lambda: V.scalar_tensor_tensor(out=aob[:], in0=ao[:], scalar=ss[:, 0:1], in1=anw[:], op0=ALU.mult, op1=ALU.mult),
                         reads=[ao, ss, anw], writes=[aob])
                    fw.dma(sp, mix_d[i * 128:(i + 1) * 128, 0:1024], aob[:], reads=[aob], writes=[mixB])
            fw.barrier()

            for st in _scope("gla" in phases):
                wa = [fw.sbuf(st, "g_w%d" % i, [128, 16, 256], BF16) for i in range(2)]
                wl = fw.sbuf(st, "g_wl", [128, 16, 64], BF16)
                mbi = fw.sbuf(st, "g_mbi", [64, 1], F32)
                lrT = fw.sbuf(st, "g_lrT", [64, TPC], BF16)
                gqT = fw.sbuf(st, "g_qT", [128, TPC], BF16); gkT = fw.sbuf(st, "g_kT", [128, TPC], BF16)
                gkt = fw.sbuf(st, "g_kt", [128, NT, 128], F32)
                gvt = fw.sbuf(st, "g_vt", [128, NT, 256], BF16)
                srt = fw.sbuf(st, "g_sr", [128, NT, 256], BF16)
                oacc2 = [fw.sbuf(st, "g_oacc%d" % i, [128, NT, 256], F32) for i in range(2)]
                gnw = fw.sbuf(st, "g_nw", [128, 256], F32)
                def two(name, shape, dt):
                    return [fw.sbuf(st, "%s_%d" % (name, i), shape, dt) for i in range(2)]
                spf2 = two("g_sp", [128, 128], BF16); spe2 = two("g_spe", [128, 128], F32)
                e1s = two("g_e1", [128, 128], F32); e2s = two("g_e2", [128, 128], F32); e3s = two("g_e3", [128, 128], F32)
                qds = two("g_qd", [128, 128], BF16); kis = two("g_ki", [128, 128], BF16); kes = two("g_ke", [128, 128], BF16)
                ams = two("g_am", [128, 128], BF16)
                decs = two("g_dec", [128, 1], F32)
                Sbs = two("g_Sb", [128, 256], BF16)
                junk = fw.sbuf(st, "g_junk", [128, 256], F32); ss = fw.sbuf(st, "g_ss", [128, 4], F32)
                ob = fw.sbuf(st, "g_ob", [128, 256], BF16); of = fw.sbuf(st, "g_of", [128, 256], F32)
                PS = [fw.psum(st, "g_ps%d" % i, [128, 512], F32) for i in range(8)]
                ps_p = [PS[0], PS[4]]
                fw.dma(sp, gnw[:], gn_bc[:, :], reads=[DB["gn_bc"]], writes=[gnw])
                fw.dma(sp, mbi[:], main_bi[:, :], reads=[DB["main_bi"]], writes=[mbi])
                fw.dma(pool, wl[:], w_lr.ap().rearrange("(kc p) n -> p kc n", p=128), reads=[DB["w_lr"]], writes=[wl])
                XO = 128
                for nt in range(TPC // 512):
                    pl = ps_p[nt % 2]
                    for kc in range(16):
                        fw.mm(pl, pl[0:64, :], wl, wl[:, kc, :], xnT, xnT[:, kc, XO + nt * 512:XO + (nt + 1) * 512], start=(kc == 0), stop=(kc == 15))
                    fw.v(lambda: V.tensor_scalar(out=lrT[:, nt * 512:(nt + 1) * 512], in0=pl[0:64, :], scalar1=mbi[:, 0:1], scalar2=None, op0=ALU.add),
                         reads=[pl, mbi], writes=[lrT])
                wi = 0
                for h in range(4):
                    w = wa[wi % 2]; wi += 1
                    fw.dma(pool, w[:, :, 0:128], w_in[:, C_GQ + h * 128:C_GQ + (h + 1) * 128].rearrange("(kc p) n -> p kc n", p=128), reads=[DB["w_in"]], writes=[w])
                    fw.dma(pool, w[:, :, 128:256], w_in[:, C_GK + h * 128:C_GK + (h + 1) * 128].rearrange("(kc p) n -> p kc n", p=128), reads=[DB["w_in"]], writes=[w])
                    for nt in range(TPC // 512):
                        for (j, dst) in ((0, gqT), (1, gkT)):
                            pl = ps_p[j]
                            for kc in range(16):
                                fw.mm(pl, pl[:], w, w[:, kc, j * 128:(j + 1) * 128], xnT, xnT[:, kc, XO + nt * 512:XO + (nt + 1) * 512], start=(kc == 0), stop=(kc == 15))
                            fw.a(lambda: A.copy(out=dst[:, nt * 512:(nt + 1) * 512], in_=pl[:]), reads=[pl], writes=[dst])
                    for t in range(NT):
                        pl = ps_p[t % 2]
                        for kc in range(16):
                            fw.mm(pl, pl[:, 0:128], xnT, xnT[:, kc, XO + t * 128:XO + (t + 1) * 128], w, w[:, kc, 128:256], start=(kc == 0), stop=(kc == 15))
                        fw.v(lambda: V.tensor_copy(out=gkt[:, t, :], in_=pl[:, 0:128]), reads=[pl], writes=[gkt])
                    for (c0, dst, isr) in ((C_GV + h * 256, gvt, False), (C_GR + h * 256, srt, True)):
                        w2 = wa[wi % 2]; wi += 1
                        fw.dma(pool, w2[:], w_in[:, c0:c0 + 256].rearrange("(kc p) n -> p kc n", p=128), reads=[DB["w_in"]], writes=[w2])
                        for t in range(NT):
                            pl = ps_p[t % 2]
                            for kc in range(16):
                                fw.mm(pl, pl[:, 0:256], xnT, xnT[:, kc, XO + t * 128:XO + (t + 1) * 128], w2, w2[:, kc, :], start=(kc == 0), stop=(kc == 15))
                            if isr:
                                fw.a(lambda: A.activation(out=dst[:, t, :], in_=pl[:, 0:256], func=AF.Silu), reads=[pl], writes=[dst])
                            else:
                                fw.v(lambda: V.tensor_copy(out=dst[:, t, :], in_=pl[:, 0:256]), reads=[pl], writes=[dst])
                    def chain(d):
                        S = Sst[d][h]
                        order = range(NT) if d == 0 else range(NT - 1, -1, -1)
                        last = 127 if d == 0 else 0
                        pA, pR, pO, pD = PS[4 * d], PS[4 * d + 1], PS[4 * d + 2], PS[4 * d + 3]
                        spe_, spf_, e1_, e2_, e3_ = spe2[d], spf2[d], e1s[d], e2s[d], e3s[d]
                        qd_, ki_, ke_, am_, dec_, Sb_, oa_ = qds[d], kis[d], kes[d], ams[d], decs[d], Sbs[d], oacc2[d]
                        for n in order:
                            if d == 0 and n % 4 == 0:
                                pump_conv(1)
                            tk = slice(n * 128, (n + 1) * 128)
                            fw.a(lambda: A.copy(out=Sb_[:], in_=S[:]), reads=[S], writes=[Sb_])
                            fw.mm(pA, pA[:, 0:128], lrT, lrT[32 * d:32 * d + 32, tk], upp, upp[32 * d:32 * d + 32, h * 128:(h + 1) * 128])
                            yield
                            fw.a(lambda: A.activation(out=spe_[:], in_=pA[:, 0:128], func=AF.Exp, scale=-1.0), reads=[pA], writes=[spe_])
                            fw.a(lambda: A.activation(out=spf_[:], in_=spe_[:], func=AF.Ln, bias=1.0), reads=[spe_], writes=[spf_])
                            yield
                            fw.mm(pA, pA[:, 0:128], spf_, spf_[:], trib, trib[:, d, :])
                            fw.mm(pR, pR[:, 0:128], trib, trib[:, 2 + d, :], spf_, spf_[:])
                            yield
                            fw.a(lambda: A.activation(out=e1_[:], in_=pA[:, 0:128], func=AF.Exp, scale=-1.0 / 16), reads=[pA], writes=[e1_])
                            fw.a(lambda: A.activation(out=e2_[:], in_=pA[:, 0:128], func=AF.Exp, scale=1.0 / 16), reads=[pA], writes=[e2_])
                            fw.a(lambda: A.activation(out=e3_[:], in_=pR[:, 0:128], func=AF.Exp, scale=-1.0 / 16), reads=[pR], writes=[e3_])
                            fw.a(lambda: A.copy(out=dec_[:], in_=e1_[:, last:last + 1]), reads=[e1_], writes=[dec_])
                            yield
                            fw.v(lambda: V.scalar_tensor_tensor(out=qd_[:], in0=gqT[:, tk], scalar=QS, in1=e1_[:], op0=ALU.mult, op1=ALU.mult),
                                 reads=[gqT, e1_], writes=[qd_])
                            fw.v(lambda: V.tensor_tensor(out=ki_[:], in0=gkT[:, tk], in1=e2_[:], op=ALU.mult), reads=[gkT, e2_], writes=[ki_])
                            fw.v(lambda: V.tensor_tensor(out=ke_[:], in0=gkt[:, n, :], in1=e3_[:], op=ALU.mult), reads=[gkt, e3_], writes=[ke_])
                            yield
                            fw.mm(pA, pA[:, 0:128], ki_, ki_[:], qd_, qd_[:])
                            yield
                            fw.v(lambda: V.tensor_tensor(out=am_[:], in0=pA[:, 0:128], in1=trif[:, d, :], op=ALU.mult), reads=[pA, trif], writes=[am_])
                            yield
                            fw.mm(pO, pO[:, 0:256], am_, am_[:], gvt, gvt[:, n, :], start=True, stop=False)
                            fw.mm(pO, pO[:, 0:256], qd_, qd_[:], Sb_, Sb_[:], start=False, stop=True)
                            fw.mm(pD, pD[:, 0:256], ke_, ke_[:], gvt, gvt[:, n, :])
                            yield
                            fw.a(lambda: A.copy(out=oa_[:, n, :], in_=pO[:, 0:256]), reads=[pO], writes=[oa_])
                            fw.v(lambda: V.scalar_tensor_tensor(out=S[:], in0=S[:], scalar=dec_[:, 0:1], in1=pD[:, 0:256], op0=ALU.mult, op1=ALU.add),
                                 reads=[S, dec_, pD], writes=[S])
                            yield

                    gens = [chain(0), chain(1)]
                    while gens:
                        for g_ in list(gens):
                            try:
                                next(g_)
                            except StopIteration:
                                gens.remove(g_)
                    for n in range(NT):
                        fw.v(lambda: V.tensor_tensor(out=of[:], in0=oacc2[0][:, n, :], in1=oacc2[1][:, n, :], op=ALU.add), reads=[oacc2[0], oacc2[1]], writes=[of])
                        rms_rstd(of[:], of, junk, ss, 256)
                        fw.v(lambda: V.scalar_tensor_tensor(out=of[:], in0=of[:], scalar=ss[:, 0:1], in1=gnw[:], op0=ALU.mult, op1=ALU.mult),
                             reads=[of, ss, gnw], writes=[of])
                        fw.v(lambda: V.tensor_tensor(out=ob[:], in0=of[:], in1=srt[:, n, :], op=ALU.mult), reads=[of, srt], writes=[ob])
                        fw.dma(sp, mix_d[n * 128:(n + 1) * 128, 1024 + h * 256:1024 + (h + 1) * 256], ob[:], reads=[ob], writes=[mixB])
            fw.barrier()

        for st in _scope("out" in phases):
            wo = fw.sbuf(st, "wo", [128, 16, D], BF16)
            wr = fw.sbuf(st, "wr", [128, 16, 36], F32)
            brt = fw.sbuf(st, "brt", [128, 36], F32)
            ln2 = fw.sbuf(st, "ln2", [128, D], F32)
            def two(name, shape, dt):
                return [fw.sbuf(st, "%s_%d" % (name, i), shape, dt) for i in range(2)]
            junk = fw.sbuf(st, "o_junk", [128, D], F32)
            PAR = dict(mt=two("mt", [128, D], BF16), mT=two("mT", [128, 16, 128], BF16), xt=two("o_xt", [128, D], F32),
                       ht=two("o_ht", [128, D], F32), ss=two("o_ss", [128, 4], F32), xn2=two("o_xn2", [128, D], F32),
                       xTf=two("o_xTf", [128, 16, 128], F32), xTb=two("o_xTb", [128, 16, 128], BF16), lg=two("lg", [128, 36], F32),
                       gm=two("gm", [128, 8], F32), ohg=two("ohg", [128, 4], F32), ge=two("ge", [128, 4], F32),
                       ig=two("ig", [128, 8], F32), tmp8=two("tmp8", [128, 8], F32), oh1=two("oh1", [128, 8], F32),
                       oh2=two("oh2", [128, 8], F32), w8=two("w8", [128, 8], F32))
            ps_t = [fw.psum(st, "o_pst%d" % i, [128, 4, 128], BF16) for i in range(2)]
            ps_h = [fw.psum(st, "o_psh%d" % i, [128, 512], F32) for i in range(4)]
            ps_f = [fw.psum(st, "o_psf%d" % i, [128, 4, 128], F32) for i in range(1)]
            ps_l = fw.psum(st, "o_psl", [128, 36], F32)
            for half in range(2):
                fw.dma(pool, wo[:, :, half * 1024:(half + 1) * 1024], w_out[:, half * 1024:(half + 1) * 1024].rearrange("(kc p) n -> p kc n", p=128),
                       reads=[DB["w_out"]], writes=[wo])
            fw.dma(sp, wr[:], w_rt.ap().rearrange("(kc p) n -> p kc n", p=128), reads=[DB["w_rt"]], writes=[wr])
            fw.dma(sp, brt[:], b_rt[:, :], reads=[DB["b_rt"]], writes=[brt])
            fw.dma(sp, ln2[:], ln2_bc[:, :], reads=[DB["ln2_bc"]], writes=[ln2])
            def tile_gen(t):
                mt, mT, xt, ht, ss, xn2, xTf, xTb, lg, gm, ohg, ge, ig, tmp8, oh1, oh2, w8 = [PAR[k][t % 2] for k in (
                    "mt", "mT", "xt", "ht", "ss", "xn2", "xTf", "xTb", "lg", "gm", "ohg", "ge", "ig", "tmp8", "oh1", "oh2", "w8")]
                rows = slice(t * 128, (t + 1) * 128)
                pump_conv(1)
                fw.dma(sp, mt[:], mix_d[rows, :], reads=[mixB], writes=[mt])
                fw.dma(sp, xt[:], xh[128 + t * 128:128 + (t + 1) * 128, :], reads=[DB["xh"]], writes=[xt])
                for g in range(4):
                    p = ps_t[g % 2]
                    for j in range(4):
                        kc = 4 * g + j
                        fw.tr(p, p[:, j, :], mt, mt[:, kc * 128:(kc + 1) * 128], idb)
                    if g % 2 == 0:
                        fw.v(lambda: V.tensor_copy(out=mT[:, 4 * g:4 * g + 4, :], in_=p[:]), reads=[p], writes=[mT])
                    else:
                        fw.a(lambda: A.copy(out=mT[:, 4 * g:4 * g + 4, :], in_=p[:]), reads=[p], writes=[mT])
                    if g % 2 == 1:
                        yield
                for dc in range(4):
                    for kc in range(16):
                        fw.mm(ps_h[dc], ps_h[dc][:], mT, mT[:, kc, :], wo, wo[:, kc, dc * 512:(dc + 1) * 512], start=(kc == 0), stop=(kc == 15))
                    fw.v(lambda: V.tensor_tensor(out=ht[:, dc * 512:(dc + 1) * 512], in0=ps_h[dc][:], in1=xt[:, dc * 512:(dc + 1) * 512], op=ALU.add),
                         reads=[ps_h[dc], xt], writes=[ht])
                    yield
                fw.dma(sp, h_d[rows, :], ht[:], reads=[ht], writes=[hB])
                rms_rstd(ht[:], ht, junk, ss, D)
                fw.v(lambda: V.scalar_tensor_tensor(out=xn2[:], in0=ht[:], scalar=ss[:, 0:1], in1=ln2[:], op0=ALU.mult, op1=ALU.mult),
                     reads=[ht, ss, ln2], writes=[xn2])
                yield
                for g in range(4):
                    p = ps_f[0]
                    for j in range(4):
                        kc = 4 * g + j
                        fw.mm(p, p[:, j, :], xn2, xn2[:, kc * 128:(kc + 1) * 128], idf, idf[:])
                    fw.a(lambda: A.copy(out=xTf[:, 4 * g:4 * g + 4, :], in_=p[:]), reads=[p], writes=[xTf])
                    if not sparse:
                        fw.v(lambda: V.tensor_copy(out=xTb[:, 4 * g:4 * g + 4, :], in_=p[:]), reads=[p], writes=[xTb])
                    yield
                if sparse:
                    fw.a(lambda: A.copy(out=mt[:], in_=xn2[:]), reads=[xn2], writes=[mt])
                    fw.dma(sp, xn2_d[rows, :], mt[:], reads=[mt], writes=[xn2B])
                else:
                    fw.dma(sp, xs_d[t, :, :, :], xTb[:], reads=[xTb], writes=[xsB])
                for kc in range(16):
                    fw.mm(ps_l, ps_l[:], xTf, xTf[:, kc, :], wr, wr[:, kc, :], start=(kc == 0), stop=(kc == 15))
                fw.v(lambda: V.tensor_tensor(out=lg[:], in0=ps_l[:], in1=brt[:], op=ALU.add), reads=[ps_l, brt], writes=[lg])
                yield
                fw.v(lambda: V.tensor_reduce(out=gm[:, 0:1], in_=lg[:, 0:4], axis=AX.X, op=ALU.max), reads=[lg], writes=[gm])
                fw.v(lambda: V.tensor_scalar(out=ohg[:], in0=lg[:, 0:4], scalar1=gm[:, 0:1], scalar2=None, op0=ALU.is_equal), reads=[lg, gm], writes=[ohg])
                fw.v(lambda: V.tensor_scalar(out=ge[:], in0=lg[:, 0:4], scalar1=gm[:, 0:1], scalar2=None, op0=ALU.subtract), reads=[lg, gm], writes=[ge])
                fw.a(lambda: A.activation(out=ge[:], in_=ge[:], func=AF.Exp, accum_out=gm[:, 1:2]), reads=[ge], writes=[ge, gm])
                fw.v(lambda: V.reciprocal(out=gm[:, 2:3], in_=gm[:, 1:2]), reads=[gm], writes=[gm])
                yield
                fw.v(lambda: V.tensor_scalar(out=ig[:], in0=lg[:, 4:12], scalar1=ohg[:, 0:1], scalar2=None, op0=ALU.mult), reads=[lg, ohg], writes=[ig])
                for g in range(1, 4):
                    fw.v(lambda: V.scalar_tensor_tensor(out=ig[:], in0=lg[:, 4 + 8 * g:12 + 8 * g], scalar=ohg[:, g:g + 1], in1=ig[:], op0=ALU.mult, op1=ALU.add),
                         reads=[lg, ohg, ig], writes=[ig])
                yield
                fw.v(lambda: V.tensor_reduce(out=gm[:, 3:4], in_=ig[:], axis=AX.X, op=ALU.max), reads=[ig], writes=[gm])
                fw.v(lambda: V.tensor_scalar(out=oh1[:], in0=ig[:], scalar1=gm[:, 3:4], scalar2=None, op0=ALU.is_equal), reads=[ig, gm], writes=[oh1])
                fw.v(lambda: V.scalar_tensor_tensor(out=tmp8[:], in0=oh1[:], scalar=-1e30, in1=ig[:], op0=ALU.mult, op1=ALU.add), reads=[oh1, ig], writes=[tmp8])
                fw.v(lambda: V.tensor_reduce(out=gm[:, 4:5], in_=tmp8[:], axis=AX.X, op=ALU.max), reads=[tmp8], writes=[gm])
                fw.v(lambda: V.tensor_scalar(out=oh2[:], in0=tmp8[:], scalar1=gm[:, 4:5], scalar2=None, op0=ALU.is_equal), reads=[tmp8, gm], writes=[oh2])
                yield
                fw.v(lambda: V.tensor_tensor(out=gm[:, 5:6], in0=gm[:, 4:5], in1=gm[:, 3:4], op=ALU.subtract), reads=[gm], writes=[gm])
                fw.a(lambda: A.activation(out=gm[:, 5:6], in_=gm[:, 5:6], func=AF.Exp), reads=[gm], writes=[gm])
                fw.v(lambda: V.tensor_scalar(out=gm[:, 6:7], in0=gm[:, 5:6], scalar1=1.0, scalar2=None, op0=ALU.add), reads=[gm], writes=[gm])
                fw.v(lambda: V.reciprocal(out=gm[:, 6:7], in_=gm[:, 6:7]), reads=[gm], writes=[gm])
                fw.v(lambda: V.tensor_tensor(out=gm[:, 6:7], in0=gm[:, 6:7], in1=gm[:, 2:3], op=ALU.mult), reads=[gm], writes=[gm])
                fw.v(lambda: V.tensor_tensor(out=gm[:, 7:8], in0=gm[:, 6:7], in1=gm[:, 5:6], op=ALU.mult), reads=[gm], writes=[gm])
                fw.v(lambda: V.tensor_scalar(out=w8[:], in0=oh1[:], scalar1=gm[:, 6:7], scalar2=None, op0=ALU.mult), reads=[oh1, gm], writes=[w8])
                fw.v(lambda: V.scalar_tensor_tensor(out=w8[:], in0=oh2[:], scalar=gm[:, 7:8], in1=w8[:], op0=ALU.mult, op1=ALU.add), reads=[oh2, gm, w8], writes=[w8])
                for g in range(4):
                    fw.v(lambda: V.tensor_scalar(out=wE[:, t, g * 8:(g + 1) * 8], in0=w8[:], scalar1=ohg[:, g:g + 1], scalar2=None, op0=ALU.mult),
                         reads=[w8, ohg], writes=[wE])
                    fw.v(lambda: V.tensor_scalar(out=A1[:, t, g * 8:(g + 1) * 8], in0=oh1[:], scalar1=ohg[:, g:g + 1], scalar2=None, op0=ALU.mult),
                         reads=[oh1, ohg], writes=[A1])
                    fw.v(lambda: V.tensor_scalar(out=A2[:, t, g * 8:(g + 1) * 8], in0=oh2[:], scalar1=ohg[:, g:g + 1], scalar2=None, op0=ALU.mult),
                         reads=[oh2, ohg], writes=[A2])
                fw.v(lambda: V.tensor_copy(out=W01[:, t, :], in_=gm[:, 6:8]), reads=[gm], writes=[W01])
                yield

            gens, t_next, rounds = [], 0, 0
            while gens or t_next < NT:
                if t_next < NT and len(gens) < 2 and (not gens or rounds % 8 == 0):
                    gens.append(tile_gen(t_next)); t_next += 1
                for g_ in list(gens):
                    try:
                        next(g_)
                    except StopIteration:
                        gens.remove(g_)
                rounds += 1
        fw.barrier()


        def dmaf(eng, fn, reads, writes):
            fw._deps(eng, reads, writes)
            owner = writes[0]
            if owner.dsem is None:
                owner.dsem = fw.new_sem("d_" + owner.name)
                fw.dbufs.append(owner)
            inst = fn()
            owner.dcount += 16
            inst.then_inc(owner.dsem, 16)
            tok = (owner.dsem, owner.dcount)
            for b in reads:
                b.reads.append(tok)
            for b in writes:
                b.last_w = tok
                b.reads = []
            fw.n_inst += 1

        pump_conv(len(conv_jobs))
        for st4 in _scope("moe" in phases and ne > 0 and sparse):
            s0i = fw.sbuf(st4, "s0i", [128, NT], I32); s1i = fw.sbuf(st4, "s1i", [128, NT], I32)
            ebi = fw.sbuf(st4, "ebi", [128, 2, NBLK], I32)
            iop = fw.sbuf(st4, "iop", [128, 1], F32)
            fw.dma(sp, iop[:], iota_p[:, :], reads=[DB["iota_p"]], writes=[iop])
            for st in _scope(True):
                Aa = fw.sbuf(st, "Aa", [128, NT, NE], F32)
                Acum = fw.sbuf(st, "Acum", [128, NE], F32)
                Pr = fw.sbuf(st, "Pr", [128, NT, NE], F32)
                cnt = fw.sbuf(st, "cnt", [128, NE], F32); pad = fw.sbuf(st, "pad", [128, NE], F32)
                sa = fw.sbuf(st, "sa", [128, NE], F32); sb = fw.sbuf(st, "sb", [128, NE], F32)
                pst = fw.sbuf(st, "pst", [128, NE], F32); pen = fw.sbuf(st, "pen", [128, NE], F32)
                tmpA = fw.sbuf(st, "tmpA", [128, NT, NE], F32)
                s0f = fw.sbuf(st, "s0f", [128, NT], F32); s1f = fw.sbuf(st, "s1f", [128, NT], F32)
                ebf = fw.sbuf(st, "ebf", [128, NBLK], F32); cmpb = fw.sbuf(st, "cmpb", [128, NE], F32)
                ps_r = fw.psum(st, "r_ps", [128, NE], F32)
                fw.v(lambda: V.tensor_tensor(out=Aa[:], in0=A1[:], in1=A2[:], op=ALU.add), reads=[A1, A2], writes=[Aa])
                fw.v(lambda: V.memset(Acum[:], 0.0), writes=[Acum])
                for t in range(NT):
                    fw.mm(ps_r, ps_r[:], trif, trif[:, 3, :], Aa, Aa[:, t, :], start=True, stop=False)
                    fw.mm(ps_r, ps_r[:], onesm, onesm[:], Acum, Acum[:], start=False, stop=True)
                    fw.a(lambda: A.copy(out=Pr[:, t, :], in_=ps_r[:]), reads=[ps_r], writes=[Pr])
                    fw.v(lambda: V.tensor_tensor(out=Acum[:], in0=Acum[:], in1=Aa[:, t, :], op=ALU.add), reads=[Acum, Aa], writes=[Acum])
                fw.mm(ps_r, ps_r[:], onesm, onesm[:], Acum, Acum[:])
                fw.a(lambda: A.copy(out=cnt[:], in_=ps_r[:]), reads=[ps_r], writes=[cnt])
                fw.v(lambda: V.tensor_scalar(out=sa[:], in0=cnt[:], scalar1=1.0 / 128, scalar2=0.49609375, op0=ALU.mult, op1=ALU.add), reads=[cnt], writes=[sa])
                fw.v(lambda: V.tensor_scalar(out=sb[:], in0=sa[:], scalar1=8388608.0, scalar2=None, op0=ALU.add), reads=[sa], writes=[sb])
                fw.v(lambda: V.tensor_scalar(out=sa[:], in0=sb[:], scalar1=-8388608.0, scalar2=None, op0=ALU.add), reads=[sb], writes=[sa])
                fw.v(lambda: V.tensor_scalar(out=pad[:], in0=sa[:], scalar1=128.0, scalar2=None, op0=ALU.mult), reads=[sa], writes=[pad])
                fw.v(lambda: V.tensor_copy(out=sa[:], in_=pad[:]), reads=[pad], writes=[sa])
                cur, nxt = sa, sb
                for sh in (1, 2, 4, 8, 16):
                    fw.v(lambda: V.tensor_copy(out=nxt[:, 0:sh], in_=cur[:, 0:sh]), reads=[cur], writes=[nxt])
                    fw.v(lambda: V.tensor_tensor(out=nxt[:, sh:NE], in0=cur[:, sh:NE], in1=cur[:, 0:NE - sh], op=ALU.add), reads=[cur], writes=[nxt])
                    cur, nxt = nxt, cur
                fw.v(lambda: V.tensor_copy(out=pen[:], in_=cur[:]), reads=[cur], writes=[pen])
                fw.v(lambda: V.tensor_tensor(out=pst[:], in0=pen[:], in1=pad[:], op=ALU.subtract), reads=[pen, pad], writes=[pst])
                for t in range(NT):
                    fw.v(lambda: V.tensor_tensor(out=Pr[:, t, :], in0=Pr[:, t, :], in1=pst[:], op=ALU.add), reads=[Pr, pst], writes=[Pr])
                for (Ak, sf, si) in ((A1, s0f, s0i), (A2, s1f, s1i)):
                    fw.v(lambda: V.tensor_tensor(out=tmpA[:], in0=Ak[:], in1=Pr[:], op=ALU.mult), reads=[Ak, Pr], writes=[tmpA])
                    fw.v(lambda: V.tensor_reduce(out=sf[:], in_=tmpA[:], axis=AX.X, op=ALU.add), reads=[tmpA], writes=[sf])
                    fw.v(lambda: V.tensor_copy(out=si[:], in_=sf[:]), reads=[sf], writes=[si])
                for b in range(NBLK):
                    fw.v(lambda: V.tensor_scalar(out=cmpb[:], in0=pen[:], scalar1=float(128 * b), scalar2=None, op0=ALU.is_le), reads=[pen], writes=[cmpb])
                    fw.v(lambda: V.tensor_reduce(out=ebf[:, b:b + 1], in_=cmpb[:], axis=AX.X, op=ALU.add), reads=[cmpb], writes=[ebf])
                fw.v(lambda: V.tensor_scalar(out=ebf[:], in0=ebf[:], scalar1=float(ne - 1), scalar2=None, op0=ALU.min), reads=[ebf], writes=[ebf])
                fw.v(lambda: V.tensor_scalar(out=ebf[:], in0=ebf[:], scalar1=128.0, scalar2=iop[:, 0:1], op0=ALU.mult, op1=ALU.add), reads=[ebf, iop], writes=[ebf])
                fw.v(lambda: V.tensor_copy(out=ebi[:, 0, :], in_=ebf[:]), reads=[ebf], writes=[ebi])
                fw.v(lambda: V.tensor_scalar(out=ebf[:], in0=ebf[:], scalar1=float(ne * 128), scalar2=None, op0=ALU.add), reads=[ebf], writes=[ebf])
                fw.v(lambda: V.tensor_copy(out=ebi[:, 1, :], in_=ebf[:]), reads=[ebf], writes=[ebi])
            fw.barrier()
            for st in _scope(True):
                xrow = [fw.sbuf(st, "xrow%d" % i, [128, D], BF16) for i in range(2)]
                for t in range(NT):
                    xr = xrow[t % 2]
                    fw.dma(sp, xr[:], xn2_d[t * 128:(t + 1) * 128, :], reads=[xn2B], writes=[xr])
                    for si in (s0i, s1i):
                        dmaf(pool, lambda: nc.gpsimd.indirect_dma_start(out=xsort_d[:, :], out_offset=bass.IndirectOffsetOnAxis(ap=si[:, t:t + 1], axis=0),
                                                                        in_=xr[:], in_offset=None), reads=[xr, si], writes=[xsortB])
            fw.barrier()
            for st in _scope(True):
                xblk = [fw.sbuf(st, "xblk%d" % i, [128, D], BF16) for i in range(2)]
                xsT = [fw.sbuf(st, "xsT%d" % i, [128, 16, 128], BF16) for i in range(2)]
                wgb = [fw.sbuf(st, "wgb%d" % i, [128, 16, 512], BF16) for i in range(2)]
                wub = [fw.sbuf(st, "wub%d" % i, [128, 16, 512], BF16) for i in range(2)]
                wdb = [fw.sbuf(st, "wdb%d" % i, [128, 4, D], BF16) for i in range(2)]
                hidT = [fw.sbuf(st, "hidT%d" % i, [128, 512], BF16) for i in range(2)]
                outb = [fw.sbuf(st, "outb%d" % i, [128, D], F32) for i in range(2)]
                ps_t = [fw.psum(st, "m_pst%d" % i, [128, 4, 128], BF16) for i in range(2)]
                hidm = [fw.sbuf(st, "hidm%d" % i, [128, 512], BF16) for i in range(2)]
                pg = fw.psum(st, "m_pg", [128, 512], F32); pu = fw.psum(st, "m_pu", [128, 512], F32)
                po = [fw.psum(st, "m_po%d" % i, [128, 512], F32) for i in range(4)]
                it = 0
                for b in range(NBLK):
                    xb_, xT_ = xblk[b % 2], xsT[b % 2]
                    fw.dma(sp, xb_[:], xsort_d[b * 128:(b + 1) * 128, :], reads=[xsortB], writes=[xb_])
                    for g in range(4):
                        p = ps_t[g % 2]
                        for j in range(4):
                            kc = 4 * g + j
                            fw.tr(p, p[:, j, :], xb_, xb_[:, kc * 128:(kc + 1) * 128], idb)
                        if g % 2 == 0:
                            fw.v(lambda: V.tensor_copy(out=xT_[:, 4 * g:4 * g + 4, :], in_=p[:]), reads=[p], writes=[xT_])
                        else:
                            fw.a(lambda: A.copy(out=xT_[:, 4 * g:4 * g + 4, :], in_=p[:]), reads=[p], writes=[xT_])
                    for half in range(2):
                        s_ = it % 2; it += 1
                        for (tab, tB, dstb) in ((wgc, wgcB, wgb[s_]), (wuc, wucB, wub[s_]), (wdc, wdcB, wdb[s_])):
                            dmaf(pool, lambda: nc.gpsimd.indirect_dma_start(out=dstb[:].rearrange("p k n -> p (k n)"), out_offset=None, in_=tab[:, :],
                                                                            in_offset=bass.IndirectOffsetOnAxis(ap=ebi[:, half, b:b + 1], axis=0)),
                                 reads=[tB, ebi], writes=[dstb])
                        for kc in range(16):
                            fw.mm(pg, pg[:], xT_, xT_[:, kc, :], wgb[s_], wgb[s_][:, kc, :], start=(kc == 0), stop=(kc == 15))
                        for kc in range(16):
                            fw.mm(pu, pu[:], xT_, xT_[:, kc, :], wub[s_], wub[s_][:, kc, :], start=(kc == 0), stop=(kc == 15))
                        hm = hidm[s_]
                        fw.a(lambda: A.activation(out=hm[:], in_=pg[:], func=AF.Silu), reads=[pg], writes=[hm])
                        fw.v(lambda: V.tensor_tensor(out=hm[:], in0=hm[:], in1=pu[:], op=ALU.mult), reads=[hm, pu], writes=[hm])
                        hT = hidT[s_]
                        p = ps_t[s_]
                        for f in range(4):
                            fw.tr(p, p[:, f, :], hm, hm[:, f * 128:(f + 1) * 128], idb)
                        fw.v(lambda: V.tensor_copy(out=hT[:].rearrange("p (f n) -> p f n", f=4), in_=p[:]), reads=[p], writes=[hT])
                        for dc in range(4):
                            for f in range(4):
                                first = (half == 0 and f == 0); last = (half == 1 and f == 3)
                                fw.op(fw.pe, lambda: nc.tensor.matmul(po[dc][:], lhsT=hT[:, f * 128:(f + 1) * 128], rhs=wdb[s_][:, f, dc * 512:(dc + 1) * 512], start=first, stop=last),
                                      reads=[hT, wdb[s_]], writes=[po[dc]], inc=(f == 3))
                    ob = outb[b % 2]
                    for dc in range(4):
                        if dc % 2 == 0:
                            fw.a(lambda: A.copy(out=ob[:, dc * 512:(dc + 1) * 512], in_=po[dc][:]), reads=[po[dc]], writes=[ob])
                        else:
                            fw.v(lambda: V.tensor_copy(out=ob[:, dc * 512:(dc + 1) * 512], in_=po[dc][:]), reads=[po[dc]], writes=[ob])
                    fw.dma(sp, oall_d[b * 128:(b + 1) * 128, :], ob[:], reads=[ob], writes=[oallB])
            fw.barrier()
            for st in _scope(True):
                g0 = [fw.sbuf(st, "g0_%d" % i, [128, D], F32) for i in range(2)]
                g1 = [fw.sbuf(st, "g1_%d" % i, [128, D], F32) for i in range(2)]
                hin = [fw.sbuf(st, "c_hin%d" % i, [128, D], F32) for i in range(2)]
                for t in range(NT):
                    rows = slice(t * 128, (t + 1) * 128)
                    a0, a1, hh = g0[t % 2], g1[t % 2], hin[t % 2]
                    fw.dma(sp, hh[:], h_d[rows, :], reads=[hB], writes=[hh])
                    dmaf(pool, lambda: nc.gpsimd.indirect_dma_start(out=a0[:], out_offset=None, in_=oall_d[:, :],
                                                                    in_offset=bass.IndirectOffsetOnAxis(ap=s0i[:, t:t + 1], axis=0)), reads=[oallB, s0i], writes=[a0])
                    dmaf(pool, lambda: nc.gpsimd.indirect_dma_start(out=a1[:], out_offset=None, in_=oall_d[:, :],
                                                                    in_offset=bass.IndirectOffsetOnAxis(ap=s1i[:, t:t + 1], axis=0)), reads=[oallB, s1i], writes=[a1])
                    fw.v(lambda: V.scalar_tensor_tensor(out=hh[:], in0=a0[:], scalar=W01[:, t, 0:1], in1=hh[:], op0=ALU.mult, op1=ALU.add), reads=[a0, W01, hh], writes=[hh])
                    fw.v(lambda: V.scalar_tensor_tensor(out=hh[:], in0=a1[:], scalar=W01[:, t, 1:2], in1=hh[:], op0=ALU.mult, op1=ALU.add), reads=[a1, W01, hh], writes=[hh])
                    fw.dma(sp, y[rows, :], hh[:], reads=[hh], writes=[yB])
        fw.barrier()

        for st in _scope("moe" in phases and ne > 0 and not sparse):
            xsT = fw.sbuf(st, "xsT", [128, 16, 512], BF16)
            yacc = fw.sbuf(st, "yacc", [128, 4, D], F32)
            hin = fw.sbuf(st, "hin", [128, D], F32)
            wgb = [fw.sbuf(st, "wgb%d" % i, [128, 16, 512], BF16) for i in range(2)]
            wub = [fw.sbuf(st, "wub%d" % i, [128, 16, 512], BF16) for i in range(2)]
            wdb = [fw.sbuf(st, "wdb%d" % i, [128, 4, D], BF16) for i in range(2)]
            hid = [[fw.sbuf(st, "hid%d_%d" % (i, f), [128, 512], BF16) for f in range(4)] for i in range(2)]
            pg = [fw.psum(st, "pg%d" % i, [128, 512], F32) for i in range(2)]
            pu = [fw.psum(st, "pu%d" % i, [128, 512], F32) for i in range(2)]
            pd = [fw.psum(st, "pd%d" % i, [128, 512], F32) for i in range(2)]
            it = 0
            for sti in range(NT // 4):
                for tt in range(4):
                    fw.dma(sp, xsT[:, :, tt * 128:(tt + 1) * 128], xs_d[sti * 4 + tt, :, :, :], reads=[xsB], writes=[xsT])
                fw.v(lambda: V.memset(yacc[:], 0.0), writes=[yacc])
                for e in range(ne):
                    for half in range(2):
                        s = it % 2
                        c0 = half * 512
                        r0 = (half * ne + e) * 128
                        fw.dma(pool, wgb[s][:].rearrange("p k n -> p (k n)"), wg[r0:r0 + 128, :], reads=[DB["wg"]], writes=[wgb[s]])
                        fw.dma(pool, wub[s][:].rearrange("p k n -> p (k n)"), wu[r0:r0 + 128, :], reads=[DB["wu"]], writes=[wub[s]])
                        fw.dma(pool, wdb[s][:].rearrange("p k n -> p (k n)"), wd[r0:r0 + 128, :], reads=[DB["wd"]], writes=[wdb[s]])
                        for f in range(4):
                            ps = (it * 4 + f) % 2
                            for kc in range(16):
                                fw.mm(pg[ps], pg[ps][:], wgb[s], wgb[s][:, kc, f * 128:(f + 1) * 128], xsT, xsT[:, kc, :], start=(kc == 0), stop=(kc == 15))
                            for kc in range(16):
                                fw.mm(pu[ps], pu[ps][:], wub[s], wub[s][:, kc, f * 128:(f + 1) * 128], xsT, xsT[:, kc, :], start=(kc == 0), stop=(kc == 15))
                            hs = hid[s][f]
                            fw.a(lambda: A.activation(out=hs[:], in_=pg[ps][:], func=AF.Silu), reads=[pg[ps]], writes=[hs])
                            fw.v(lambda: V.tensor_tensor(out=hs[:], in0=hs[:], in1=pu[ps][:], op=ALU.mult), reads=[hs, pu[ps]], writes=[hs])
                        j = 0
                        for tt in range(4):
                            for dc in range(4):
                                pp = pd[j % 2]; j += 1
                                for f in range(4):
                                    fw.mm(pp, pp[:], hid[s][f], hid[s][f][:, tt * 128:(tt + 1) * 128], wdb[s], wdb[s][:, f, dc * 512:(dc + 1) * 512],
                                          start=(f == 0), stop=(f == 3))
                                fw.v(lambda: V.scalar_tensor_tensor(out=yacc[:, tt, dc * 512:(dc + 1) * 512], in0=pp[:], scalar=wE[:, sti * 4 + tt, e:e + 1],
                                                                    in1=yacc[:, tt, dc * 512:(dc + 1) * 512], op0=ALU.mult, op1=ALU.add),
                                     reads=[pp, wE, yacc], writes=[yacc])
                        it += 1
                for tt in range(4):
                    rows = slice((sti * 4 + tt) * 128, (sti * 4 + tt + 1) * 128)
                    fw.dma(sp, hin[:], h_d[rows, :], reads=[hB], writes=[hin])
                    fw.v(lambda: V.tensor_tensor(out=yacc[:, tt, :], in0=yacc[:, tt, :], in1=hin[:], op=ALU.add), reads=[yacc, hin], writes=[yacc])
                    fw.dma(sp, y[rows, :], yacc[:, tt, :], reads=[yacc], writes=[yB])
        if not ("moe" in phases and ne > 0):
            for st in _scope(True):
                hin = fw.sbuf(st, "dbg_h", [128, D], F32)
                for t in range(NT):
                    fw.dma(sp, hin[:], h_d[t * 128:(t + 1) * 128, :], reads=[hB], writes=[hin])
                    fw.dma(sp, y[t * 128:(t + 1) * 128, :], hin[:], reads=[hin], writes=[yB])
        fw.finish(fw.sp, [yB])
        fw.barrier()
        stats = (fw.n_inst, fw.n_wait, fw.nsem)
    return nc, stats


def _rope_tables(pos):
    half = 16
    inv_freq = np.power(np.float32(500000.0), -np.arange(half, dtype=np.float32) * np.float32(2.0 / 32)).astype(np.float32)
    ang = pos.astype(np.float32)[:, None] * inv_freq[None, :]
    return np.cos(ang).astype(np.float32), np.sin(ang).astype(np.float32)


def make_in_maps(inputs, cores=range(NCORES), ne=NE, npre=NPRE):
    f32 = np.float32
    bf = ml_dtypes.bfloat16
    x = np.asarray(inputs["x"], f32).reshape(SEQ, D)
    w_in = np.ascontiguousarray(np.asarray(inputs["w_in"], f32)[0])
    rep = lambda v: np.ascontiguousarray(np.broadcast_to(np.asarray(v, f32).reshape(1, -1), (128, np.asarray(v).size)))
    j = np.arange(128)[:, None]; i = np.arange(128)[None, :]
    tri = np.stack([(j <= i), (j >= i), (j > i), (j < i)]).astype(f32)
    mP = np.tile((j >= i).astype(f32), (1, 4)).astype(bf)
    mN = np.tile((j <= i).astype(f32), (1, 4)).astype(bf)
    w_lr = np.zeros((D, 64), f32); w_lr[:, 0:16] = w_in[:, C_LR:C_LR + 16]; w_lr[:, 32:48] = w_in[:, C_LR + 16:C_LR + 32]
    up_pad = np.zeros((64, 512), f32)
    up_pad[0:16] = np.asarray(inputs["gla_gate_up_f"], f32)[0]; up_pad[16] = np.asarray(inputs["gla_gate_bias_f"], f32)[0]
    up_pad[32:48] = np.asarray(inputs["gla_gate_up_b"], f32)[0]; up_pad[48] = np.asarray(inputs["gla_gate_bias_b"], f32)[0]
    w_rt = np.ascontiguousarray(np.concatenate([np.asarray(inputs["w_group"], f32)[0], np.asarray(inputs["w_router"], f32)[0]], axis=1))
    b_rt = rep(np.concatenate([np.asarray(inputs["b_group"], f32)[0], np.asarray(inputs["b_router"], f32)[0]]))
    main_bi = np.zeros((64, 1), f32); main_bi[16] = 1.0; main_bi[48] = 1.0
    shared = dict(
        tri=tri, maskP=mP, maskN=mN, ident_b=np.eye(128, dtype=f32).astype(bf), ident_f=np.eye(128, dtype=f32),
        ones_c=np.ones((128, 1), f32), ones_m=np.ones((128, 128), f32), iota_p=np.arange(128, dtype=f32).reshape(128, 1), ln1_bc=rep(inputs["ln1_w"]), w_in=w_in, w_lr=w_lr,
        qn_bc=rep(inputs["q_norm_w"]), kn_bc=rep(inputs["k_norm_w"]), sink_bc=rep(inputs["attn_sink"]),
        an_bc=rep(inputs["attn_out_norm_w"]), up_pad=up_pad, gn_bc=rep(inputs["gla_out_norm_w"]),
        w_out=np.ascontiguousarray(np.asarray(inputs["w_out"], f32)[0]), ln2_bc=rep(inputs["ln2_w"]), w_rt=w_rt, b_rt=b_rt,
        main_bi=main_bi,
    )
    if ne > 0:
        gu = lambda w: np.ascontiguousarray(np.asarray(w, f32)[0][:ne].reshape(ne, 16, 128, 2, 512).transpose(3, 0, 2, 1, 4)).reshape(2 * ne * 128, 8192)
        shared.update(wg=gu(inputs["w_gate_e"]), wu=gu(inputs["w_up_e"]),
                      wd=np.ascontiguousarray(np.asarray(inputs["w_down_e"], f32)[0][:ne].reshape(ne, 2, 4, 128, D).transpose(1, 0, 3, 2, 4)).reshape(2 * ne * 128, 8192))
    maps = []
    nch = SEQ // 128
    for c in cores:
        m = dict(shared)
        lo = c * TPC - 128
        xh = np.zeros((NTH * 128, D), f32)
        a, b = max(lo, 0), min(lo + NTH * 128, SEQ)
        xh[a - lo:b - lo] = x[a:b]
        m["xh"] = xh
        pos = np.arange(lo, lo + NTH * 128)
        cs, sn = _rope_tables(pos)
        m["cosq"] = np.ascontiguousarray(np.tile(cs, (1, 8))); m["sinq"] = np.ascontiguousarray(np.tile(sn, (1, 8)))
        m["maskP0"] = mP if c > 0 else np.zeros_like(mP)
        m["maskNL"] = mN if c < NCORES - 1 else np.zeros_like(mN)
        fwd = list(range(0, c * NT))
        bwd = list(range(nch - 1, (c + 1) * NT - 1, -1))
        slots = (fwd + bwd)[:npre] if npre < NPRE else (fwd + bwd)
        n = max(npre, 1)
        xp = np.zeros((n * 128, D), f32)
        sc = np.zeros((64, n), f32); bi = np.zeros((64, n), f32); pm = np.zeros((128, n, 2), f32)
        pf = np.zeros((128, n, 3), f32); pf[:, :, 0] = 1.0
        nf_ = min(len(fwd), len(slots))
        if nf_ < len(slots):
            pf[:, nf_, 0] = 0.0
            pf[:, :, 2] = 1.0
        if nf_ > 0:
            pf[:, nf_ - 1, 1] = 1.0
        for s, ch in enumerate(slots):
            xp[s * 128:(s + 1) * 128] = x[ch * 128:(ch + 1) * 128]
            isf = s < len(fwd)
            if isf:
                sc[0:32, s] = 1.0; bi[16, s] = 1.0; pm[:, s, 0] = 1.0
            else:
                sc[32:64, s] = 1.0; bi[48, s] = 1.0; pm[:, s, 1] = 1.0
        m["xpre"] = xp; m["pre_sc"] = sc; m["pre_bi"] = bi; m["pre_m"] = pm; m["pre_f"] = pf
        maps.append(m)
    return maps


_CACHE = {}


def kernel(**inputs):
    if "nc" not in _CACHE:
        _CACHE["nc"] = build_program()[0]
    nc = _CACHE["nc"]
    maps = make_in_maps(inputs)
    res = run_bass_kernel_spmd(nc, maps, core_ids=list(range(NCORES)))
    out = np.concatenate([np.asarray(r["y"], np.float32) for r in res.results], axis=0)
    return out.reshape(1, SEQ, D)
```

```python
import math
import os
from contextlib import ExitStack

import numpy as np
import ml_dtypes
import concourse.bass as bass
import concourse.mybir as mybir
from concourse.bass_utils import run_bass_kernel_spmd

F32 = mybir.dt.float32
BF16 = mybir.dt.bfloat16
I32 = mybir.dt.int32
NBLK = 64
ALU = mybir.AluOpType
AF = mybir.ActivationFunctionType
AX = mybir.AxisListType

KDBG = os.environ.get('KDBG', '')
NCORES = 8
SEQ = 16384
D = 2048
TPC = SEQ // NCORES
NT = TPC // 128
NTH = NT + 2
NPRE = (NCORES - 1) * NT
INW = 4640
NE = 32
DFF = 1024
EPS = 1e-6
C_AQ, C_AK, C_AV, C_GQ, C_GK, C_GV, C_GR, C_LR = 0, 1024, 1280, 1536, 2048, 2560, 3584, 4608


class Eng:
    def __init__(self, fw, name, handle):
        self.name = name
        self.h = handle
        self.sem = fw.new_sem("s_" + name)
        self.count = 0
        self.seen = {}
        self.pend = []


class Buf:
    def __init__(self, fw, t, name=""):
        self.t = t
        self.name = name
        self.last_w = None
        self.reads = []
        self.dsem = None
        self.dcount = 0
        self.is_psum = False

    def __getitem__(self, idx):
        return self.t[idx]


class FW:
    def __init__(self, nc, stack):
        self.nc = nc
        self.gstack = stack
        self.nsem = 0
        self.pe = Eng(self, "pe", nc.tensor)
        self.dve = Eng(self, "dve", nc.vector)
        self.act = Eng(self, "act", nc.scalar)
        self.pool = Eng(self, "pool", nc.gpsimd)
        self.sp = Eng(self, "sp", nc.sync)
        self.pe.seen[id(self.pe.sem)] = 1 << 60
        self.engs = [self.pe, self.dve, self.act, self.pool, self.sp]
        self.dbufs = []
        self.n_inst = 0
        self.n_wait = 0

    def new_sem(self, name):
        self.nsem += 1
        return self.gstack.enter_context(self.nc.semaphore("%s_%d" % (name, self.nsem)))

    def sbuf(self, st, name, shape, dt):
        return Buf(self, st.enter_context(self.nc.sbuf_tensor(name, list(shape), dt)), name)

    def psum(self, st, name, shape, dt):
        b = Buf(self, st.enter_context(self.nc.psum_tensor(name, list(shape), dt)), name)
        b.is_psum = True
        return b

    def _wait(self, eng, dep):
        if dep is None:
            return
        sem, val = dep
        key = id(sem)
        if eng.seen.get(key, 0) >= val:
            return
        eng.h.wait_ge(sem, val)
        eng.seen[key] = val
        self.n_wait += 1

    def _deps(self, eng, reads, writes):
        for b in reads:
            self._wait(eng, b.last_w)
            if b.is_psum:
                for r in b.reads:
                    self._wait(eng, r)
        for b in writes:
            self._wait(eng, b.last_w)
            for r in b.reads:
                self._wait(eng, r)

    def op(self, eng, fn, reads=(), writes=(), inc=True):
        self._deps(eng, reads, writes)
        inst = fn()
        self.n_inst += 1
        eng.pend.append((tuple(reads), tuple(writes)))
        if not inc:
            return inst
        eng.count += 1
        inst.then_inc(eng.sem, 1)
        tok = (eng.sem, eng.count)
        for rs, ws in eng.pend:
            for b in rs:
                b.reads.append(tok)
            for b in ws:
                b.last_w = tok
                b.reads = []
        eng.pend = []
        return inst

    def v(self, fn, reads=(), writes=()):
        return self.op(self.dve, fn, reads, writes)

    def a(self, fn, reads=(), writes=()):
        return self.op(self.act, fn, reads, writes)

    def mm(self, outB, out_ap, lB, l_ap, rB, r_ap, start=True, stop=True):
        nc = self.nc
        return self.op(self.pe, lambda: nc.tensor.matmul(out_ap, lhsT=l_ap, rhs=r_ap, start=start, stop=stop),
                       reads=[lB, rB], writes=[outB], inc=stop)

    def tr(self, outB, out_ap, inB, in_ap, idB):
        nc = self.nc
        return self.op(self.pe, lambda: nc.tensor.transpose(out=out_ap, in_=in_ap, identity=idB[:]),
                       reads=[inB, idB], writes=[outB])

    def dma(self, eng, out_ap, in_ap, reads=(), writes=()):
        self._deps(eng, reads, writes)
        owner = writes[0] if writes else reads[0]
        if owner.dsem is None:
            owner.dsem = self.new_sem("d_" + owner.name)
            self.dbufs.append(owner)
        inst = eng.h.dma_start(out=out_ap, in_=in_ap)
        owner.dcount += 16
        inst.then_inc(owner.dsem, 16)
        tok = (owner.dsem, owner.dcount)
        for b in reads:
            b.reads.append(tok)
        for b in writes:
            b.last_w = tok
            b.reads = []
        self.n_inst += 1
        return inst

    def barrier(self):
        for e in self.engs:
            assert not e.pend
        for e in self.engs:
            for o in self.engs:
                if o is not e and o.count > 0:
                    self._wait(e, (o.sem, o.count))
            for b in self.dbufs:
                self._wait(e, (b.dsem, b.dcount))

    def finish(self, eng, bufs):
        for b in bufs:
            self._wait(eng, b.last_w)
            for r in b.reads:
                self._wait(eng, r)


def _scope(flag):
    if flag:
        with ExitStack() as st:
            yield st


def build_program(ne=NE, npre=NPRE, phases=("pre", "xn", "attn", "gla", "out", "moe"), sparse=True):
    nc = bass.Bass("TRN2", target_bir_lowering=False)
    dt_in = {}

    def din(name, shape, dt=F32):
        t = nc.dram_tensor(name, list(shape), dt, kind="ExternalInput")
        dt_in[name] = t
        return t

    xh = din("xh", [NTH * 128, D])
    xpre = din("xpre", [max(npre, 1) * 128, D])
    pre_sc = din("pre_sc", [64, max(npre, 1)])
    pre_bi = din("pre_bi", [64, max(npre, 1)])
    pre_m = din("pre_m", [128, max(npre, 1), 2])
    pre_f = din("pre_f", [128, max(npre, 1), 3])
    main_bi = din("main_bi", [64, 1])
    cosq = din("cosq", [NTH * 128, 128])
    sinq = din("sinq", [NTH * 128, 128])
    maskP = din("maskP", [128, 512], BF16)
    maskN = din("maskN", [128, 512], BF16)
    maskP0 = din("maskP0", [128, 512], BF16)
    maskNL = din("maskNL", [128, 512], BF16)
    tri = din("tri", [4, 128, 128])
    ident_b = din("ident_b", [128, 128], BF16)
    ident_f = din("ident_f", [128, 128])
    ones_c = din("ones_c", [128, 1])
    ln1_bc = din("ln1_bc", [128, D])
    w_in = din("w_in", [D, INW])
    w_lr = din("w_lr", [D, 64])
    qn_bc = din("qn_bc", [128, 128])
    kn_bc = din("kn_bc", [128, 128])
    sink_bc = din("sink_bc", [128, 8])
    an_bc = din("an_bc", [128, 1024])
    up_pad = din("up_pad", [64, 512])
    gn_bc = din("gn_bc", [128, 256])
    w_out = din("w_out", [D, D])
    ln2_bc = din("ln2_bc", [128, D])
    w_rt = din("w_rt", [D, 36])
    b_rt = din("b_rt", [128, 36])
    if ne > 0:
        wg = din("wg", [2 * ne * 128, 8192])
        wu = din("wu", [2 * ne * 128, 8192])
        wd = din("wd", [2 * ne * 128, 8192])
    y = nc.dram_tensor("y", [TPC, D], F32, kind="ExternalOutput")
    mix_d = nc.dram_tensor("mix_d", [TPC, D], BF16, kind="Internal")
    h_d = nc.dram_tensor("h_d", [TPC, D], F32, kind="Internal")
    xs_d = nc.dram_tensor("xs_d", [NT, 128, 16, 128], BF16, kind="Internal")
    xn2_d = nc.dram_tensor("xn2_d", [TPC, D], BF16, kind="Internal")
    xsort_d = nc.dram_tensor("xsort_d", [NBLK * 128, D], BF16, kind="Internal")
    oall_d = nc.dram_tensor("oall_d", [NBLK * 128, D], F32, kind="Internal")
    ones_m = din("ones_m", [128, 128])
    if ne > 0 and sparse:
        wgc = nc.dram_tensor("wgc", [2 * ne * 128, 8192], BF16, kind="Internal")
        wuc = nc.dram_tensor("wuc", [2 * ne * 128, 8192], BF16, kind="Internal")
        wdc = nc.dram_tensor("wdc", [2 * ne * 128, 8192], BF16, kind="Internal")
    iota_p = din("iota_p", [128, 1])

    V, A = nc.vector, nc.scalar
    QS = 1.0 / math.sqrt(128.0)

    with ExitStack() as gst:
        fw = FW(nc, gst)
        DB = {k: Buf(fw, t, k) for k, t in dt_in.items()}
        yB = Buf(fw, y, "y"); mixB = Buf(fw, mix_d, "mix_d"); hB = Buf(fw, h_d, "h_d"); xsB = Buf(fw, xs_d, "xs_d")
        xn2B = Buf(fw, xn2_d, "xn2_d"); xsortB = Buf(fw, xsort_d, "xsort_d"); oallB = Buf(fw, oall_d, "oall_d")
        conv_jobs = []
        if ne > 0 and sparse and "moe" in phases:
            wgcB = Buf(fw, wgc, "wgc"); wucB = Buf(fw, wuc, "wuc"); wdcB = Buf(fw, wdc, "wdc")
            for r0 in range(0, 2 * ne * 128, 128):
                for (src, nm, dst, dB) in ((wg, "wg", wgc, wgcB), (wu, "wu", wuc, wucB), (wd, "wd", wdc, wdcB)):
                    conv_jobs.append((src, nm, dst, dB, r0))

        def pump_conv(n):
            for _ in range(min(n, len(conv_jobs))):
                src, nm, dst, dB, r0 = conv_jobs.pop(0)
                fw.dma(pool, dst[r0:r0 + 128, :], src[r0:r0 + 128, :], reads=[DB[nm]], writes=[dB])
        sp, pool = fw.sp, fw.pool

        idb = fw.sbuf(gst, "idb", [128, 128], BF16)
        idf = fw.sbuf(gst, "idf", [128, 128], F32)
        ones = fw.sbuf(gst, "ones", [128, 1], F32)
        trif = fw.sbuf(gst, "trif", [128, 4, 128], F32)
        upp = fw.sbuf(gst, "upp", [64, 512], BF16)
        Sst = [[fw.sbuf(gst, "S_%d_%d" % (d, h), [128, 256], F32) for h in range(4)] for d in range(2)]
        wE = fw.sbuf(gst, "wE", [128, NT, NE], F32)
        A1 = fw.sbuf(gst, "A1", [128, NT, NE], F32)
        A2 = fw.sbuf(gst, "A2", [128, NT, NE], F32)
        W01 = fw.sbuf(gst, "W01", [128, NT, 2], F32)
        onesm = fw.sbuf(gst, "onesm", [128, 128], F32)
        fw.dma(sp, onesm[:], ones_m[:, :], reads=[DB["ones_m"]], writes=[onesm])
        fw.dma(sp, idb[:], ident_b[:, :], reads=[DB["ident_b"]], writes=[idb])
        fw.dma(sp, idf[:], ident_f[:, :], reads=[DB["ident_f"]], writes=[idf])
        fw.dma(sp, ones[:], ones_c[:, :], reads=[DB["ones_c"]], writes=[ones])
        fw.dma(sp, trif[:], tri.ap().rearrange("m p n -> p m n"), reads=[DB["tri"]], writes=[trif])
        fw.dma(pool, upp[:], up_pad[:, :], reads=[DB["up_pad"]], writes=[upp])
        for d in range(2):
            for h in range(4):
                fw.v(lambda: V.memset(Sst[d][h][:], 0.0), writes=[Sst[d][h]])
        trib = fw.sbuf(gst, "trib", [128, 4, 128], BF16)
        onesb = fw.sbuf(gst, "onesb", [128, 1], BF16)
        fw.v(lambda: V.tensor_copy(out=trib[:], in_=trif[:]), reads=[trif], writes=[trib])
        fw.v(lambda: V.tensor_copy(out=onesb[:], in_=ones[:]), reads=[ones], writes=[onesb])

        def rms_rstd(src_ap, srcB, junk, ss, n):
            fw.a(lambda: A.activation(out=junk[:, 0:n], in_=src_ap, func=AF.Square, accum_out=ss[:, 0:1]),
                 reads=[srcB], writes=[junk, ss])
            fw.a(lambda: A.activation(out=ss[:, 0:1], in_=ss[:, 0:1], func=AF.Sqrt, scale=1.0 / n, bias=EPS),
                 reads=[ss], writes=[ss])
            fw.v(lambda: V.reciprocal(out=ss[:, 0:1], in_=ss[:, 0:1]), reads=[ss], writes=[ss])

        def norm1_tile(st_bufs, x_src_ap, xsrcB, dstT, dst_ap_fn):
            xt, xb, junk, ss, ln1, pT = st_bufs
            fw.dma(sp, xt[:], x_src_ap, reads=[xsrcB], writes=[xt])
            rms_rstd(xt[:], xt, junk, ss, D)
            fw.v(lambda: V.scalar_tensor_tensor(out=xb[:], in0=xt[:], scalar=ss[:, 0:1], in1=ln1[:], op0=ALU.mult, op1=ALU.mult),
                 reads=[xt, ss, ln1], writes=[xb])
            for g in range(4):
                p = pT[g % 2]
                for j in range(4):
                    kc = 4 * g + j
                    fw.tr(p, p[:, j, :], xb, xb[:, kc * 128:(kc + 1) * 128], idb)
                if g % 2 == 0:
                    fw.v(lambda: V.tensor_copy(out=dst_ap_fn(4 * g), in_=p[:]), reads=[p], writes=[dstT])
                else:
                    fw.a(lambda: A.copy(out=dst_ap_fn(4 * g), in_=p[:]), reads=[p], writes=[dstT])

        if npre > 0 and "pre" in phases:
            with ExitStack() as st:
                def two(name, shape, dt):
                    return [fw.sbuf(st, "%s_%d" % (name, i), shape, dt) for i in range(2)]
                xt = two("p1_xt", [128, D], F32); xb = two("p1_xb", [128, D], BF16)
                junk = fw.sbuf(st, "p1_junk", [128, D], F32); ss = two("p1_ss", [128, 4], F32)
                ln1 = fw.sbuf(st, "p1_ln1", [128, D], F32)
                xT = two("p1_xT", [128, 16, 128], BF16)
                wk = fw.sbuf(st, "p1_wk", [128, 16, 512], BF16)
                wv = fw.sbuf(st, "p1_wv", [128, 16, 1024], BF16)
                wl = fw.sbuf(st, "p1_wl", [128, 16, 64], BF16)
                psc = fw.sbuf(st, "p1_psc", [64, npre], F32); pbi = fw.sbuf(st, "p1_pbi", [64, npre], F32)
                pm = fw.sbuf(st, "p1_pm", [128, npre, 2], F32)
                gk = two("p1_gk", [128, 512], F32); gv = two("p1_gv", [128, 1024], BF16)
                lrT = two("p1_lrT", [64, 128], BF16)
                spf = two("p1_sp", [128, 512], BF16); spe = two("p1_spe", [128, 512], F32); e3 = two("p1_e3", [128, 512], F32)
                kend = two("p1_kend", [128, 512], BF16)
                tsel = two("p1_tsel", [128, 128], BF16); ttmp = two("p1_ttmp", [128, 128], F32)
                dec = two("p1_dec", [128, 4], F32); dd = two("p1_dd", [128, 2, 4], F32)
                om = two("p1_om", [128, 2], F32)
                dsm = two("p1_dsm", [128, 256], F32)
                pT = [fw.psum(st, "p1_pT%d" % i, [128, 4, 128], BF16) for i in range(2)]
                ps_k = fw.psum(st, "p1_psk", [128, 512], F32)
                ps_v = [fw.psum(st, "p1_psv%d" % i, [128, 512], F32) for i in range(2)]
                ps_l = fw.psum(st, "p1_psl", [64, 128], F32)
                ps_g = fw.psum(st, "p1_psg", [128, 512], F32)
                ps_d = [fw.psum(st, "p1_psd0", [128, 512], F32)] * 2
                pf = fw.sbuf(st, "p1_pf", [128, npre, 3], F32)
                Scur = [fw.sbuf(st, "p1_Scur%d" % h, [128, 256], F32) for h in range(4)]
                for h in range(4):
                    fw.v(lambda: V.memset(Scur[h][:], 0.0), writes=[Scur[h]])
                fw.dma(sp, pf[:], pre_f[:, :, :], reads=[DB["pre_f"]], writes=[pf])
                fw.dma(sp, ln1[:], ln1_bc[:, :], reads=[DB["ln1_bc"]], writes=[ln1])
                fw.dma(sp, psc[:], pre_sc[:, :], reads=[DB["pre_sc"]], writes=[psc])
                fw.dma(sp, pbi[:], pre_bi[:, :], reads=[DB["pre_bi"]], writes=[pbi])
                fw.dma(sp, pm[:], pre_m[:, :, :], reads=[DB["pre_m"]], writes=[pm])
                fw.dma(pool, wk[:], w_in[:, C_GK:C_GK + 512].rearrange("(kc p) n -> p kc n", p=128), reads=[DB["w_in"]], writes=[wk])
                fw.dma(pool, wv[:], w_in[:, C_GV:C_GV + 1024].rearrange("(kc p) n -> p kc n", p=128), reads=[DB["w_in"]], writes=[wv])
                fw.dma(pool, wl[:], w_lr.ap().rearrange("(kc p) n -> p kc n", p=128), reads=[DB["w_lr"]], writes=[wl])

                def front(s):
                    q = s % 2
                    xTs = xT[q]
                    xt_, xb_, ss_ = xt[q], xb[q], ss[q]
                    fw.dma(sp, xt_[:], xpre[s * 128:(s + 1) * 128, :], reads=[DB["xpre"]], writes=[xt_])
                    rms_rstd(xt_[:], xt_, junk, ss_, D)
                    fw.v(lambda: V.scalar_tensor_tensor(out=xb_[:], in0=xt_[:], scalar=ss_[:, 0:1], in1=ln1[:], op0=ALU.mult, op1=ALU.mult),
                         reads=[xt_, ss_, ln1], writes=[xb_])
                    yield
                    for g in range(4):
                        p = pT[g % 2]
                        for j in range(4):
                            kc = 4 * g + j
                            fw.tr(p, p[:, j, :], xb_, xb_[:, kc * 128:(kc + 1) * 128], idb)
                        if g % 2 == 0:
                            fw.v(lambda: V.tensor_copy(out=xTs[:, 4 * g:4 * g + 4, :], in_=p[:]), reads=[p], writes=[xTs])
                        else:
                            fw.a(lambda: A.copy(out=xTs[:, 4 * g:4 * g + 4, :], in_=p[:]), reads=[p], writes=[xTs])
                        yield
                    for kc in range(16):
                        fw.mm(ps_k, ps_k[:], xTs, xTs[:, kc, :], wk, wk[:, kc, :], start=(kc == 0), stop=(kc == 15))
                    fw.a(lambda: A.copy(out=gk[q][:], in_=ps_k[:]), reads=[ps_k], writes=[gk[q]])
                    yield
                    for half in range(2):
                        for kc in range(16):
                            fw.mm(ps_v[half], ps_v[half][:], xTs, xTs[:, kc, :], wv, wv[:, kc, half * 512:(half + 1) * 512],
                                  start=(kc == 0), stop=(kc == 15))
                        fw.v(lambda: V.tensor_copy(out=gv[q][:, half * 512:(half + 1) * 512], in_=ps_v[half][:]), reads=[ps_v[half]], writes=[gv[q]])
                        yield
                    for kc in range(16):
                        fw.mm(ps_l, ps_l[:], wl, wl[:, kc, :], xTs, xTs[:, kc, :], start=(kc == 0), stop=(kc == 15))
                    fw.v(lambda: V.tensor_scalar(out=lrT[q][:], in0=ps_l[:], scalar1=psc[:, s:s + 1], scalar2=pbi[:, s:s + 1],
                                                 op0=ALU.mult, op1=ALU.add), reads=[ps_l, psc, pbi], writes=[lrT[q]])
                    fw.v(lambda: V.tensor_scalar(out=ttmp[q][:], in0=trif[:, 3, :], scalar1=pm[:, s, 1:2], scalar2=None, op0=ALU.mult),
                         reads=[trif, pm], writes=[ttmp[q]])
                    fw.v(lambda: V.scalar_tensor_tensor(out=tsel[q][:], in0=trif[:, 2, :], scalar=pm[:, s, 0:1], in1=ttmp[q][:],
                                                        op0=ALU.mult, op1=ALU.add), reads=[trif, pm, ttmp[q]], writes=[tsel[q]])
                    yield

                def back(s):
                    q = s % 2
                    fw.mm(ps_g, ps_g[:], lrT[q], lrT[q][:], upp, upp[:])
                    yield
                    fw.a(lambda: A.activation(out=spe[q][:], in_=ps_g[:], func=AF.Exp, scale=-1.0), reads=[ps_g], writes=[spe[q]])
                    fw.a(lambda: A.activation(out=spf[q][:], in_=spe[q][:], func=AF.Ln, bias=1.0), reads=[spe[q]], writes=[spf[q]])
                    yield
                    fw.mm(ps_g, ps_g[:], tsel[q], tsel[q][:], spf[q], spf[q][:])
                    yield
                    fw.a(lambda: A.activation(out=e3[q][:], in_=ps_g[:], func=AF.Exp, scale=-1.0 / 16), reads=[ps_g], writes=[e3[q]])
                    fw.v(lambda: V.tensor_tensor(out=kend[q][:], in0=gk[q][:], in1=e3[q][:], op=ALU.mult), reads=[gk[q], e3[q]], writes=[kend[q]])
                    yield
                    for h in range(4):
                        fw.op(fw.pe, lambda: nc.tensor.matmul(ps_g[:, h:h + 1], lhsT=spf[q][:, h * 128:(h + 1) * 128], rhs=onesb[:, 0:1], start=True, stop=True),
                              reads=[spf[q], onesb], writes=[ps_g], inc=(h == 3))
                    yield
                    fw.a(lambda: A.activation(out=dec[q][:], in_=ps_g[:, 0:4], func=AF.Exp, scale=-1.0 / 16), reads=[ps_g], writes=[dec[q]])
                    fw.v(lambda: V.tensor_scalar(out=dd[q][:, 0, :], in0=dec[q][:], scalar1=pf[:, s, 0:1], scalar2=None, op0=ALU.mult),
                         reads=[dec[q], pf], writes=[dd[q]])
                    yield
                    for h in range(4):
                        pd = ps_d[0]
                        fw.mm(pd, pd[:, 0:256], kend[q], kend[q][:, h * 128:(h + 1) * 128], gv[q], gv[q][:, h * 256:(h + 1) * 256])
                        fw.v(lambda: V.scalar_tensor_tensor(out=Scur[h][:], in0=Scur[h][:], scalar=dd[q][:, 0, h:h + 1], in1=pd[:, 0:256],
                                                            op0=ALU.mult, op1=ALU.add), reads=[Scur[h], dd[q], pd], writes=[Scur[h]])
                        fw.v(lambda: V.scalar_tensor_tensor(out=Sst[0][h][:], in0=Scur[h][:], scalar=pf[:, s, 1:2], in1=Sst[0][h][:],
                                                            op0=ALU.mult, op1=ALU.add), reads=[Scur[h], pf, Sst[0][h]], writes=[Sst[0][h]])
                        if h % 2 == 1:
                            yield

                def zip_emit(*gens):
                    gens = [g for g in gens if g is not None]
                    while gens:
                        for g in list(gens):
                            try:
                                next(g)
                            except StopIteration:
                                gens.remove(g)

                zip_emit(front(0))
                for s in range(npre):
                    zip_emit(back(s), front(s + 1) if s + 1 < npre else None)
                    pump_conv(1)
                for h in range(4):
                    fw.v(lambda: V.tensor_scalar(out=Sst[1][h][:], in0=Scur[h][:], scalar1=pf[:, 0, 2:3], scalar2=None, op0=ALU.mult),
                         reads=[Scur[h], pf], writes=[Sst[1][h]])
            fw.barrier()

        with ExitStack() as st2:
            xnT = fw.sbuf(st2, "xnT", [128, 16, NTH * 128], BF16)
            for st in _scope("xn" in phases):
                xt = fw.sbuf(st, "p2_xt", [128, D], F32); xb = fw.sbuf(st, "p2_xb", [128, D], BF16)
                junk = fw.sbuf(st, "p2_junk", [128, D], F32); ss = fw.sbuf(st, "p2_ss", [128, 4], F32)
                ln1 = fw.sbuf(st, "p2_ln1", [128, D], F32)
                pT = [fw.psum(st, "p2_pT%d" % i, [128, 4, 128], BF16) for i in range(2)]
                fw.dma(sp, ln1[:], ln1_bc[:, :], reads=[DB["ln1_bc"]], writes=[ln1])
                for t in range(NTH):
                    norm1_tile((xt, xb, junk, ss, ln1, pT), xh[t * 128:(t + 1) * 128, :], DB["xh"], xnT,
                               lambda kc: xnT[:, kc:kc + 4, t * 128:(t + 1) * 128])
                    pump_conv(1)
            fw.barrier()

            for st in _scope("attn" in phases):
                aqT = fw.sbuf(st, "aqT", [128, NT, 1024], BF16)
                akT = fw.sbuf(st, "akT", [128, 2, NTH * 128], BF16)
                av = fw.sbuf(st, "av", [128, NTH, 2, 129], BF16)
                wq = [fw.sbuf(st, "wq%d" % i, [128, 16, 512], BF16) for i in range(2)]
                qn = fw.sbuf(st, "qn", [128, 128], F32); kn = fw.sbuf(st, "kn", [128, 128], F32)
                cs = fw.sbuf(st, "cs", [128, 128], F32); sn = fw.sbuf(st, "sn", [128, 128], F32)
                qf = fw.sbuf(st, "qf", [128, 512], F32); qb = fw.sbuf(st, "qb", [128, 512], BF16)
                junk = fw.sbuf(st, "a_junk", [128, 1024], F32); ss = fw.sbuf(st, "a_ss", [128, 8], F32)
                r1 = fw.sbuf(st, "r1", [128, 4, 16], F32); r2 = fw.sbuf(st, "r2", [128, 4, 16], F32)
                r3 = fw.sbuf(st, "r3", [128, 4, 16], F32); r4 = fw.sbuf(st, "r4", [128, 4, 16], F32)
                mP = fw.sbuf(st, "mP", [128, 512], BF16); mN = fw.sbuf(st, "mN", [128, 512], BF16)
                mP0 = fw.sbuf(st, "mP0", [128, 512], BF16); mNL = fw.sbuf(st, "mNL", [128, 512], BF16)
                esk = fw.sbuf(st, "esk", [128, 8], F32)
                anw = fw.sbuf(st, "anw", [128, 1024], F32)
                pTt = [fw.sbuf(st, "pTt%d" % i, [128, 512], BF16) for i in range(3)]
                ao = fw.sbuf(st, "ao", [128, 1024], F32); aob = fw.sbuf(st, "aob", [128, 1024], BF16)
                den = fw.sbuf(st, "den", [128, 4], F32)
                pst1 = ExitStack()
                ps_q = [fw.psum(pst1, "ps_q%d" % i, [128, 512], F32) for i in range(2)]
                ps_t = [fw.psum(pst1, "ps_t%d" % i, [128, 4, 128], BF16) for i in range(2)]
                for (dst, src, nm) in ((qn, qn_bc, "qn_bc"), (kn, kn_bc, "kn_bc"), (esk, sink_bc, "sink_bc"), (anw, an_bc, "an_bc")):
                    fw.dma(sp, dst[:], src[:, :], reads=[DB[nm]], writes=[dst])
                for (dst, src, nm) in ((mP, maskP, "maskP"), (mN, maskN, "maskN"), (mP0, maskP0, "maskP0"), (mNL, maskNL, "maskNL")):
                    fw.dma(sp, dst[:], src[:, :], reads=[DB[nm]], writes=[dst])
                fw.a(lambda: A.activation(out=esk[:], in_=esk[:], func=AF.Exp), reads=[esk], writes=[esk])
                fw.v(lambda: V.memset(av[:], 1.0), writes=[av])

                def qk_post(ps, nh, nw, dstT, dst_fn, t):
                    for h in range(nh):
                        fw.a(lambda: A.activation(out=junk[:, 0:128], in_=ps[:, h * 128:(h + 1) * 128], func=AF.Square, accum_out=ss[:, h:h + 1]),
                             reads=[ps], writes=[junk, ss])
                    fw.a(lambda: A.activation(out=ss[:, 0:nh], in_=ss[:, 0:nh], func=AF.Sqrt, scale=1.0 / 128, bias=EPS), reads=[ss], writes=[ss])
                    fw.v(lambda: V.reciprocal(out=ss[:, 0:nh], in_=ss[:, 0:nh]), reads=[ss], writes=[ss])
                    for h in range(nh):
                        fw.v(lambda: V.scalar_tensor_tensor(out=qf[:, h * 128:(h + 1) * 128], in0=ps[:, h * 128:(h + 1) * 128], scalar=ss[:, h:h + 1],
                                                            in1=nw[:], op0=ALU.mult, op1=ALU.mult), reads=[ps, ss, nw], writes=[qf])
                    q3 = qf[:, 0:nh * 128].rearrange("p (h d) -> p h d", d=128)
                    c3 = cs[:, 0:nh * 16].rearrange("p (h d) -> p h d", d=16)
                    s3 = sn[:, 0:nh * 16].rearrange("p (h d) -> p h d", d=16)
                    x1, x2 = q3[:, :, 0:16], q3[:, :, 16:32]
                    fw.v(lambda: V.tensor_tensor(out=r1[:, 0:nh, :], in0=x1, in1=c3, op=ALU.mult), reads=[qf, cs], writes=[r1])
                    fw.v(lambda: V.tensor_tensor(out=r2[:, 0:nh, :], in0=x2, in1=s3, op=ALU.mult), reads=[qf, sn], writes=[r2])
                    fw.v(lambda: V.tensor_tensor(out=r3[:, 0:nh, :], in0=x2, in1=c3, op=ALU.mult), reads=[qf, cs], writes=[r3])
                    fw.v(lambda: V.tensor_tensor(out=r4[:, 0:nh, :], in0=x1, in1=s3, op=ALU.mult), reads=[qf, sn], writes=[r4])
                    fw.v(lambda: V.tensor_tensor(out=x1, in0=r1[:, 0:nh, :], in1=r2[:, 0:nh, :], op=ALU.subtract), reads=[r1, r2], writes=[qf])
                    fw.v(lambda: V.tensor_tensor(out=x2, in0=r3[:, 0:nh, :], in1=r4[:, 0:nh, :], op=ALU.add), reads=[r3, r4], writes=[qf])
                    fw.a(lambda: A.copy(out=qb[:, 0:nh * 128], in_=qf[:, 0:nh * 128]), reads=[qf], writes=[qb])
                    p = ps_t[t % 2]
                    for h in range(nh):
                        fw.tr(p, p[:, h, :], qb, qb[:, h * 128:(h + 1) * 128], idb)
                    fw.v(lambda: V.tensor_copy(out=dst_fn(None), in_=p[:, 0:nh, :]), reads=[p], writes=[dstT])

                groups = [(C_AQ, "q", 0), (C_AQ + 512, "q", 4), (C_AK, "kv", 0)]
                for gi, (c0, kind, h0) in enumerate(groups):
                    w = wq[gi % 2]
                    fw.dma(pool, w[:], w_in[:, c0:c0 + 512].rearrange("(kc p) n -> p kc n", p=128), reads=[DB["w_in"]], writes=[w])
                    tiles = range(1, NT + 1) if kind == "q" else range(NTH)
                    for t in tiles:
                        if t % 2 == 0:
                            pump_conv(1)
                        ps = ps_q[t % 2]
                        for kc in range(16):
                            fw.mm(ps, ps[:], xnT, xnT[:, kc, t * 128:(t + 1) * 128], w, w[:, kc, :], start=(kc == 0), stop=(kc == 15))
                        fw.dma(sp, cs[:], cosq[t * 128:(t + 1) * 128, :], reads=[DB["cosq"]], writes=[cs])
                        fw.dma(sp, sn[:], sinq[t * 128:(t + 1) * 128, :], reads=[DB["sinq"]], writes=[sn])
                        if kind == "q":
                            qk_post(ps, 4, qn, aqT, lambda h: aqT[:, t - 1, h0 * 128:(h0 + 4) * 128].rearrange("p (h d) -> p h d", h=4), t)
                        else:
                            qk_post(ps, 2, kn, akT, lambda h: akT[:, 0:2, t * 128:(t + 1) * 128], t)
                            for hk in range(2):
                                fw.a(lambda: A.copy(out=av[:, t, hk, 0:128], in_=ps[:, 256 + hk * 128:256 + (hk + 1) * 128]), reads=[ps], writes=[av])

                fw.barrier()
                pst1.close()
                ps_s = [fw.psum(st, "ps_s%d" % i, [128, 512], F32) for i in range(3)]
                ps_o = [fw.psum(st, "ps_o%d" % i, [128, 2, 129], F32) for i in range(2)]
                for i in range(NT):
                    pump_conv(1)
                    for hk in range(2):
                        for kb in range(3):
                            kt = i + kb
                            fw.mm(ps_s[kb], ps_s[kb][:], akT, akT[:, hk, kt * 128:(kt + 1) * 128],
                                  aqT, aqT[:, i, hk * 512:(hk + 1) * 512])
                            fw.a(lambda: A.activation(out=pTt[kb][:], in_=ps_s[kb][:], func=AF.Exp, scale=QS), reads=[ps_s[kb]], writes=[pTt[kb]])
                        mk = mP0 if i == 0 else mP
                        fw.v(lambda: V.tensor_tensor(out=pTt[0][:], in0=pTt[0][:], in1=mk[:], op=ALU.mult), reads=[pTt[0], mk], writes=[pTt[0]])
                        mk2 = mNL if i == NT - 1 else mN
                        fw.v(lambda: V.tensor_tensor(out=pTt[2][:], in0=pTt[2][:], in1=mk2[:], op=ALU.mult), reads=[pTt[2], mk2], writes=[pTt[2]])
                        for g in range(4):
                            po = ps_o[g // 2]
                            for kb in range(3):
                                kt = i + kb
                                fw.mm(po, po[:, g % 2, :], pTt[kb], pTt[kb][:, g * 128:(g + 1) * 128], av, av[:, kt, hk, :],
                                      start=(kb == 0), stop=(kb == 2))
                        for g in range(4):
                            po = ps_o[g // 2]
                            hq = 4 * hk + g
                            fw.v(lambda: V.tensor_scalar(out=den[:, g:g + 1], in0=po[:, g % 2, 128:129], scalar1=esk[:, hq:hq + 1], scalar2=None, op0=ALU.add),
                                 reads=[po, esk], writes=[den])
                        fw.v(lambda: V.reciprocal(out=den[:], in_=den[:]), reads=[den], writes=[den])
                        for g in range(4):
                            po = ps_o[g // 2]
                            hq = 4 * hk + g
                            fw.v(lambda: V.tensor_scalar(out=ao[:, hq * 128:(hq + 1) * 128], in0=po[:, g % 2, 0:128], scalar1=den[:, g:g + 1], scalar2=None, op0=ALU.mult),
                                 reads=[po, den], writes=[ao])
                    rms_rstd(ao[:], ao, junk, ss, 1024)
                    fw.v(lambda: V.scalar_tensor_tensor(out=aob[:], in0=ao[:], scalar=ss[:, 0:1], in1=anw[:], op0=ALU.mult, op1=ALU.mult),
                         reads=[ao, ss, anw], writes=[aob])
                    fw.dma(sp, mix_d[i * 128:(i + 1) * 128, 0:1024], aob[:], reads=[aob], writes=[mixB])
            fw.barrier()

            for st in _scope("gla" in phases):
                wa = [fw.sbuf(st, "g_w%d" % i, [128, 16, 256], BF16) for i in range(2)]
                wl = fw.sbuf(st, "g_wl", [128, 16, 64], BF16)
                mbi = fw.sbuf(st, "g_mbi", [64, 1], F32)
                lrT = fw.sbuf(st, "g_lrT", [64, TPC], BF16)
                gqT = fw.sbuf(st, "g_qT", [128, TPC], BF16); gkT = fw.sbuf(st, "g_kT", [128, TPC], BF16)
                gkt = fw.sbuf(st, "g_kt", [128, NT, 128], F32)
                gvt = fw.sbuf(st, "g_vt", [128, NT, 256], BF16)
                srt = fw.sbuf(st, "g_sr", [128, NT, 256], BF16)
                oacc2 = [fw.sbuf(st, "g_oacc%d" % i, [128, NT, 256], F32) for i in range(2)]
                gnw = fw.sbuf(st, "g_nw", [128, 256], F32)
                def two(name, shape, dt):
                    return [fw.sbuf(st, "%s_%d" % (name, i), shape, dt) for i in range(2)]
                spf2 = two("g_sp", [128, 128], BF16); spe2 = two("g_spe", [128, 128], F32)
                e1s = two("g_e1", [128, 128], F32); e2s = two("g_e2", [128, 128], F32); e3s = two("g_e3", [128, 128], F32)
                qds = two("g_qd", [128, 128], BF16); kis = two("g_ki", [128, 128], BF16); kes = two("g_ke", [128, 128], BF16)
                ams = two("g_am", [128, 128], BF16)
                decs = two("g_dec", [128, 1], F32)
                Sbs = two("g_Sb", [128, 256], BF16)
                junk = fw.sbuf(st, "g_junk", [128, 256], F32); ss = fw.sbuf(st, "g_ss", [128, 4], F32)
                ob = fw.sbuf(st, "g_ob", [128, 256], BF16); of = fw.sbuf(st, "g_of", [128, 256], F32)
                PS = [fw.psum(st, "g_ps%d" % i, [128, 512], F32) for i in range(8)]
                ps_p = [PS[0], PS[4]]
                fw.dma(sp, gnw[:], gn_bc[:, :], reads=[DB["gn_bc"]], writes=[gnw])
                fw.dma(sp, mbi[:], main_bi[:, :], reads=[DB["main_bi"]], writes=[mbi])
                fw.dma(pool, wl[:], w_lr.ap().rearrange("(kc p) n -> p kc n", p=128), reads=[DB["w_lr"]], writes=[wl])
                XO = 128
                for nt in range(TPC // 512):
                    pl = ps_p[nt % 2]
                    for kc in range(16):
                        fw.mm(pl, pl[0:64, :], wl, wl[:, kc, :], xnT, xnT[:, kc, XO + nt * 512:XO + (nt + 1) * 512], start=(kc == 0), stop=(kc == 15))
                    fw.v(lambda: V.tensor_scalar(out=lrT[:, nt * 512:(nt + 1) * 512], in0=pl[0:64, :], scalar1=mbi[:, 0:1], scalar2=None, op0=ALU.add),
                         reads=[pl, mbi], writes=[lrT])
                wi = 0
                for h in range(4):
                    pump_conv(4)
                    w = wa[wi % 2]; wi += 1
                    fw.dma(pool, w[:, :, 0:128], w_in[:, C_GQ + h * 128:C_GQ + (h + 1) * 128].rearrange("(kc p) n -> p kc n", p=128), reads=[DB["w_in"]], writes=[w])
                    fw.dma(pool, w[:, :, 128:256], w_in[:, C_GK + h * 128:C_GK + (h + 1) * 128].rearrange("(kc p) n -> p kc n", p=128), reads=[DB["w_in"]], writes=[w])
                    for nt in range(TPC // 512):
                        for (j, dst) in ((0, gqT), (1, gkT)):
                            pl = ps_p[j]
                            for kc in range(16):
                                fw.mm(pl, pl[:], w, w[:, kc, j * 128:(j + 1) * 128], xnT, xnT[:, kc, XO + nt * 512:XO + (nt + 1) * 512], start=(kc == 0), stop=(kc == 15))
                            fw.a(lambda: A.copy(out=dst[:, nt * 512:(nt + 1) * 512], in_=pl[:]), reads=[pl], writes=[dst])
                    for t in range(NT):
                        pl = ps_p[t % 2]
                        for kc in range(16):
                            fw.mm(pl, pl[:, 0:128], xnT, xnT[:, kc, XO + t * 128:XO + (t + 1) * 128], w, w[:, kc, 128:256], start=(kc == 0), stop=(kc == 15))
                        fw.v(lambda: V.tensor_copy(out=gkt[:, t, :], in_=pl[:, 0:128]), reads=[pl], writes=[gkt])
                    for (c0, dst, isr) in ((C_GV + h * 256, gvt, False), (C_GR + h * 256, srt, True)):
                        w2 = wa[wi % 2]; wi += 1
                        fw.dma(pool, w2[:], w_in[:, c0:c0 + 256].rearrange("(kc p) n -> p kc n", p=128), reads=[DB["w_in"]], writes=[w2])
                        for t in range(NT):
                            pl = ps_p[t % 2]
                            for kc in range(16):
                                fw.mm(pl, pl[:, 0:256], xnT, xnT[:, kc, XO + t * 128:XO + (t + 1) * 128], w2, w2[:, kc, :], start=(kc == 0), stop=(kc == 15))
                            if isr:
                                fw.a(lambda: A.activation(out=dst[:, t, :], in_=pl[:, 0:256], func=AF.Silu), reads=[pl], writes=[dst])
                            else:
                                fw.v(lambda: V.tensor_copy(out=dst[:, t, :], in_=pl[:, 0:256]), reads=[pl], writes=[dst])
                    def chain(d):
                        S = Sst[d][h]
                        order = range(NT) if d == 0 else range(NT - 1, -1, -1)
                        last = 127 if d == 0 else 0
                        pA, pR, pO, pD = PS[4 * d], PS[4 * d + 1], PS[4 * d + 2], PS[4 * d + 3]
                        spe_, spf_, e1_, e2_, e3_ = spe2[d], spf2[d], e1s[d], e2s[d], e3s[d]
                        qd_, ki_, ke_, am_, dec_, Sb_, oa_ = qds[d], kis[d], kes[d], ams[d], decs[d], Sbs[d], oacc2[d]
                        for n in order:
                            tk = slice(n * 128, (n + 1) * 128)
                            fw.a(lambda: A.copy(out=Sb_[:], in_=S[:]), reads=[S], writes=[Sb_])
                            fw.mm(pA, pA[:, 0:128], lrT, lrT[32 * d:32 * d + 32, tk], upp, upp[32 * d:32 * d + 32, h * 128:(h + 1) * 128])
                            yield
                            fw.a(lambda: A.activation(out=spe_[:], in_=pA[:, 0:128], func=AF.Exp, scale=-1.0), reads=[pA], writes=[spe_])
                            fw.a(lambda: A.activation(out=spf_[:], in_=spe_[:], func=AF.Ln, bias=1.0), reads=[spe_], writes=[spf_])
                            yield
                            fw.mm(pA, pA[:, 0:128], spf_, spf_[:], trib, trib[:, d, :])
                            fw.mm(pR, pR[:, 0:128], trib, trib[:, 2 + d, :], spf_, spf_[:])
                            yield
                            fw.a(lambda: A.activation(out=e1_[:], in_=pA[:, 0:128], func=AF.Exp, scale=-1.0 / 16), reads=[pA], writes=[e1_])
                            fw.a(lambda: A.activation(out=e2_[:], in_=pA[:, 0:128], func=AF.Exp, scale=1.0 / 16), reads=[pA], writes=[e2_])
                            fw.a(lambda: A.activation(out=e3_[:], in_=pR[:, 0:128], func=AF.Exp, scale=-1.0 / 16), reads=[pR], writes=[e3_])
                            fw.a(lambda: A.copy(out=dec_[:], in_=e1_[:, last:last + 1]), reads=[e1_], writes=[dec_])
                            yield
                            fw.v(lambda: V.scalar_tensor_tensor(out=qd_[:], in0=gqT[:, tk], scalar=QS, in1=e1_[:], op0=ALU.mult, op1=ALU.mult),
                                 reads=[gqT, e1_], writes=[qd_])
                            fw.v(lambda: V.tensor_tensor(out=ki_[:], in0=gkT[:, tk], in1=e2_[:], op=ALU.mult), reads=[gkT, e2_], writes=[ki_])
                            fw.v(lambda: V.tensor_tensor(out=ke_[:], in0=gkt[:, n, :], in1=e3_[:], op=ALU.mult), reads=[gkt, e3_], writes=[ke_])
                            yield
                            fw.mm(pA, pA[:, 0:128], ki_, ki_[:], qd_, qd_[:])
                            yield
                            fw.v(lambda: V.tensor_tensor(out=am_[:], in0=pA[:, 0:128], in1=trif[:, d, :], op=ALU.mult), reads=[pA, trif], writes=[am_])
                            yield
                            fw.mm(pO, pO[:, 0:256], am_, am_[:], gvt, gvt[:, n, :], start=True, stop=False)
                            fw.mm(pO, pO[:, 0:256], qd_, qd_[:], Sb_, Sb_[:], start=False, stop=True)
                            fw.mm(pD, pD[:, 0:256], ke_, ke_[:], gvt, gvt[:, n, :])
                            yield
                            fw.a(lambda: A.copy(out=oa_[:, n, :], in_=pO[:, 0:256]), reads=[pO], writes=[oa_])
                            fw.v(lambda: V.scalar_tensor_tensor(out=S[:], in0=S[:], scalar=dec_[:, 0:1], in1=pD[:, 0:256], op0=ALU.mult, op1=ALU.add),
                                 reads=[S, dec_, pD], writes=[S])
                            yield

                    gens = [chain(0), chain(1)]
                    while gens:
                        for g_ in list(gens):
                            try:
                                next(g_)
                            except StopIteration:
                                gens.remove(g_)
                    for n in range(NT):
                        fw.v(lambda: V.tensor_tensor(out=of[:], in0=oacc2[0][:, n, :], in1=oacc2[1][:, n, :], op=ALU.add), reads=[oacc2[0], oacc2[1]], writes=[of])
                        rms_rstd(of[:], of, junk, ss, 256)
                        fw.v(lambda: V.scalar_tensor_tensor(out=of[:], in0=of[:], scalar=ss[:, 0:1], in1=gnw[:], op0=ALU.mult, op1=ALU.mult),
                             reads=[of, ss, gnw], writes=[of])
                        fw.v(lambda: V.tensor_tensor(out=ob[:], in0=of[:], in1=srt[:, n, :], op=ALU.mult), reads=[of, srt], writes=[ob])
                        fw.dma(sp, mix_d[n * 128:(n + 1) * 128, 1024 + h * 256:1024 + (h + 1) * 256], ob[:], reads=[ob], writes=[mixB])
            fw.barrier()

        for st in _scope("out" in phases):
            wo = fw.sbuf(st, "wo", [128, 16, D], BF16)
            wr = fw.sbuf(st, "wr", [128, 16, 36], F32)
            brt = fw.sbuf(st, "brt", [128, 36], F32)
            ln2 = fw.sbuf(st, "ln2", [128, D], F32)
            def two(name, shape, dt):
                return [fw.sbuf(st, "%s_%d" % (name, i), shape, dt) for i in range(2)]
            junk = fw.sbuf(st, "o_junk", [128, D], F32)
            PAR = dict(mt=two("mt", [128, D], BF16), mT=two("mT", [128, 16, 128], BF16), xt=two("o_xt", [128, D], F32),
                       ht=two("o_ht", [128, D], F32), ss=two("o_ss", [128, 4], F32), xn2=two("o_xn2", [128, D], F32),
                       xTf=two("o_xTf", [128, 16, 128], F32), xTb=two("o_xTb", [128, 16, 128], BF16), lg=two("lg", [128, 36], F32),
                       gm=two("gm", [128, 8], F32), ohg=two("ohg", [128, 4], F32), ge=two("ge", [128, 4], F32),
                       ig=two("ig", [128, 8], F32), tmp8=two("tmp8", [128, 8], F32), oh1=two("oh1", [128, 8], F32),
                       oh2=two("oh2", [128, 8], F32), w8=two("w8", [128, 8], F32))
            ps_t = [fw.psum(st, "o_pst%d" % i, [128, 4, 128], BF16) for i in range(2)]
            ps_h = [fw.psum(st, "o_psh%d" % i, [128, 512], F32) for i in range(4)]
            ps_f = [fw.psum(st, "o_psf%d" % i, [128, 4, 128], F32) for i in range(1)]
            ps_l = fw.psum(st, "o_psl", [128, 36], F32)
            for half in range(2):
                fw.dma(pool, wo[:, :, half * 1024:(half + 1) * 1024], w_out[:, half * 1024:(half + 1) * 1024].rearrange("(kc p) n -> p kc n", p=128),
                       reads=[DB["w_out"]], writes=[wo])
            fw.dma(sp, wr[:], w_rt.ap().rearrange("(kc p) n -> p kc n", p=128), reads=[DB["w_rt"]], writes=[wr])
            fw.dma(sp, brt[:], b_rt[:, :], reads=[DB["b_rt"]], writes=[brt])
            fw.dma(sp, ln2[:], ln2_bc[:, :], reads=[DB["ln2_bc"]], writes=[ln2])
            def tile_gen(t):
                mt, mT, xt, ht, ss, xn2, xTf, xTb, lg, gm, ohg, ge, ig, tmp8, oh1, oh2, w8 = [PAR[k][t % 2] for k in (
                    "mt", "mT", "xt", "ht", "ss", "xn2", "xTf", "xTb", "lg", "gm", "ohg", "ge", "ig", "tmp8", "oh1", "oh2", "w8")]
                rows = slice(t * 128, (t + 1) * 128)
                pump_conv(1)
                fw.dma(sp, mt[:], mix_d[rows, :], reads=[mixB], writes=[mt])
                fw.dma(sp, xt[:], xh[128 + t * 128:128 + (t + 1) * 128, :], reads=[DB["xh"]], writes=[xt])
                for g in range(4):
                    p = ps_t[g % 2]
                    for j in range(4):
                        kc = 4 * g + j
                        fw.tr(p, p[:, j, :], mt, mt[:, kc * 128:(kc + 1) * 128], idb)
                    if g % 2 == 0:
                        fw.v(lambda: V.tensor_copy(out=mT[:, 4 * g:4 * g + 4, :], in_=p[:]), reads=[p], writes=[mT])
                    else:
                        fw.a(lambda: A.copy(out=mT[:, 4 * g:4 * g + 4, :], in_=p[:]), reads=[p], writes=[mT])
                    if g % 2 == 1:
                        yield
                for dc in range(4):
                    for kc in range(16):
                        fw.mm(ps_h[dc], ps_h[dc][:], mT, mT[:, kc, :], wo, wo[:, kc, dc * 512:(dc + 1) * 512], start=(kc == 0), stop=(kc == 15))
                    fw.v(lambda: V.tensor_tensor(out=ht[:, dc * 512:(dc + 1) * 512], in0=ps_h[dc][:], in1=xt[:, dc * 512:(dc + 1) * 512], op=ALU.add),
                         reads=[ps_h[dc], xt], writes=[ht])
                    yield
                fw.dma(sp, h_d[rows, :], ht[:], reads=[ht], writes=[hB])
                rms_rstd(ht[:], ht, junk, ss, D)
                fw.v(lambda: V.scalar_tensor_tensor(out=xn2[:], in0=ht[:], scalar=ss[:, 0:1], in1=ln2[:], op0=ALU.mult, op1=ALU.mult),
                     reads=[ht, ss, ln2], writes=[xn2])
                yield
                for g in range(4):
                    p = ps_f[0]
                    for j in range(4):
                        kc = 4 * g + j
                        fw.mm(p, p[:, j, :], xn2, xn2[:, kc * 128:(kc + 1) * 128], idf, idf[:])
                    fw.a(lambda: A.copy(out=xTf[:, 4 * g:4 * g + 4, :], in_=p[:]), reads=[p], writes=[xTf])
                    if not sparse:
                        fw.v(lambda: V.tensor_copy(out=xTb[:, 4 * g:4 * g + 4, :], in_=p[:]), reads=[p], writes=[xTb])
                    yield
                if sparse:
                    fw.a(lambda: A.copy(out=mt[:], in_=xn2[:]), reads=[xn2], writes=[mt])
                    fw.dma(sp, xn2_d[rows, :], mt[:], reads=[mt], writes=[xn2B])
                else:
                    fw.dma(sp, xs_d[t, :, :, :], xTb[:], reads=[xTb], writes=[xsB])
                for kc in range(16):
                    fw.mm(ps_l, ps_l[:], xTf, xTf[:, kc, :], wr, wr[:, kc, :], start=(kc == 0), stop=(kc == 15))
                fw.v(lambda: V.tensor_tensor(out=lg[:], in0=ps_l[:], in1=brt[:], op=ALU.add), reads=[ps_l, brt], writes=[lg])
                yield
                fw.v(lambda: V.tensor_reduce(out=gm[:, 0:1], in_=lg[:, 0:4], axis=AX.X, op=ALU.max), reads=[lg], writes=[gm])
                fw.v(lambda: V.tensor_scalar(out=ohg[:], in0=lg[:, 0:4], scalar1=gm[:, 0:1], scalar2=None, op0=ALU.is_equal), reads=[lg, gm], writes=[ohg])
                fw.v(lambda: V.tensor_scalar(out=ge[:], in0=lg[:, 0:4], scalar1=gm[:, 0:1], scalar2=None, op0=ALU.subtract), reads=[lg, gm], writes=[ge])
                fw.a(lambda: A.activation(out=ge[:], in_=ge[:], func=AF.Exp, accum_out=gm[:, 1:2]), reads=[ge], writes=[ge, gm])
                fw.v(lambda: V.reciprocal(out=gm[:, 2:3], in_=gm[:, 1:2]), reads=[gm], writes=[gm])
                yield
                fw.v(lambda: V.tensor_scalar(out=ig[:], in0=lg[:, 4:12], scalar1=ohg[:, 0:1], scalar2=None, op0=ALU.mult), reads=[lg, ohg], writes=[ig])
                for g in range(1, 4):
                    fw.v(lambda: V.scalar_tensor_tensor(out=ig[:], in0=lg[:, 4 + 8 * g:12 + 8 * g], scalar=ohg[:, g:g + 1], in1=ig[:], op0=ALU.mult, op1=ALU.add),
                         reads=[lg, ohg, ig], writes=[ig])
                yield
                fw.v(lambda: V.tensor_reduce(out=gm[:, 3:4], in_=ig[:], axis=AX.X, op=ALU.max), reads=[ig], writes=[gm])
                fw.v(lambda: V.tensor_scalar(out=oh1[:], in0=ig[:], scalar1=gm[:, 3:4], scalar2=None, op0=ALU.is_equal), reads=[ig, gm], writes=[oh1])
                fw.v(lambda: V.scalar_tensor_tensor(out=tmp8[:], in0=oh1[:], scalar=-1e30, in1=ig[:], op0=ALU.mult, op1=ALU.add), reads=[oh1, ig], writes=[tmp8])
                fw.v(lambda: V.tensor_reduce(out=gm[:, 4:5], in_=tmp8[:], axis=AX.X, op=ALU.max), reads=[tmp8], writes=[gm])
                fw.v(lambda: V.tensor_scalar(out=oh2[:], in0=tmp8[:], scalar1=gm[:, 4:5], scalar2=None, op0=ALU.is_equal), reads=[tmp8, gm], writes=[oh2])
                yield
                fw.v(lambda: V.tensor_tensor(out=gm[:, 5:6], in0=gm[:, 4:5], in1=gm[:, 3:4], op=ALU.subtract), reads=[gm], writes=[gm])
                fw.a(lambda: A.activation(out=gm[:, 5:6], in_=gm[:, 5:6], func=AF.Exp), reads=[gm], writes=[gm])
                fw.v(lambda: V.tensor_scalar(out=gm[:, 6:7], in0=gm[:, 5:6], scalar1=1.0, scalar2=None, op0=ALU.add), reads=[gm], writes=[gm])
                fw.v(lambda: V.reciprocal(out=gm[:, 6:7], in_=gm[:, 6:7]), reads=[gm], writes=[gm])
                fw.v(lambda: V.tensor_tensor(out=gm[:, 6:7], in0=gm[:, 6:7], in1=gm[:, 2:3], op=ALU.mult), reads=[gm], writes=[gm])
                fw.v(lambda: V.tensor_tensor(out=gm[:, 7:8], in0=gm[:, 6:7], in1=gm[:, 5:6], op=ALU.mult), reads=[gm], writes=[gm])
                fw.v(lambda: V.tensor_scalar(out=w8[:], in0=oh1[:], scalar1=gm[:, 6:7], scalar2=None, op0=ALU.mult), reads=[oh1, gm], writes=[w8])
                fw.v(lambda: V.scalar_tensor_tensor(out=w8[:], in0=oh2[:], scalar=gm[:, 7:8], in1=w8[:], op0=ALU.mult, op1=ALU.add), reads=[oh2, gm, w8], writes=[w8])
                for g in range(4):
                    fw.v(lambda: V.tensor_scalar(out=wE[:, t, g * 8:(g + 1) * 8], in0=w8[:], scalar1=ohg[:, g:g + 1], scalar2=None, op0=ALU.mult),
                         reads=[w8, ohg], writes=[wE])
                    fw.v(lambda: V.tensor_scalar(out=A1[:, t, g * 8:(g + 1) * 8], in0=oh1[:], scalar1=ohg[:, g:g + 1], scalar2=None, op0=ALU.mult),
                         reads=[oh1, ohg], writes=[A1])
                    fw.v(lambda: V.tensor_scalar(out=A2[:, t, g * 8:(g + 1) * 8], in0=oh2[:], scalar1=ohg[:, g:g + 1], scalar2=None, op0=ALU.mult),
                         reads=[oh2, ohg], writes=[A2])
                fw.v(lambda: V.tensor_copy(out=W01[:, t, :], in_=gm[:, 6:8]), reads=[gm], writes=[W01])
                yield

            gens, t_next, rounds = [], 0, 0
            while gens or t_next < NT:
                if t_next < NT and len(gens) < 2 and (not gens or rounds % 8 == 0):
                    gens.append(tile_gen(t_next)); t_next += 1
                for g_ in list(gens):
                    try:
                        next(g_)
                    except StopIteration:
                        gens.remove(g_)
                rounds += 1
        fw.barrier()


        def dmaf(eng, fn, reads, writes):
            fw._deps(eng, reads, writes)
            owner = writes[0]
            if owner.dsem is None:
                owner.dsem = fw.new_sem("d_" + owner.name)
                fw.dbufs.append(owner)
            inst = fn()
            owner.dcount += 16
            inst.then_inc(owner.dsem, 16)
            tok = (owner.dsem, owner.dcount)
            for b in reads:
                b.reads.append(tok)
            for b in writes:
                b.last_w = tok
                b.reads = []
            fw.n_inst += 1

        pump_conv(len(conv_jobs))
        for st4 in _scope("moe" in phases and ne > 0 and sparse):
            s0i = fw.sbuf(st4, "s0i", [128, NT], I32); s1i = fw.sbuf(st4, "s1i", [128, NT], I32)
            ebi = fw.sbuf(st4, "ebi", [128, 2, NBLK], I32)
            iop = fw.sbuf(st4, "iop", [128, 1], F32)
            fw.dma(sp, iop[:], iota_p[:, :], reads=[DB["iota_p"]], writes=[iop])
            for st in _scope(True):
                Aa = fw.sbuf(st, "Aa", [128, NT, NE], F32)
                Acum = fw.sbuf(st, "Acum", [128, NE], F32)
                Pr = fw.sbuf(st, "Pr", [128, NT, NE], F32)
                cnt = fw.sbuf(st, "cnt", [128, NE], F32); pad = fw.sbuf(st, "pad", [128, NE], F32)
                sa = fw.sbuf(st, "sa", [128, NE], F32); sb = fw.sbuf(st, "sb", [128, NE], F32)
                pst = fw.sbuf(st, "pst", [128, NE], F32); pen = fw.sbuf(st, "pen", [128, NE], F32)
                tmpA = fw.sbuf(st, "tmpA", [128, NT, NE], F32)
                s0f = fw.sbuf(st, "s0f", [128, NT], F32); s1f = fw.sbuf(st, "s1f", [128, NT], F32)
                ebf = fw.sbuf(st, "ebf", [128, NBLK], F32); cmpb = fw.sbuf(st, "cmpb", [128, NE], F32)
                ps_r = fw.psum(st, "r_ps", [128, NE], F32)
                fw.v(lambda: V.tensor_tensor(out=Aa[:], in0=A1[:], in1=A2[:], op=ALU.add), reads=[A1, A2], writes=[Aa])
                fw.v(lambda: V.memset(Acum[:], 0.0), writes=[Acum])
                for t in range(NT):
                    fw.mm(ps_r, ps_r[:], trif, trif[:, 3, :], Aa, Aa[:, t, :], start=True, stop=False)
                    fw.mm(ps_r, ps_r[:], onesm, onesm[:], Acum, Acum[:], start=False, stop=True)
                    fw.a(lambda: A.copy(out=Pr[:, t, :], in_=ps_r[:]), reads=[ps_r], writes=[Pr])
                    fw.v(lambda: V.tensor_tensor(out=Acum[:], in0=Acum[:], in1=Aa[:, t, :], op=ALU.add), reads=[Acum, Aa], writes=[Acum])
                fw.mm(ps_r, ps_r[:], onesm, onesm[:], Acum, Acum[:])
                fw.a(lambda: A.copy(out=cnt[:], in_=ps_r[:]), reads=[ps_r], writes=[cnt])
                fw.v(lambda: V.tensor_scalar(out=sa[:], in0=cnt[:], scalar1=1.0 / 128, scalar2=0.49609375, op0=ALU.mult, op1=ALU.add), reads=[cnt], writes=[sa])
                fw.v(lambda: V.tensor_scalar(out=sb[:], in0=sa[:], scalar1=8388608.0, scalar2=None, op0=ALU.add), reads=[sa], writes=[sb])
                fw.v(lambda: V.tensor_scalar(out=sa[:], in0=sb[:], scalar1=-8388608.0, scalar2=None, op0=ALU.add), reads=[sb], writes=[sa])
                fw.v(lambda: V.tensor_scalar(out=pad[:], in0=sa[:], scalar1=128.0, scalar2=None, op0=ALU.mult), reads=[sa], writes=[pad])
                fw.v(lambda: V.tensor_copy(out=sa[:], in_=pad[:]), reads=[pad], writes=[sa])
                cur, nxt = sa, sb
                for sh in (1, 2, 4, 8, 16):
                    fw.v(lambda: V.tensor_copy(out=nxt[:, 0:sh], in_=cur[:, 0:sh]), reads=[cur], writes=[nxt])
                    fw.v(lambda: V.tensor_tensor(out=nxt[:, sh:NE], in0=cur[:, sh:NE], in1=cur[:, 0:NE - sh], op=ALU.add), reads=[cur], writes=[nxt])
                    cur, nxt = nxt, cur
                fw.v(lambda: V.tensor_copy(out=pen[:], in_=cur[:]), reads=[cur], writes=[pen])
                fw.v(lambda: V.tensor_tensor(out=pst[:], in0=pen[:], in1=pad[:], op=ALU.subtract), reads=[pen, pad], writes=[pst])
                for t in range(NT):
                    fw.v(lambda: V.tensor_tensor(out=Pr[:, t, :], in0=Pr[:, t, :], in1=pst[:], op=ALU.add), reads=[Pr, pst], writes=[Pr])
                for (Ak, sf, si) in ((A1, s0f, s0i), (A2, s1f, s1i)):
                    fw.v(lambda: V.tensor_tensor(out=tmpA[:], in0=Ak[:], in1=Pr[:], op=ALU.mult), reads=[Ak, Pr], writes=[tmpA])
                    fw.v(lambda: V.tensor_reduce(out=sf[:], in_=tmpA[:], axis=AX.X, op=ALU.add), reads=[tmpA], writes=[sf])
                    fw.v(lambda: V.tensor_copy(out=si[:], in_=sf[:]), reads=[sf], writes=[si])
                for b in range(NBLK):
                    fw.v(lambda: V.tensor_scalar(out=cmpb[:], in0=pen[:], scalar1=float(128 * b), scalar2=None, op0=ALU.is_le), reads=[pen], writes=[cmpb])
                    fw.v(lambda: V.tensor_reduce(out=ebf[:, b:b + 1], in_=cmpb[:], axis=AX.X, op=ALU.add), reads=[cmpb], writes=[ebf])
                fw.v(lambda: V.tensor_scalar(out=ebf[:], in0=ebf[:], scalar1=float(ne - 1), scalar2=None, op0=ALU.min), reads=[ebf], writes=[ebf])
                fw.v(lambda: V.tensor_scalar(out=ebf[:], in0=ebf[:], scalar1=128.0, scalar2=iop[:, 0:1], op0=ALU.mult, op1=ALU.add), reads=[ebf, iop], writes=[ebf])
                fw.v(lambda: V.tensor_copy(out=ebi[:, 0, :], in_=ebf[:]), reads=[ebf], writes=[ebi])
                fw.v(lambda: V.tensor_scalar(out=ebf[:], in0=ebf[:], scalar1=float(ne * 128), scalar2=None, op0=ALU.add), reads=[ebf], writes=[ebf])
                fw.v(lambda: V.tensor_copy(out=ebi[:, 1, :], in_=ebf[:]), reads=[ebf], writes=[ebi])
            fw.barrier()
            for st in _scope(True):
                xrow = [fw.sbuf(st, "xrow%d" % i, [128, D], BF16) for i in range(2)]
                for t in range(NT):
                    xr = xrow[t % 2]
                    fw.dma(sp, xr[:], xn2_d[t * 128:(t + 1) * 128, :], reads=[xn2B], writes=[xr])
                    for si in (s0i, s1i):
                        dmaf(pool, lambda: nc.gpsimd.indirect_dma_start(out=xsort_d[:, :], out_offset=bass.IndirectOffsetOnAxis(ap=si[:, t:t + 1], axis=0),
                                                                        in_=xr[:], in_offset=None), reads=[xr, si], writes=[xsortB])
            fw.barrier()
            for st in _scope(True):
                xblk = [fw.sbuf(st, "xblk%d" % i, [128, D], BF16) for i in range(2)]
                xsT = [fw.sbuf(st, "xsT%d" % i, [128, 16, 128], BF16) for i in range(2)]
                wgb = [fw.sbuf(st, "wgb%d" % i, [128, 16, 512], BF16) for i in range(2)]
                wub = [fw.sbuf(st, "wub%d" % i, [128, 16, 512], BF16) for i in range(2)]
                wdb = [fw.sbuf(st, "wdb%d" % i, [128, 4, D], BF16) for i in range(2)]
                hidT = [fw.sbuf(st, "hidT%d" % i, [128, 512], BF16) for i in range(2)]
                outb = [fw.sbuf(st, "outb%d" % i, [128, D], F32) for i in range(2)]
                ps_t = [fw.psum(st, "m_pst%d" % i, [128, 4, 128], BF16) for i in range(2)]
                hidm = [fw.sbuf(st, "hidm%d" % i, [128, 512], BF16) for i in range(2)]
                pg = fw.psum(st, "m_pg", [128, 512], F32); pu = fw.psum(st, "m_pu", [128, 512], F32)
                po = [fw.psum(st, "m_po%d" % i, [128, 512], F32) for i in range(4)]
                it = 0
                for b in range(NBLK):
                    xb_, xT_ = xblk[b % 2], xsT[b % 2]
                    if b == 0:
                        fw.dma(sp, xb_[:], xsort_d[0:128, :], reads=[xsortB], writes=[xb_])
                    if b + 1 < NBLK:
                        xn_ = xblk[(b + 1) % 2]
                        fw.dma(sp, xn_[:], xsort_d[(b + 1) * 128:(b + 2) * 128, :], reads=[xsortB], writes=[xn_])
                    for g in range(4):
                        p = ps_t[g % 2]
                        for j in range(4):
                            kc = 4 * g + j
                            fw.tr(p, p[:, j, :], xb_, xb_[:, kc * 128:(kc + 1) * 128], idb)
                        if g % 2 == 0:
                            fw.v(lambda: V.tensor_copy(out=xT_[:, 4 * g:4 * g + 4, :], in_=p[:]), reads=[p], writes=[xT_])
                        else:
                            fw.a(lambda: A.copy(out=xT_[:, 4 * g:4 * g + 4, :], in_=p[:]), reads=[p], writes=[xT_])
                    for half in range(2):
                        s_ = it % 2; it += 1
                        for (tab, tB, dstb) in ((wgc, wgcB, wgb[s_]), (wuc, wucB, wub[s_]), (wdc, wdcB, wdb[s_])):
                            dmaf(pool, lambda: nc.gpsimd.indirect_dma_start(out=dstb[:].rearrange("p k n -> p (k n)"), out_offset=None, in_=tab[:, :],
                                                                            in_offset=bass.IndirectOffsetOnAxis(ap=ebi[:, half, b:b + 1], axis=0)),
                                 reads=[tB, ebi], writes=[dstb])
                        for kc in range(16):
                            fw.mm(pg, pg[:], xT_, xT_[:, kc, :], wgb[s_], wgb[s_][:, kc, :], start=(kc == 0), stop=(kc == 15))
                        for kc in range(16):
                            fw.mm(pu, pu[:], xT_, xT_[:, kc, :], wub[s_], wub[s_][:, kc, :], start=(kc == 0), stop=(kc == 15))
                        hm = hidm[s_]
                        fw.a(lambda: A.activation(out=hm[:], in_=pg[:], func=AF.Silu), reads=[pg], writes=[hm])
                        fw.v(lambda: V.tensor_tensor(out=hm[:], in0=hm[:], in1=pu[:], op=ALU.mult), reads=[hm, pu], writes=[hm])
                        hT = hidT[s_]
                        p = ps_t[s_]
                        for f in range(4):
                            fw.tr(p, p[:, f, :], hm, hm[:, f * 128:(f + 1) * 128], idb)
                        fw.v(lambda: V.tensor_copy(out=hT[:].rearrange("p (f n) -> p f n", f=4), in_=p[:]), reads=[p], writes=[hT])
                        for dc in range(4):
                            for f in range(4):
                                first = (half == 0 and f == 0); last = (half == 1 and f == 3)
                                fw.op(fw.pe, lambda: nc.tensor.matmul(po[dc][:], lhsT=hT[:, f * 128:(f + 1) * 128], rhs=wdb[s_][:, f, dc * 512:(dc + 1) * 512], start=first, stop=last),
                                      reads=[hT, wdb[s_]], writes=[po[dc]], inc=(f == 3))
                    ob = outb[b % 2]
                    for dc in range(4):
                        if dc % 2 == 0:
                            fw.a(lambda: A.copy(out=ob[:, dc * 512:(dc + 1) * 512], in_=po[dc][:]), reads=[po[dc]], writes=[ob])
                        else:
                            fw.v(lambda: V.tensor_copy(out=ob[:, dc * 512:(dc + 1) * 512], in_=po[dc][:]), reads=[po[dc]], writes=[ob])
                    fw.dma(sp, oall_d[b * 128:(b + 1) * 128, :], ob[:], reads=[ob], writes=[oallB])
            fw.barrier()
            for st in _scope(True):
                g0 = [fw.sbuf(st, "g0_%d" % i, [128, D], F32) for i in range(2)]
                g1 = [fw.sbuf(st, "g1_%d" % i, [128, D], F32) for i in range(2)]
                hin = [fw.sbuf(st, "c_hin%d" % i, [128, D], F32) for i in range(2)]
                for t in range(NT):
                    rows = slice(t * 128, (t + 1) * 128)
                    a0, a1, hh = g0[t % 2], g1[t % 2], hin[t % 2]
                    fw.dma(sp, hh[:], h_d[rows, :], reads=[hB], writes=[hh])
                    dmaf(pool, lambda: nc.gpsimd.indirect_dma_start(out=a0[:], out_offset=None, in_=oall_d[:, :],
                                                                    in_offset=bass.IndirectOffsetOnAxis(ap=s0i[:, t:t + 1], axis=0)), reads=[oallB, s0i], writes=[a0])
                    dmaf(pool, lambda: nc.gpsimd.indirect_dma_start(out=a1[:], out_offset=None, in_=oall_d[:, :],
                                                                    in_offset=bass.IndirectOffsetOnAxis(ap=s1i[:, t:t + 1], axis=0)), reads=[oallB, s1i], writes=[a1])
                    fw.v(lambda: V.scalar_tensor_tensor(out=hh[:], in0=a0[:], scalar=W01[:, t, 0:1], in1=hh[:], op0=ALU.mult, op1=ALU.add), reads=[a0, W01, hh], writes=[hh])
                    fw.v(lambda: V.scalar_tensor_tensor(out=hh[:], in0=a1[:], scalar=W01[:, t, 1:2], in1=hh[:], op0=ALU.mult, op1=ALU.add), reads=[a1, W01, hh], writes=[hh])
                    fw.dma(sp, y[rows, :], hh[:], reads=[hh], writes=[yB])
        fw.barrier()

        for st in _scope("moe" in phases and ne > 0 and not sparse):
            xsT = fw.sbuf(st, "xsT", [128, 16, 512], BF16)
            yacc = fw.sbuf(st, "yacc", [128, 4, D], F32)
            hin = fw.sbuf(st, "hin", [128, D], F32)
            wgb = [fw.sbuf(st, "wgb%d" % i, [128, 16, 512], BF16) for i in range(2)]
            wub = [fw.sbuf(st, "wub%d" % i, [128, 16, 512], BF16) for i in range(2)]
            wdb = [fw.sbuf(st, "wdb%d" % i, [128, 4, D], BF16) for i in range(2)]
            hid = [[fw.sbuf(st, "hid%d_%d" % (i, f), [128, 512], BF16) for f in range(4)] for i in range(2)]
            pg = [fw.psum(st, "pg%d" % i, [128, 512], F32) for i in range(2)]
            pu = [fw.psum(st, "pu%d" % i, [128, 512], F32) for i in range(2)]
            pd = [fw.psum(st, "pd%d" % i, [128, 512], F32) for i in range(2)]
            it = 0
            for sti in range(NT // 4):
                for tt in range(4):
                    fw.dma(sp, xsT[:, :, tt * 128:(tt + 1) * 128], xs_d[sti * 4 + tt, :, :, :], reads=[xsB], writes=[xsT])
                fw.v(lambda: V.memset(yacc[:], 0.0), writes=[yacc])
                for e in range(ne):
                    for half in range(2):
                        s = it % 2
                        c0 = half * 512
                        r0 = (half * ne + e) * 128
                        fw.dma(pool, wgb[s][:].rearrange("p k n -> p (k n)"), wg[r0:r0 + 128, :], reads=[DB["wg"]], writes=[wgb[s]])
                        fw.dma(pool, wub[s][:].rearrange("p k n -> p (k n)"), wu[r0:r0 + 128, :], reads=[DB["wu"]], writes=[wub[s]])
                        fw.dma(pool, wdb[s][:].rearrange("p k n -> p (k n)"), wd[r0:r0 + 128, :], reads=[DB["wd"]], writes=[wdb[s]])
                        for f in range(4):
                            ps = (it * 4 + f) % 2
                            for kc in range(16):
                                fw.mm(pg[ps], pg[ps][:], wgb[s], wgb[s][:, kc, f * 128:(f + 1) * 128], xsT, xsT[:, kc, :], start=(kc == 0), stop=(kc == 15))
                            for kc in range(16):
                                fw.mm(pu[ps], pu[ps][:], wub[s], wub[s][:, kc, f * 128:(f + 1) * 128], xsT, xsT[:, kc, :], start=(kc == 0), stop=(kc == 15))
                            hs = hid[s][f]
                            fw.a(lambda: A.activation(out=hs[:], in_=pg[ps][:], func=AF.Silu), reads=[pg[ps]], writes=[hs])
                            fw.v(lambda: V.tensor_tensor(out=hs[:], in0=hs[:], in1=pu[ps][:], op=ALU.mult), reads=[hs, pu[ps]], writes=[hs])
                        j = 0
                        for tt in range(4):
                            for dc in range(4):
                                pp = pd[j % 2]; j += 1
                                for f in range(4):
                                    fw.mm(pp, pp[:], hid[s][f], hid[s][f][:, tt * 128:(tt + 1) * 128], wdb[s], wdb[s][:, f, dc * 512:(dc + 1) * 512],
                                          start=(f == 0), stop=(f == 3))
                                fw.v(lambda: V.scalar_tensor_tensor(out=yacc[:, tt, dc * 512:(dc + 1) * 512], in0=pp[:], scalar=wE[:, sti * 4 + tt, e:e + 1],
                                                                    in1=yacc[:, tt, dc * 512:(dc + 1) * 512], op0=ALU.mult, op1=ALU.add),
                                     reads=[pp, wE, yacc], writes=[yacc])
                        it += 1
                for tt in range(4):
                    rows = slice((sti * 4 + tt) * 128, (sti * 4 + tt + 1) * 128)
                    fw.dma(sp, hin[:], h_d[rows, :], reads=[hB], writes=[hin])
                    fw.v(lambda: V.tensor_tensor(out=yacc[:, tt, :], in0=yacc[:, tt, :], in1=hin[:], op=ALU.add), reads=[yacc, hin], writes=[yacc])
                    fw.dma(sp, y[rows, :], yacc[:, tt, :], reads=[yacc], writes=[yB])
        if not ("moe" in phases and ne > 0):
            for st in _scope(True):
                hin = fw.sbuf(st, "dbg_h", [128, D], F32)
                for t in range(NT):
                    fw.dma(sp, hin[:], h_d[t * 128:(t + 1) * 128, :], reads=[hB], writes=[hin])
                    fw.dma(sp, y[t * 128:(t + 1) * 128, :], hin[:], reads=[hin], writes=[yB])
        fw.finish(fw.sp, [yB])
        fw.barrier()
        stats = (fw.n_inst, fw.n_wait, fw.nsem)
    return nc, stats


def _rope_tables(pos):
    half = 16
    inv_freq = np.power(np.float32(500000.0), -np.arange(half, dtype=np.float32) * np.float32(2.0 / 32)).astype(np.float32)
    ang = pos.astype(np.float32)[:, None] * inv_freq[None, :]
    return np.cos(ang).astype(np.float32), np.sin(ang).astype(np.float32)


def make_in_maps(inputs, cores=range(NCORES), ne=NE, npre=NPRE):
    f32 = np.float32
    bf = ml_dtypes.bfloat16
    x = np.asarray(inputs["x"], f32).reshape(SEQ, D)
    w_in = np.ascontiguousarray(np.asarray(inputs["w_in"], f32)[0])
    rep = lambda v: np.ascontiguousarray(np.broadcast_to(np.asarray(v, f32).reshape(1, -1), (128, np.asarray(v).size)))
    j = np.arange(128)[:, None]; i = np.arange(128)[None, :]
    tri = np.stack([(j <= i), (j >= i), (j > i), (j < i)]).astype(f32)
    mP = np.tile((j >= i).astype(f32), (1, 4)).astype(bf)
    mN = np.tile((j <= i).astype(f32), (1, 4)).astype(bf)
    w_lr = np.zeros((D, 64), f32); w_lr[:, 0:16] = w_in[:, C_LR:C_LR + 16]; w_lr[:, 32:48] = w_in[:, C_LR + 16:C_LR + 32]
    up_pad = np.zeros((64, 512), f32)
    up_pad[0:16] = np.asarray(inputs["gla_gate_up_f"], f32)[0]; up_pad[16] = np.asarray(inputs["gla_gate_bias_f"], f32)[0]
    up_pad[32:48] = np.asarray(inputs["gla_gate_up_b"], f32)[0]; up_pad[48] = np.asarray(inputs["gla_gate_bias_b"], f32)[0]
    w_rt = np.ascontiguousarray(np.concatenate([np.asarray(inputs["w_group"], f32)[0], np.asarray(inputs["w_router"], f32)[0]], axis=1))
    b_rt = rep(np.concatenate([np.asarray(inputs["b_group"], f32)[0], np.asarray(inputs["b_router"], f32)[0]]))
    main_bi = np.zeros((64, 1), f32); main_bi[16] = 1.0; main_bi[48] = 1.0
    shared = dict(
        tri=tri, maskP=mP, maskN=mN, ident_b=np.eye(128, dtype=f32).astype(bf), ident_f=np.eye(128, dtype=f32),
        ones_c=np.ones((128, 1), f32), ones_m=np.ones((128, 128), f32), iota_p=np.arange(128, dtype=f32).reshape(128, 1), ln1_bc=rep(inputs["ln1_w"]), w_in=w_in, w_lr=w_lr,
        qn_bc=rep(inputs["q_norm_w"]), kn_bc=rep(inputs["k_norm_w"]), sink_bc=rep(inputs["attn_sink"]),
        an_bc=rep(inputs["attn_out_norm_w"]), up_pad=up_pad, gn_bc=rep(inputs["gla_out_norm_w"]),
        w_out=np.ascontiguousarray(np.asarray(inputs["w_out"], f32)[0]), ln2_bc=rep(inputs["ln2_w"]), w_rt=w_rt, b_rt=b_rt,
        main_bi=main_bi,
    )
    if ne > 0:
        gu = lambda w: np.ascontiguousarray(np.asarray(w, f32)[0][:ne].reshape(ne, 16, 128, 2, 512).transpose(3, 0, 2, 1, 4)).reshape(2 * ne * 128, 8192)
        shared.update(wg=gu(inputs["w_gate_e"]), wu=gu(inputs["w_up_e"]),
                      wd=np.ascontiguousarray(np.asarray(inputs["w_down_e"], f32)[0][:ne].reshape(ne, 2, 4, 128, D).transpose(1, 0, 3, 2, 4)).reshape(2 * ne * 128, 8192))
    maps = []
    nch = SEQ // 128
    for c in cores:
        m = dict(shared)
        lo = c * TPC - 128
        xh = np.zeros((NTH * 128, D), f32)
        a, b = max(lo, 0), min(lo + NTH * 128, SEQ)
        xh[a - lo:b - lo] = x[a:b]
        m["xh"] = xh
        pos = np.arange(lo, lo + NTH * 128)
        cs, sn = _rope_tables(pos)
        m["cosq"] = np.ascontiguousarray(np.tile(cs, (1, 8))); m["sinq"] = np.ascontiguousarray(np.tile(sn, (1, 8)))
        m["maskP0"] = mP if c > 0 else np.zeros_like(mP)
        m["maskNL"] = mN if c < NCORES - 1 else np.zeros_like(mN)
        fwd = list(range(0, c * NT))
        bwd = list(range(nch - 1, (c + 1) * NT - 1, -1))
        slots = (fwd + bwd)[:npre] if npre < NPRE else (fwd + bwd)
        n = max(npre, 1)
        xp = np.zeros((n * 128, D), f32)
        sc = np.zeros((64, n), f32); bi = np.zeros((64, n), f32); pm = np.zeros((128, n, 2), f32)
        pf = np.zeros((128, n, 3), f32); pf[:, :, 0] = 1.0
        nf_ = min(len(fwd), len(slots))
        if nf_ < len(slots):
            pf[:, nf_, 0] = 0.0
            pf[:, :, 2] = 1.0
        if nf_ > 0:
            pf[:, nf_ - 1, 1] = 1.0
        for s, ch in enumerate(slots):
            xp[s * 128:(s + 1) * 128] = x[ch * 128:(ch + 1) * 128]
            isf = s < len(fwd)
            if isf:
                sc[0:32, s] = 1.0; bi[16, s] = 1.0; pm[:, s, 0] = 1.0
            else:
                sc[32:64, s] = 1.0; bi[48, s] = 1.0; pm[:, s, 1] = 1.0
        m["xpre"] = xp; m["pre_sc"] = sc; m["pre_bi"] = bi; m["pre_m"] = pm; m["pre_f"] = pf
        maps.append(m)
    return maps


_CACHE = {}


def kernel(**inputs):
    if "nc" not in _CACHE:
        _CACHE["nc"] = build_program()[0]
    nc = _CACHE["nc"]
    maps = make_in_maps(inputs)
    res = run_bass_kernel_spmd(nc, maps, core_ids=list(range(NCORES)))
    out = np.concatenate([np.asarray(r["y"], np.float32) for r in res.results], axis=0)
    return out.reshape(1, SEQ, D)
```
